# Optimizing a Trainium2 kernel written in Bass

```python
import math
import jax, jax.numpy as jnp
from jax import lax
import numpy as np

D_MODEL = 1024
BATCH = 8
SEQ = 4096
DEPTH = 1

MLSTM_HEADS = 4
MLSTM_HEAD_DIM = 256
MLSTM_WIDTH = MLSTM_HEADS * MLSTM_HEAD_DIM
MLSTM_CHUNK = 128
N_GATE_COLS = 4 * MLSTM_HEADS
LRU_WIDTH = D_MODEL
LRU_BLOCKS = 8
LRU_BLOCK_DIM = LRU_WIDTH // LRU_BLOCKS
LRU_C = 8.0
CONV_WIDTH = 4
CONV_PAD = (2, 1)
PEER_HEADS = 8
PEER_N_KEYS = 128
PEER_N_EXPERTS = PEER_N_KEYS * PEER_N_KEYS
PEER_QUERY_DIM = 256
PEER_HALF = PEER_QUERY_DIM // 2
PEER_TOPK = 16
PEER_TOKEN_BLOCK = 128
EPS = 1e-6

IN_SPLIT_SIZES = (MLSTM_WIDTH, MLSTM_WIDTH, N_GATE_COLS, LRU_WIDTH, LRU_WIDTH, 2 * D_MODEL)
D_IN = MLSTM_WIDTH * 2 + N_GATE_COLS + LRU_WIDTH * 2 + 2 * D_MODEL

kernel_name = "hybrid_mlstm_rglru_peer_encoder"


def rms_norm(x, g):
    x32 = x.astype(jnp.float32)
    y = x32 * lax.rsqrt(jnp.mean(x32 * x32, axis=-1, keepdims=True) + EPS)
    return (y * g.astype(jnp.float32)).astype(x.dtype)


def dwconv_centred(x, w, b):
    y = lax.conv_general_dilated(
        x, w[:, None, :].astype(x.dtype), window_strides=(1,), padding=[CONV_PAD],
        dimension_numbers=("NWC", "WIO", "NWC"), feature_group_count=x.shape[-1])
    return y + b.astype(x.dtype)


def mlstm_scan(q, k, v, log_i, log_f):
    B, H, S, d = q.shape
    nc = S // MLSTM_CHUNK

    def to_chunks(t):
        return jnp.moveaxis(t.reshape((B, H, nc, MLSTM_CHUNK) + t.shape[3:]), 2, 0)

    tril = jnp.tril(jnp.ones((MLSTM_CHUNK, MLSTM_CHUNK), dtype=bool))

    def body(carry, xs):
        C, n, m = carry
        qc, kc, vc, ic, fc = xs
        b = jnp.cumsum(fc, axis=-1)
        d_log = jnp.where(tril, b[..., :, None] - b[..., None, :] + ic[..., None, :], -jnp.inf)
        inter = b + m[..., None]
        m_t = jnp.maximum(inter, jnp.max(d_log, axis=-1))
        w_intra = jnp.exp(d_log - m_t[..., None])
        w_inter = jnp.exp(inter - m_t)
        s = jnp.einsum("bhtd,bhsd->bhts", qc, kc) * w_intra
        num = w_inter[..., None] * jnp.einsum("bhtd,bhde->bhte", qc, C) + jnp.einsum("bhts,bhse->bhte", s, vc)
        den = w_inter * jnp.einsum("bhtd,bhd->bht", qc, n) + jnp.sum(s, axis=-1)
        h = num / jnp.maximum(jnp.abs(den), jnp.exp(-m_t))[..., None]
        g = b[..., -1]
        w_log = g[..., None] - b + ic
        m_new = jnp.maximum(g + m, jnp.max(w_log, axis=-1))
        decay = jnp.exp(g + m - m_new)
        w_state = jnp.exp(w_log - m_new[..., None])
        kw = kc * w_state[..., None]
        C_new = decay[..., None, None] * C + jnp.einsum("bhsd,bhse->bhde", kw, vc)
        n_new = decay[..., None] * n + jnp.sum(kw, axis=2)
        return (C_new, n_new, m_new), h

    init = (jnp.zeros((B, H, d, d), jnp.float32), jnp.zeros((B, H, d), jnp.float32),
            jnp.zeros((B, H), jnp.float32))
    _, h = lax.scan(body, init, (to_chunks(q), to_chunks(k), to_chunks(v), to_chunks(log_i), to_chunks(log_f)))
    return jnp.moveaxis(h, 0, 2).reshape(B, H, S, d)


def mlstm_branch(xm, o_pre, gate_pre, conv_w, conv_b, w_q, w_k, w_v, norm_g):
    B, S, _ = xm.shape
    H, d = MLSTM_HEADS, MLSTM_HEAD_DIM
    xc = jax.nn.silu(dwconv_centred(xm, conv_w, conv_b)).reshape(B, S, H, d)
    xv = xm.reshape(B, S, H, d)
    q = jnp.einsum("bshd,hde->bhse", xc, w_q).astype(jnp.float32)
    k = (jnp.einsum("bshd,hde->bhse", xc, w_k) / math.sqrt(d)).astype(jnp.float32)
    v = jnp.einsum("bshd,hde->bhse", xv, w_v).astype(jnp.float32)
    gates = gate_pre.astype(jnp.float32).reshape(B, S, 4, H).transpose(2, 0, 3, 1)
    i_f, f_f, i_b, f_b = gates[0], gates[1], gates[2], gates[3]
    flip = lambda t: jnp.flip(t, axis=2)
    h_fwd = mlstm_scan(q, k, v, i_f, jax.nn.log_sigmoid(f_f))
    h_bwd = flip(mlstm_scan(flip(q), flip(k), flip(v), flip(i_b), flip(jax.nn.log_sigmoid(f_b))))
    h = jnp.transpose(h_fwd + h_bwd, (0, 2, 1, 3))
    h = rms_norm(h, norm_g.reshape(H, d)).reshape(B, S, MLSTM_WIDTH)
    return (jax.nn.sigmoid(o_pre.astype(jnp.float32)) * h.astype(jnp.float32)).astype(xm.dtype)


def rglru_dir(x, w_r, b_r, w_i, b_i, lam):
    B, S, C = x.shape
    xb = x.reshape(B, S, LRU_BLOCKS, LRU_BLOCK_DIM)
    r = jax.nn.sigmoid(jnp.einsum("bsnc,ncd->bsnd", xb, w_r).reshape(B, S, C) + b_r)
    i = jax.nn.sigmoid(jnp.einsum("bsnc,ncd->bsnd", xb, w_i).reshape(B, S, C) + b_i)
    log_a = -LRU_C * r * jax.nn.softplus(-lam)
    a = jnp.exp(log_a)
    u = jnp.sqrt(-jnp.expm1(2.0 * log_a)) * (i * x)

    def combine(left, right):
        a1, b1 = left
        a2, b2 = right
        return a1 * a2, a2 * b1 + b2

    _, h = lax.associative_scan(combine, (a, u), axis=1)
    return h


def lru_branch(xl, gate_pre, conv_w, conv_b, w_r, b_r, w_i, b_i, lam):
    xc = dwconv_centred(xl, conv_w, conv_b).astype(jnp.float32)
    w_r, b_r, w_i, b_i, lam = (t.astype(jnp.float32) for t in (w_r, b_r, w_i, b_i, lam))
    h_fwd = rglru_dir(xc, w_r[0], b_r[0], w_i[0], b_i[0], lam[0])
    h_bwd = jnp.flip(rglru_dir(jnp.flip(xc, axis=1), w_r[1], b_r[1], w_i[1], b_i[1], lam[1]), axis=1)
    y = (h_fwd + h_bwd) * jax.nn.gelu(gate_pre.astype(jnp.float32), approximate=False)
    return y.astype(xl.dtype)


def peer(h, w_q, sub_keys, expert_u, expert_v):
    B, S, D = h.shape
    nb = (B * S) // PEER_TOKEN_BLOCK
    keys32 = sub_keys.astype(jnp.float32)

    def block_fn(hb):
        tb = hb.shape[0]
        q = jnp.einsum("td,de->te", hb, w_q).astype(jnp.float32).reshape(tb, PEER_HEADS, 2, PEER_HALF)
        scores = jnp.einsum("thpc,hpnc->thpn", q, keys32)
        s_top, i_top = lax.top_k(scores, PEER_TOPK)
        cand = (s_top[:, :, 0, :, None] + s_top[:, :, 1, None, :]).reshape(tb, PEER_HEADS, PEER_TOPK * PEER_TOPK)
        cand_idx = (i_top[:, :, 0, :, None] * PEER_N_KEYS + i_top[:, :, 1, None, :]).reshape(tb, PEER_HEADS, PEER_TOPK * PEER_TOPK)
        best, pos = lax.top_k(cand, PEER_TOPK)
        idx = jnp.take_along_axis(cand_idx, pos, axis=-1).reshape(tb, PEER_HEADS * PEER_TOPK)
        gate = jax.nn.softmax(best, axis=-1).reshape(tb, PEER_HEADS * PEER_TOPK)
        u = jnp.take(expert_u, idx, axis=0)
        act = jax.nn.gelu(jnp.einsum("tkd,td->tk", u, hb).astype(jnp.float32), approximate=False)
        vv = jnp.take(expert_v, idx, axis=0)
        return jnp.einsum("tk,tkd->td", (gate * act).astype(hb.dtype), vv)

    out = lax.map(block_fn, h.reshape(nb, PEER_TOKEN_BLOCK, D))
    return out.reshape(B, S, D)


def setup_inputs(seed: int = 0) -> dict:
    key = jax.random.key(seed)
    ks = jax.random.split(key, 32)
    nrm = lambda k, shape, scale: jax.random.normal(k, shape, jnp.float32) * scale
    L = DEPTH
    f_base = jnp.linspace(3.0, 6.0, MLSTM_HEADS)
    z = jnp.zeros((MLSTM_HEADS,), jnp.float32)
    gate_base = jnp.stack([z, f_base, z, f_base])
    b_gates = (gate_base[None] + nrm(ks[3], (L, 4, MLSTM_HEADS), 0.1)).reshape(L, N_GATE_COLS)
    a0 = jax.random.uniform(ks[16], (L, 2, LRU_WIDTH), jnp.float32, 0.9, 0.999) ** (1.0 / LRU_C)
    lru_lambda = jnp.log(a0) - jnp.log1p(-a0)
    return {
        "x": nrm(ks[0], (BATCH, SEQ, D_MODEL), 1.0),
        "norm1_g": 1.0 + nrm(ks[1], (L, D_MODEL), 0.02),
        "w_in": nrm(ks[2], (L, D_MODEL, D_IN), D_MODEL ** -0.5),
        "b_gates": b_gates,
        "mlstm_conv_w": nrm(ks[4], (L, CONV_WIDTH, MLSTM_WIDTH), CONV_WIDTH ** -0.5),
        "mlstm_conv_b": nrm(ks[5], (L, MLSTM_WIDTH), 0.02),
        "mlstm_w_q": nrm(ks[6], (L, MLSTM_HEADS, MLSTM_HEAD_DIM, MLSTM_HEAD_DIM), MLSTM_HEAD_DIM ** -0.5),
        "mlstm_w_k": nrm(ks[7], (L, MLSTM_HEADS, MLSTM_HEAD_DIM, MLSTM_HEAD_DIM), MLSTM_HEAD_DIM ** -0.5),
        "mlstm_w_v": nrm(ks[8], (L, MLSTM_HEADS, MLSTM_HEAD_DIM, MLSTM_HEAD_DIM), MLSTM_HEAD_DIM ** -0.5),
        "mlstm_norm_g": 1.0 + nrm(ks[9], (L, MLSTM_WIDTH), 0.02),
        "lru_conv_w": nrm(ks[10], (L, CONV_WIDTH, LRU_WIDTH), CONV_WIDTH ** -0.5),
        "lru_conv_b": nrm(ks[11], (L, LRU_WIDTH), 0.02),
        "lru_w_r": nrm(ks[12], (L, 2, LRU_BLOCKS, LRU_BLOCK_DIM, LRU_BLOCK_DIM), LRU_BLOCK_DIM ** -0.5),
        "lru_b_r": nrm(ks[13], (L, 2, LRU_WIDTH), 0.02),
        "lru_w_i": nrm(ks[14], (L, 2, LRU_BLOCKS, LRU_BLOCK_DIM, LRU_BLOCK_DIM), LRU_BLOCK_DIM ** -0.5),
        "lru_b_i": nrm(ks[15], (L, 2, LRU_WIDTH), 0.02),
        "lru_lambda": lru_lambda,
        "w_branch_mlstm": nrm(ks[17], (L, MLSTM_WIDTH, D_MODEL), MLSTM_WIDTH ** -0.5),
        "w_branch_lru": nrm(ks[18], (L, LRU_WIDTH, D_MODEL), LRU_WIDTH ** -0.5),
        "w_out": nrm(ks[19], (L, D_MODEL, D_MODEL), D_MODEL ** -0.5),
        "norm2_g": 1.0 + nrm(ks[20], (L, D_MODEL), 0.02),
        "peer_w_q": nrm(ks[21], (L, D_MODEL, PEER_HEADS * PEER_QUERY_DIM), D_MODEL ** -0.5),
        "peer_sub_keys": nrm(ks[22], (L, PEER_HEADS, 2, PEER_N_KEYS, PEER_HALF), PEER_HALF ** -0.5),
        "peer_u": nrm(ks[23], (L, PEER_N_EXPERTS, D_MODEL), D_MODEL ** -0.5),
        "peer_v": nrm(ks[24], (L, PEER_N_EXPERTS, D_MODEL), PEER_HEADS ** -0.5),
        "final_norm_g": 1.0 + nrm(ks[25], (D_MODEL,), 0.02),
    }


def reference(x, norm1_g, w_in, b_gates, mlstm_conv_w, mlstm_conv_b, mlstm_w_q, mlstm_w_k, mlstm_w_v,
              mlstm_norm_g, lru_conv_w, lru_conv_b, lru_w_r, lru_b_r, lru_w_i, lru_b_i, lru_lambda,
              w_branch_mlstm, w_branch_lru, w_out, norm2_g, peer_w_q, peer_sub_keys, peer_u, peer_v,
              final_norm_g):
    split_points = np.cumsum(IN_SPLIT_SIZES)[:-1].tolist()
    for l in range(DEPTH):
        h = rms_norm(x, norm1_g[l])
        proj = jnp.einsum("bsd,de->bse", h, w_in[l])
        xm, o_pre, gate_pre, xl, lru_gate, merge_pre = jnp.split(proj, split_points, axis=-1)
        gate_pre = gate_pre + b_gates[l].astype(gate_pre.dtype)
        y_m = mlstm_branch(xm, o_pre, gate_pre, mlstm_conv_w[l], mlstm_conv_b[l], mlstm_w_q[l],
                           mlstm_w_k[l], mlstm_w_v[l], mlstm_norm_g[l])
        y_l = lru_branch(xl, lru_gate, lru_conv_w[l], lru_conv_b[l], lru_w_r[l], lru_b_r[l],
                         lru_w_i[l], lru_b_i[l], lru_lambda[l])
        g_m, g_l = jnp.split(jax.nn.sigmoid(merge_pre), 2, axis=-1)
        merged = (g_m * jnp.einsum("bsc,cd->bsd", y_m, w_branch_mlstm[l])
                  + g_l * jnp.einsum("bsc,cd->bsd", y_l, w_branch_lru[l]))
        x = x + jnp.einsum("bsd,de->bse", merged, w_out[l])
        h2 = rms_norm(x, norm2_g[l])
        x = x + peer(h2, peer_w_q[l], peer_sub_keys[l], peer_u[l], peer_v[l])
    return rms_norm(x, final_norm_g)
```

```python
import numpy as np
from contextlib import ExitStack
import concourse.bass as bass
import concourse.mybir as mybir
from concourse.bass_utils import run_bass_kernel_spmd

F32 = mybir.dt.float32
BF16 = mybir.dt.bfloat16
U32 = mybir.dt.uint32
I32 = mybir.dt.int32
AF = mybir.ActivationFunctionType
ALU = mybir.AluOpType

T = 4096
D = 1024
NT = T // 128
NG = T // 512
DIN = 6160
LN16 = float(np.log(16.0))
EPS = 1e-6


class Buf:
    __slots__ = ("name", "w", "r", "excl")

    def __init__(self, name, excl=False):
        self.name = name
        self.w = None
        self.r = {}
        self.excl = excl


class _Stop(Exception):
    pass


class KB:
    def __init__(self, nc, es, n_dma_sems=32):
        self.nc = nc
        self.es = es
        self.engs = {"pe": nc.tensor, "act": nc.scalar, "dve": nc.vector, "pool": nc.gpsimd, "sp": nc.sync}
        self.sem = {}
        self.cnt = {}
        self.semobj = {}
        for e in self.engs:
            s = es.enter_context(nc.semaphore("sem_" + e))
            self.semobj["E" + e] = s
            self.cnt["E" + e] = 0
        self.dma_keys = {"sp": [], "pool": []}
        for q, n in (("sp", n_dma_sems), ("pool", n_dma_sems)):
            for i in range(n):
                s = es.enter_context(nc.semaphore("sem_dma_%s%d" % (q, i)))
                k = "D%s%d" % (q, i)
                self.semobj[k] = s
                self.cnt[k] = 0
                self.dma_keys[q].append(k)
        self.dma_rr = {"sp": 0, "pool": 0}
        self.waited = {e: {} for e in self.engs}
        self.nbuf = 0
        self.all_dma_events = {}

    def buf(self, name=None, excl=False):
        self.nbuf += 1
        return Buf(name or "b%d" % self.nbuf, excl)

    def bufs(self, n, name="b", excl=False):
        return [self.buf("%s%d" % (name, i), excl) for i in range(n)]

    def _wait(self, eng, deps):
        wd = self.waited[eng]
        best = {}
        for (k, v) in deps:
            if wd.get(k, 0) >= v:
                continue
            if eng == "pe" and k == "Epe":
                continue
            if best.get(k, 0) < v:
                best[k] = v
        for k, v in best.items():
            self.engs[eng].wait_ge(self.semobj[k], v)
            wd[k] = v

    def _deps(self, reads, writes):
        deps = []
        for b in reads:
            if b.w is not None:
                deps.append(b.w)
            if b.excl:
                deps.extend(b.r.items())
        for b in writes:
            if b.w is not None:
                deps.append(b.w)
            deps.extend(b.r.items())
        return deps

    def _record(self, ev, reads, writes):
        k, v = ev
        for b in reads:
            if b.r.get(k, 0) < v:
                b.r[k] = v
        for b in writes:
            b.w = ev
            b.r = {}

    def op(self, eng, fn, reads=(), writes=()):
        self._wait(eng, self._deps(reads, writes))
        inst = fn(self.engs[eng])
        k = "E" + eng
        self.cnt[k] += 1
        inst.then_inc(self.semobj[k], 1)
        self._record((k, self.cnt[k]), reads, writes)

    def dma(self, eng, fn, reads=(), writes=()):
        k = self.dma_keys[eng][self.dma_rr[eng]]
        self.dma_rr[eng] = (self.dma_rr[eng] + 1) % len(self.dma_keys[eng])
        deps = self._deps(reads, writes)
        if self.cnt[k] > 0:
            deps.append((k, self.cnt[k]))
        self._wait(eng, deps)
        inst = fn(self.engs[eng])
        self.cnt[k] += 16
        inst.then_inc(self.semobj[k], 16)
        ev = (k, self.cnt[k])
        self._record(ev, reads, writes)
        self.all_dma_events[k] = self.cnt[k]
        return ev

    def barrier(self):
        deps = [(k, v) for k, v in self.cnt.items() if v > 0]
        for e in self.engs:
            self._wait(e, deps)

    def finish(self):
        deps = [(k, v) for k, v in self.cnt.items() if v > 0]
        self._wait("sp", deps)


def build_program(dbg=None):
    nc = bass.Bass("TRN2", target_bir_lowering=False)

    in_names = []
    need_peer = dbg is None or dbg[0] == "full"

    def din(name, shape, dt=F32):
        if name in ("peer_u", "peer_v") and not need_peer:
            return None
        in_names.append(name)
        return nc.dram_tensor(name, list(shape), dt, kind="ExternalInput").ap()

    x = din("x", [T, D])
    norm1_g = din("norm1_g", [D])
    w_in = din("w_in", [D, DIN])
    b_gates = din("b_gates", [16])
    m_conv_w = din("mlstm_conv_w", [4, D])
    m_conv_b = din("mlstm_conv_b", [D])
    m_wq = din("mlstm_w_q", [4, 256, 256])
    m_wk = din("mlstm_w_k", [4, 256, 256])
    m_wv = din("mlstm_w_v", [4, 256, 256])
    m_norm_g = din("mlstm_norm_g", [D])
    l_conv_w = din("lru_conv_w", [4, D])
    l_conv_b = din("lru_conv_b", [D])
    l_wr = din("lru_w_r", [2, 8, 128, 128])
    l_br = din("lru_b_r", [2, D])
    l_wi = din("lru_w_i", [2, 8, 128, 128])
    l_bi = din("lru_b_i", [2, D])
    l_lam = din("lru_lambda", [2, D])
    w_bm = din("w_branch_mlstm", [D, D])
    w_bl = din("w_branch_lru", [D, D])
    w_out = din("w_out", [D, D])
    norm2_g = din("norm2_g", [D])
    p_wq = din("peer_w_q", [D, 2048])
    p_keys = din("peer_sub_keys", [16, 128, 128])
    p_u = din("peer_u", [16384, D])
    p_v = din("peer_v", [16384, D])
    fin_g = din("final_norm_g", [D])
    c_ident = din("c_ident", [128, 128])
    c_ut = din("c_ut", [128, 128])
    c_lt = din("c_lt", [128, 128])
    c_iota = din("c_iota", [128, 32 + 2 * NT])

    y_out = nc.dram_tensor("y", [T, D], F32, kind="ExternalOutput").ap()
    ymT = nc.dram_tensor("scr_ymT", [D, T], BF16, kind="Internal").ap()
    ylT = nc.dram_tensor("scr_ylT", [D, T], BF16, kind="Internal").ap()
    hf_scr = nc.dram_tensor("scr_hf", [T, 256], F32, kind="Internal").ap()
    mgT = nc.dram_tensor("scr_mgT", [D, T], BF16, kind="Internal").ap()
    uv_tab = nc.dram_tensor("scr_uv", [16384, 2 * D], BF16, kind="Internal").ap()
    dbg_out = None
    if dbg is not None:
        dbg_out = nc.dram_tensor("dbg", list(dbg[1]), F32, kind="ExternalOutput").ap()

    es = ExitStack()
    with es:
        E = es.enter_context
        kb = KB(nc, es)
        op, dma = kb.op, kb.dma

        uid = [0]

        def sb(name, shape, dt=F32, ctx=None):
            uid[0] += 1
            return (ctx or es).enter_context(nc.sbuf_tensor("%s_%d" % (name, uid[0]), list(shape), dt))

        def ps(name, shape, dt=F32, ctx=None):
            uid[0] += 1
            return (ctx or es).enter_context(nc.psum_tensor("%s_%d" % (name, uid[0]), list(shape), dt))

        ident_f = sb("ident_f", [128, 128]); ident_b = sb("ident_b", [128, 128], BF16)
        ut_f = sb("ut_f", [128, 128]); lt_f = sb("lt_f", [128, 128])
        ones_f = sb("ones_f", [128, 128])
        iota16 = sb("iota16", [128, 16]); iota16m = sb("iota16m", [128, 16])
        onezero = sb("onezero", [128, NT, 2])
        B_const = kb.buf("const")
        dma("sp", lambda e: e.dma_start(out=ident_f[:], in_=c_ident[:, :]), writes=[B_const])
        dma("sp", lambda e: e.dma_start(out=ut_f[:], in_=c_ut[:, :]), writes=[B_const])
        dma("sp", lambda e: e.dma_start(out=lt_f[:], in_=c_lt[:, :]), writes=[B_const])
        dma("sp", lambda e: e.dma_start(out=iota16[:], in_=c_iota[:, 0:16]), writes=[B_const])
        dma("sp", lambda e: e.dma_start(out=iota16m[:], in_=c_iota[:, 16:32]), writes=[B_const])
        dma("sp", lambda e: e.dma_start(out=onezero[:].rearrange("p a b -> p (a b)"), in_=c_iota[:, 32:32 + 2 * NT]), writes=[B_const])
        op("dve", lambda e: e.tensor_copy(out=ident_b[:], in_=ident_f[:]), reads=[B_const], writes=[B_const])
        op("dve", lambda e: e.memset(ones_f[:], 1.0), writes=[B_const])
        NV = 0
        vec_specs = [("g1", norm1_g, None), ("mcb", m_conv_b, None), ("mng", m_norm_g, None), ("lcb", l_conv_b, None)]
        for j in range(4):
            vec_specs.append(("mcw%d" % j, m_conv_w, j))
            vec_specs.append(("lcw%d" % j, l_conv_w, j))
        for d_ in range(2):
            vec_specs.append(("lbr%d" % d_, l_br, d_))
            vec_specs.append(("lbi%d" % d_, l_bi, d_))
            vec_specs.append(("lam%d" % d_, l_lam, d_))
        vecs = sb("vecs", [128, len(vec_specs), 8])
        vcol = {}
        for i, (nm, ap_, row) in enumerate(vec_specs):
            src = ap_ if row is None else ap_[row]
            src = src.rearrange("(c p) -> p c", p=128)
            dma("sp", lambda e, i=i, src=src: e.dma_start(out=vecs[:, i, :], in_=src, allow_slow_non_contiguous=True),
                writes=[B_const])
            vcol[nm] = (lambda i: (lambda c: vecs[:, i, c:c + 1]))(i)
        cdec = sb("cdec", [128, 2, 8])
        for d_ in range(2):
            li = [i for i, s in enumerate(vec_specs) if s[0] == "lam%d" % d_][0]
            op("act", lambda e: e.activation(out=cdec[:, d_, :], in_=vecs[:, li, :], func=AF.Exp, scale=-1.0),
               reads=[B_const], writes=[B_const])
            op("act", lambda e: e.activation(out=cdec[:, d_, :], in_=cdec[:, d_, :], func=AF.Ln, bias=1.0, scale=1.0),
               reads=[B_const], writes=[B_const])
            op("dve", lambda e: e.tensor_scalar(out=cdec[:, d_, :], in0=cdec[:, d_, :], scalar1=-8.0, scalar2=None,
                                                op0=ALU.mult), reads=[B_const], writes=[B_const])
        bg_bc = sb("bg_bc", [128, 16])
        dma("sp", lambda e: e.dma_start(out=bg_bc[:], in_=b_gates.partition_broadcast(128)), writes=[B_const])
        epsc = sb("epsc", [128, 4])
        op("dve", lambda e: e.memset(epsc[:, 0:1], EPS), writes=[B_const])
        op("dve", lambda e: e.memset(epsc[:, 1:2], -LN16), writes=[B_const])
        op("dve", lambda e: e.memset(epsc[:, 2:3], 1.0), writes=[B_const])
        op("dve", lambda e: e.memset(epsc[:, 3:4], 0.0), writes=[B_const])
        eps_col, nln16_col, one_col, zero_col = epsc[:, 0:1], epsc[:, 1:2], epsc[:, 2:3], epsc[:, 3:4]

        hT_scope = ExitStack()
        hT = sb("hT", [128, 8, T], BF16, hT_scope)
        B_hT = kb.bufs(NG, "hT")

        def load_w_bf16(dst, src_rows, bufs_w, reads=()):
            dma("pool", lambda e: e.dma_start(out=dst, in_=src_rows), reads=list(reads), writes=list(bufs_w))

        def rms_rstd(ctx_name, src, n, ss, tmp_junk, B_in, B_ss, B_junk):
            op("act", lambda e: e.activation(out=tmp_junk, in_=src, func=AF.Square, accum_out=ss),
               reads=[B_in], writes=[B_ss, B_junk])
            op("act", lambda e: e.activation(out=ss, in_=ss, func=AF.Sqrt, scale=1.0 / n, bias=eps_col),
               reads=[B_ss, B_const], writes=[B_ss])
            op("dve", lambda e: e.reciprocal(out=ss, in_=ss), reads=[B_ss], writes=[B_ss])

        with ExitStack() as cs:
            xin = [sb("xa%d" % i, [128, D], F32, cs) for i in range(2)]
            xsb = [sb("xsb%d" % i, [128, D], BF16, cs) for i in range(2)]
            junk = sb("junkA", [128, D], BF16, cs)
            ssA = [sb("ssA%d" % i, [128, 1], F32, cs) for i in range(2)]
            tpA = [ps("tpA%d" % i, [128, 8, 128], BF16, cs) for i in range(2)]
            Bx = kb.bufs(2, "xa"); Bxs = kb.bufs(2, "xsb"); Bj = kb.buf("junkA"); Bss = kb.bufs(2, "ssA"); Btp = kb.bufs(2, "tpA", excl=True)
            for i in range(NT):
                s = i % 2
                dma("sp", lambda e: e.dma_start(out=xin[s][:], in_=x[i * 128:(i + 1) * 128, :]), writes=[Bx[s]])
                rms_rstd("A", xin[s][:], D, ssA[s][:], junk[:], Bx[s], Bss[s], Bj)
                op("dve", lambda e: e.tensor_scalar(out=xsb[s][:], in0=xin[s][:], scalar1=ssA[s][:], scalar2=None, op0=ALU.mult),
                   reads=[Bx[s], Bss[s]], writes=[Bxs[s]])
                for c in range(8):
                    op("pe", lambda e: e.transpose(out=tpA[s][:, c, :], in_=xsb[s][:, c * 128:(c + 1) * 128], identity=ident_b[:]),
                       reads=[Bxs[s], B_const], writes=[Btp[s]])
                g1b = vecs[:, 0, :].unsqueeze(2).to_broadcast([128, 8, 128])
                op("dve", lambda e: e.tensor_tensor(out=hT[:, :, i * 128:(i + 1) * 128], in0=tpA[s][:], in1=g1b, op=ALU.mult),
                   reads=[Btp[s], B_const], writes=[B_hT[i // 4]])
            kb.barrier()

        if dbg is not None and dbg[0] == "hT":
            with ExitStack() as cs:
                t32 = sb("dbg32", [128, 8, 512], F32, cs)
                Bt = kb.buf()
                for g in range(NG):
                    op("dve", lambda e: e.tensor_copy(out=t32[:], in_=hT[:, :, g * 512:(g + 1) * 512]), reads=[B_hT[g]], writes=[Bt])
                    dma("sp", lambda e: e.dma_start(out=dbg_out.rearrange("(c p) t -> p c t", p=128)[:, :, g * 512:(g + 1) * 512], in_=t32[:]), reads=[Bt])
                kb.barrier()

        B_ylT = kb.buf("ylT")
        B_ymT = kb.buf("ymT")
        XL0 = 2064
        LG0 = 3088
        if dbg is None or dbg[0] in ("yl", "full", "x1", "idx"):
          with ExitStack() as cs:
            wxl = sb("wxl", [128, 8, 128], BF16, cs); wlg = sb("wlg", [128, 8, 128], BF16, cs)
            wgate = sb("wgate", [128, 4, 128], BF16, cs)
            XL = sb("XL", [128, T + 4], F32, cs)
            XC = sb("XC", [128, T], F32, cs)
            XCB = sb("XCB", [128, T], BF16, cs)
            LG = sb("LG", [128, T], BF16, cs)
            HF = sb("HF", [128, T], F32, cs)
            NTMP = 2
            R_ = [sb("R%d" % i, [128, 512], F32, cs) for i in range(NTMP)]
            IG_ = [sb("IG%d" % i, [128, 512], F32, cs) for i in range(NTMP)]
            T_ = [sb("T%d" % i, [128, 512], F32, cs) for i in range(NTMP)]
            pL = [ps("pL%d" % i, [128, 512], F32, cs) for i in range(4)]
            Bw = kb.buf("wL"); BXL = kb.buf("XL"); BXC = kb.buf("XC"); BXCB = kb.buf("XCB"); BLG = kb.buf("LG"); BHF = kb.buf("HF")
            BR = kb.bufs(NTMP, "R"); BIG = kb.bufs(NTMP, "IG"); BT = kb.bufs(NTMP, "T"); BpL = kb.bufs(4, "pL", excl=True)
            HB = XL
            pli = 0
            for n in range(8):
                for c in range(8):
                    load_w_bf16(wxl[:, c, :], w_in[c * 128:(c + 1) * 128, XL0 + n * 128: XL0 + (n + 1) * 128], [Bw])
                    load_w_bf16(wlg[:, c, :], w_in[c * 128:(c + 1) * 128, LG0 + n * 128: LG0 + (n + 1) * 128], [Bw])
                for d_ in range(2):
                    load_w_bf16(wgate[:, d_ * 2 + 0, :], l_wr[d_, n], [Bw])
                    load_w_bf16(wgate[:, d_ * 2 + 1, :], l_wi[d_, n], [Bw])
                op("pool", lambda e: e.memset(XL[:, 0:2], 0.0), writes=[BXL])
                op("pool", lambda e: e.memset(XL[:, T + 2:T + 4], 0.0), writes=[BXL])
                for g in range(NG):
                    p1 = pli % 4; pli += 1
                    for c in range(8):
                        op("pe", lambda e: e.matmul(pL[p1][:], lhsT=wxl[:, c, :], rhs=hT[:, c, g * 512:(g + 1) * 512], start=(c == 0), stop=(c == 7)),
                           reads=[Bw, B_hT[g]], writes=[BpL[p1]])
                    op("act", lambda e: e.copy(out=XL[:, 2 + g * 512: 2 + (g + 1) * 512], in_=pL[p1][:]), reads=[BpL[p1]], writes=[BXL])
                    p2 = pli % 4; pli += 1
                    for c in range(8):
                        op("pe", lambda e: e.matmul(pL[p2][:], lhsT=wlg[:, c, :], rhs=hT[:, c, g * 512:(g + 1) * 512], start=(c == 0), stop=(c == 7)),
                           reads=[Bw, B_hT[g]], writes=[BpL[p2]])
                    op("act", lambda e: e.activation(out=LG[:, g * 512:(g + 1) * 512], in_=pL[p2][:], func=AF.Gelu), reads=[BpL[p2]], writes=[BLG])
                op("dve", lambda e: e.tensor_scalar(out=XC[:], in0=XL[:, 0:T], scalar1=vcol["lcw0"](n), scalar2=vcol["lcb"](n), op0=ALU.mult, op1=ALU.add),
                   reads=[BXL, B_const], writes=[BXC])
                for j in range(1, 4):
                    op("dve", lambda e: e.scalar_tensor_tensor(out=XC[:], in0=XL[:, j:j + T], scalar=vcol["lcw%d" % j](n), in1=XC[:], op0=ALU.mult, op1=ALU.add),
                       reads=[BXL, BXC, B_const], writes=[BXC])
                op("pool", lambda e: e.tensor_copy(out=XCB[:], in_=XC[:]), reads=[BXC], writes=[BXCB])
                ti = 0
                for d_ in range(2):
                    groups = list(range(NG)) if d_ == 0 else list(range(NG - 1, -1, -1))
                    Hd = HF if d_ == 0 else HB
                    BHd = BHF if d_ == 0 else BXL
                    hoff = 0 if d_ == 0 else 2
                    prev = None
                    for g in groups:
                        s = ti % NTMP; ti += 1
                        sl = slice(g * 512, (g + 1) * 512)
                        p1 = pli % 4; pli += 1
                        op("pe", lambda e: e.matmul(pL[p1][:], lhsT=wgate[:, d_ * 2, :], rhs=XCB[:, sl], start=True, stop=True),
                           reads=[Bw, BXCB], writes=[BpL[p1]])
                        op("act", lambda e: e.activation(out=R_[s][:], in_=pL[p1][:], func=AF.Sigmoid, bias=vcol["lbr%d" % d_](n)),
                           reads=[BpL[p1], B_const], writes=[BR[s]])
                        p2 = pli % 4; pli += 1
                        op("pe", lambda e: e.matmul(pL[p2][:], lhsT=wgate[:, d_ * 2 + 1, :], rhs=XCB[:, sl], start=True, stop=True),
                           reads=[Bw, BXCB], writes=[BpL[p2]])
                        op("act", lambda e: e.activation(out=IG_[s][:], in_=pL[p2][:], func=AF.Sigmoid, bias=vcol["lbi%d" % d_](n)),
                           reads=[BpL[p2], B_const], writes=[BIG[s]])
                        op("act", lambda e: e.activation(out=R_[s][:], in_=R_[s][:], func=AF.Exp, scale=cdec[:, d_, n:n + 1]),
                           reads=[BR[s], B_const], writes=[BR[s]])
                        op("pool", lambda e: e.tensor_tensor(out=T_[s][:], in0=R_[s][:], in1=R_[s][:], op=ALU.mult), reads=[BR[s]], writes=[BT[s]])
                        op("act", lambda e: e.activation(out=T_[s][:], in_=T_[s][:], func=AF.Sqrt, scale=-1.0, bias=one_col),
                           reads=[BT[s], B_const], writes=[BT[s]])
                        op("pool", lambda e: e.tensor_tensor(out=IG_[s][:], in0=IG_[s][:], in1=XC[:, sl], op=ALU.mult), reads=[BIG[s], BXC], writes=[BIG[s]])
                        op("pool", lambda e: e.tensor_tensor(out=T_[s][:], in0=T_[s][:], in1=IG_[s][:], op=ALU.mult), reads=[BT[s], BIG[s]], writes=[BT[s]])
                        if d_ == 0:
                            init = 0.0 if prev is None else HF[:, prev * 512 + 511: prev * 512 + 512]
                            op("dve", lambda e: e.tensor_tensor_scan(out=HF[:, sl], data0=R_[s][:], data1=T_[s][:], initial=init, op0=ALU.mult, op1=ALU.add),
                               reads=[BR[s], BT[s], BHF], writes=[BHF])
                        else:
                            init = 0.0 if prev is None else HB[:, 2 + prev * 512: 2 + prev * 512 + 1]
                            op("dve", lambda e: e.tensor_tensor_scan(out=HB[:, 2 + g * 512: 2 + (g + 1) * 512][:, ::-1], data0=R_[s][:, ::-1], data1=T_[s][:, ::-1],
                                                                    initial=init, op0=ALU.mult, op1=ALU.add),
                               reads=[BR[s], BT[s], BXL], writes=[BXL])
                        prev = g
                op("dve", lambda e: e.tensor_tensor(out=HF[:], in0=HF[:], in1=HB[:, 2:2 + T], op=ALU.add), reads=[BHF, BXL], writes=[BHF])
                op("dve", lambda e: e.tensor_tensor(out=XCB[:], in0=HF[:], in1=LG[:], op=ALU.mult), reads=[BHF, BLG], writes=[BXCB])
                dma("sp", lambda e: e.dma_start(out=ylT[n * 128:(n + 1) * 128, :], in_=XCB[:]), reads=[BXCB], writes=[B_ylT])
            kb.barrier()

        if dbg is not None and dbg[0] == "yl":
            with ExitStack() as cs:
                tb = sb("dbgb", [128, T], BF16, cs); t32 = sb("dbg32", [128, T], F32, cs)
                Bt = kb.buf(); Bt2 = kb.buf()
                for n in range(8):
                    dma("sp", lambda e: e.dma_start(out=tb[:], in_=ylT[n * 128:(n + 1) * 128, :]), reads=[B_ylT], writes=[Bt])
                    op("dve", lambda e: e.tensor_copy(out=t32[:], in_=tb[:]), reads=[Bt], writes=[Bt2])
                    dma("sp", lambda e: e.dma_start(out=dbg_out[n * 128:(n + 1) * 128, :], in_=t32[:]), reads=[Bt2])
                kb.barrier()


        B_hf = kb.bufs(NT, "hfscr")
        stopM = dbg[2] if (dbg is not None and len(dbg) > 2) else 0
        if dbg is None or dbg[0] in ("ym", "full", "x1", "idx"):
         try:
          with ExitStack() as ms:
            wg = sb("wg", [128, 8, 16], BF16, ms)
            G = sb("G", [128, NT, 16], F32, ms)
            LF = [sb("LF%d" % d_, [128, NT, 4], F32, ms) for d_ in range(2)]
            SC1 = [sb("SC1%d" % d_, [128, NT, 4], F32, ms) for d_ in range(2)]
            EB = [sb("EB%d" % d_, [128, NT, 4], F32, ms) for d_ in range(2)]
            EG = [sb("EG%d" % d_, [128, NT, 4], F32, ms) for d_ in range(2)]
            KWS = [sb("KWS%d" % d_, [128, NT, 4], F32, ms) for d_ in range(2)]
            pb = [ps("pM%d" % i, [128, 512], F32, ms) for i in range(7)]
            pTb = ps("pMT", [128, 1024], BF16, ms)
            Bpb = kb.bufs(8, "pM", excl=True)
            ut_b = sb("ut_b", [128, 128], BF16, ms); lt_b = sb("lt_b", [128, 128], BF16, ms); ones_b = sb("ones_b", [128, 128], BF16, ms)
            op("dve", lambda e: e.tensor_copy(out=ut_b[:], in_=ut_f[:]), reads=[B_const], writes=[B_const])
            op("dve", lambda e: e.tensor_copy(out=lt_b[:], in_=lt_f[:]), reads=[B_const], writes=[B_const])
            op("dve", lambda e: e.memset(ones_b[:], 1.0), writes=[B_const])
            LFh = sb("LFh", [128, NT * 4], BF16, ms); LFl = sb("LFl", [128, NT * 4], BF16, ms); LFr = sb("LFr", [128, NT * 4], F32, ms)
            Bwg = kb.buf("wg"); BG = kb.buf("G"); BGS = kb.buf("Gscal")
            wgf = sb("wgf", [128, 8, 16], F32, ms)
            Bwgf = kb.buf("wgf")
            dma("sp", lambda e: e.dma_start(out=wgf[:], in_=w_in[:, 2048:2064].rearrange("(c p) n -> p c n", p=128)), writes=[Bwgf])
            op("dve", lambda e: e.tensor_copy(out=wg[:], in_=wgf[:]), reads=[Bwgf], writes=[Bwg])
            for i in range(NT):
                p = i % 2
                for c in range(8):
                    op("pe", lambda e: e.matmul(pb[p][:, 0:16], lhsT=hT[:, c, i * 128:(i + 1) * 128], rhs=wg[:, c, :], start=(c == 0), stop=(c == 7)),
                       reads=[Bwg, B_hT[i // 4]], writes=[Bpb[p]])
                op("dve", lambda e: e.tensor_tensor(out=G[:, i, :], in0=pb[p][:, 0:16], in1=bg_bc[:], op=ALU.add), reads=[Bpb[p], B_const], writes=[BG])
            mask = [ut_f, lt_f]
            for d_ in range(2):
                fcol = (2 * d_ + 1) * 4
                icol = (2 * d_) * 4
                op("act", lambda e: e.activation(out=LF[d_][:], in_=G[:, :, fcol:fcol + 4], func=AF.Exp, scale=-1.0), reads=[BG], writes=[BGS])
                op("act", lambda e: e.activation(out=LF[d_][:], in_=LF[d_][:], func=AF.Ln, bias=one_col, scale=1.0), reads=[BGS, B_const], writes=[BGS])
                op("dve", lambda e: e.tensor_scalar(out=LF[d_][:], in0=LF[d_][:], scalar1=-1.0, scalar2=None, op0=ALU.mult), reads=[BGS], writes=[BGS])
                pB = pb[2 + d_ * 2]; pGt = pb[3 + d_ * 2]
                maskb = [ut_b, lt_b]
                lf2 = LF[d_][:].rearrange("p a b -> p (a b)")
                op("dve", lambda e: e.tensor_copy(out=LFh[:], in_=lf2), reads=[BGS], writes=[BGS])
                op("dve", lambda e: e.tensor_tensor(out=LFr[:], in0=lf2, in1=LFh[:], op=ALU.subtract), reads=[BGS], writes=[BGS])
                op("dve", lambda e: e.tensor_copy(out=LFl[:], in_=LFr[:]), reads=[BGS], writes=[BGS])
                op("pe", lambda e: e.matmul(pB[:, 0:128], lhsT=maskb[d_][:], rhs=LFh[:], start=True, stop=False), reads=[BGS, B_const], writes=[Bpb[2 + d_ * 2]])
                op("pe", lambda e: e.matmul(pB[:, 0:128], lhsT=maskb[d_][:], rhs=LFl[:], start=False, stop=True), reads=[BGS, B_const], writes=[Bpb[2 + d_ * 2]])
                op("pe", lambda e: e.matmul(pGt[:, 0:128], lhsT=ones_b[:], rhs=LFh[:], start=True, stop=False), reads=[BGS, B_const], writes=[Bpb[3 + d_ * 2]])
                op("pe", lambda e: e.matmul(pGt[:, 0:128], lhsT=ones_b[:], rhs=LFl[:], start=False, stop=True), reads=[BGS, B_const], writes=[Bpb[3 + d_ * 2]])
                pBv = pB[:, 0:128].rearrange("p (a b) -> p a b", b=4)
                pGv = pGt[:, 0:128].rearrange("p (a b) -> p a b", b=4)
                op("dve", lambda e: e.tensor_tensor(out=SC1[d_][:], in0=G[:, :, icol:icol + 4], in1=pBv, op=ALU.subtract), reads=[BG, Bpb[2 + d_ * 2]], writes=[BGS])
                op("act", lambda e: e.activation(out=SC1[d_][:], in_=SC1[d_][:], func=AF.Exp, bias=nln16_col, scale=1.0), reads=[BGS, B_const], writes=[BGS])
                op("act", lambda e: e.activation(out=EB[d_][:], in_=pBv, func=AF.Exp), reads=[Bpb[2 + d_ * 2]], writes=[BGS])
                op("act", lambda e: e.activation(out=EG[d_][:], in_=pGv, func=AF.Exp), reads=[Bpb[3 + d_ * 2]], writes=[BGS])
                op("dve", lambda e: e.tensor_tensor(out=KWS[d_][:], in0=SC1[d_][:], in1=EG[d_][:], op=ALU.mult), reads=[BGS], writes=[BGS])

            for h in range(4 if stopM == 0 else (0 if stopM == 1 else 1)):
              with ExitStack() as hs:
                wxm = sb("wxm", [128, 8, 256], BF16, hs); wo = sb("wo", [128, 8, 256], BF16, hs)
                wq = sb("wq", [128, 2, 256], BF16, hs); wk = sb("wk", [128, 2, 256], BF16, hs); wv = sb("wv", [128, 2, 256], BF16, hs)
                XC = sb("mXC", [128, 2, T], BF16, hs)
                V = sb("mV", [128, NT, 260], BF16, hs)
                Bwh = kb.buf("wh"); BXC = kb.buf("mXC"); BV = kb.buf("mV")
                for c in range(8):
                    load_w_bf16(wxm[:, c, :], w_in[c * 128:(c + 1) * 128, h * 256:(h + 1) * 256], [Bwh])
                    load_w_bf16(wo[:, c, :], w_in[c * 128:(c + 1) * 128, 1024 + h * 256: 1024 + (h + 1) * 256], [Bwh])
                for dc in range(2):
                    load_w_bf16(wq[:, dc, :], m_wq[h, dc * 128:(dc + 1) * 128, :], [Bwh])
                    load_w_bf16(wk[:, dc, :], m_wk[h, dc * 128:(dc + 1) * 128, :], [Bwh])
                    load_w_bf16(wv[:, dc, :], m_wv[h, dc * 128:(dc + 1) * 128, :], [Bwh])
                op("dve", lambda e: e.tensor_copy(out=V[:, :, 256:258], in_=onezero[:]), reads=[B_const], writes=[BV])
                pi = 0
                with ExitStack() as s1:
                    XM = sb("mXM", [128, T + 4], F32, s1)
                    XMB = sb("mXMB", [128, 2, T], BF16, s1)
                    XCF = [sb("mXCF%d" % i, [128, 1024], F32, s1) for i in range(2)]
                    XSG = [sb("mXSG%d" % i, [128, 1024], F32, s1) for i in range(2)]; BXSG = kb.bufs(2, "mXSG")
                    BXM = kb.buf("mXM"); BXMB = kb.buf("mXMB"); BXCF = kb.bufs(2, "mXCF")
                    op("pool", lambda e: e.memset(XM[:, 0:2], 0.0), writes=[BXM])
                    op("pool", lambda e: e.memset(XM[:, T + 2:T + 4], 0.0), writes=[BXM])
                    for cc in range(2):
                        ch = h * 2 + cc
                        for g in range(NG):
                            p = pi % 4; pi += 1
                            for c in range(8):
                                op("pe", lambda e: e.matmul(pb[p][:], lhsT=wxm[:, c, cc * 128:(cc + 1) * 128], rhs=hT[:, c, g * 512:(g + 1) * 512], start=(c == 0), stop=(c == 7)),
                                   reads=[Bwh, B_hT[g]], writes=[Bpb[p]])
                            op("act", lambda e: e.copy(out=XM[:, 2 + g * 512:2 + (g + 1) * 512], in_=pb[p][:]), reads=[Bpb[p]], writes=[BXM])
                            op("dve", lambda e: e.tensor_copy(out=XMB[:, cc, g * 512:(g + 1) * 512], in_=pb[p][:]), reads=[Bpb[p]], writes=[BXMB])
                        for q4 in range(4):
                            s = q4 % 2
                            o0 = q4 * 1024
                            op("dve", lambda e: e.tensor_scalar(out=XCF[s][:], in0=XM[:, o0:o0 + 1024], scalar1=vcol["mcw0"](ch), scalar2=vcol["mcb"](ch), op0=ALU.mult, op1=ALU.add),
                               reads=[BXM, B_const], writes=[BXCF[s]])
                            for j in range(1, 4):
                                op("dve", lambda e: e.scalar_tensor_tensor(out=XCF[s][:], in0=XM[:, o0 + j:o0 + j + 1024], scalar=vcol["mcw%d" % j](ch), in1=XCF[s][:], op0=ALU.mult, op1=ALU.add),
                                   reads=[BXM, BXCF[s], B_const], writes=[BXCF[s]])
                            op("act", lambda e: e.activation(out=XSG[s][:], in_=XCF[s][:], func=AF.Sigmoid), reads=[BXCF[s]], writes=[BXSG[s]])
                            op("dve", lambda e: e.tensor_tensor(out=XC[:, cc, o0:o0 + 1024], in0=XCF[s][:], in1=XSG[s][:], op=ALU.mult), reads=[BXCF[s], BXSG[s]], writes=[BXC])
                    for i in range(NT):
                        p = 4 + (i % 2)
                        for dc in range(2):
                            op("pe", lambda e: e.matmul(pb[p][:, 0:256], lhsT=XMB[:, dc, i * 128:(i + 1) * 128], rhs=wv[:, dc, :], start=(dc == 0), stop=(dc == 1)),
                               reads=[BXMB, Bwh], writes=[Bpb[p]])
                        op("act", lambda e: e.copy(out=V[:, i, 0:256], in_=pb[p][:, 0:256]), reads=[Bpb[p]], writes=[BV])
                    kb.barrier()
                if stopM == 2:
                    continue
                with ExitStack() as s2:
                    QT = sb("mQT", [128, 2, T], BF16, s2); KT = sb("mKT", [128, 2, T], BF16, s2)
                    BQT = kb.buf("mQT"); BKT = kb.buf("mKT")
                    for (dst, Bd, wmat) in ((QT, BQT, wq), (KT, BKT, wk)):
                        for ec in range(2):
                            for g in range(NG):
                                p = pi % 4; pi += 1
                                for dc in range(2):
                                    op("pe", lambda e: e.matmul(pb[p][:], lhsT=wmat[:, dc, ec * 128:(ec + 1) * 128], rhs=XC[:, dc, g * 512:(g + 1) * 512], start=(dc == 0), stop=(dc == 1)),
                                       reads=[Bwh, BXC], writes=[Bpb[p]])
                                op("act", lambda e: e.copy(out=dst[:, ec, g * 512:(g + 1) * 512], in_=pb[p][:]), reads=[Bpb[p]], writes=[Bd])
                    ST = sb("mST", [128, 128], BF16, s2); KW = sb("mKW", [128, 256], BF16, s2)
                    Cst = sb("mCst", [128, 2, 258], F32, s2); Cbf = sb("mCbf", [128, 2, 258], BF16, s2)
                    t2 = sb("mt2", [128, 1], F32, s2); Bt2 = kb.buf()
                    t1 = sb("mt1", [128, 1], F32, s2); HFc = sb("mHFc", [128, 256], F32, s2); HS = sb("mHS", [128, 256], F32, s2)
                    junk = sb("mjunk", [128, 256], BF16, s2); ssq = sb("mssq", [128, 1], F32, s2)
                    HN = sb("mHN", [128, 256], BF16, s2); SG = sb("mSG", [128, 2, 128], F32, s2); YM = sb("mYM", [128, 2, 512], BF16, s2)
                    BST = kb.buf(); BKW = kb.buf(); BCst = kb.buf(); BCbf = kb.buf(); Bt1 = kb.buf(); BHFc = kb.buf(); BHS = kb.buf()
                    Bjk = kb.buf(); Bssq = kb.buf(); BHN = kb.buf(); BSG = kb.buf(); BYM = kb.buf()
                    pS, pO, pK, pP0, pP1, pOT = pb[0], pb[1], pb[2], pb[3], pb[4], pb[6]
                    BpS, BpO, BpK, BpP0, BpP1, BpT, BpOT = Bpb[0], Bpb[1], Bpb[2], Bpb[3], Bpb[4], Bpb[5], Bpb[6]
                    pTv = pTb[:, 0:256].rearrange("p (a b) -> p a b", b=128)
                    for d_ in range(2 if stopM == 0 else (0 if stopM == 3 else 1)):
                        op("dve", lambda e: e.memset(Cst[:], 0.0), writes=[BCst])
                        op("dve", lambda e: e.memset(Cbf[:], 0.0), writes=[BCbf])
                        chunks = list(range(NT)) if d_ == 0 else list(range(NT - 1, -1, -1))
                        if stopM == 4:
                            chunks = chunks[:4]
                        for c in chunks:
                            tsl = slice(c * 128, (c + 1) * 128)
                            for dc in range(2):
                                op("pe", lambda e: e.matmul(pS[:, 0:128], lhsT=KT[:, dc, tsl], rhs=QT[:, dc, tsl], start=(dc == 0), stop=(dc == 1)),
                                   reads=[BKT, BQT], writes=[BpS])
                            op("dve", lambda e: e.scalar_tensor_tensor(out=ST[:], in0=pS[:, 0:128], scalar=SC1[d_][:, c, h:h + 1], in1=mask[d_][:], op0=ALU.mult, op1=ALU.mult),
                               reads=[BpS, BGS, B_const], writes=[BST])
                            for dc in range(2):
                                op("pe", lambda e: e.matmul(pO[:, 0:258], lhsT=QT[:, dc, tsl], rhs=Cbf[:, dc, :], start=(dc == 0), stop=False),
                                   reads=[BQT, BCbf], writes=[BpO])
                            op("pe", lambda e: e.matmul(pO[:, 0:258], lhsT=ST[:], rhs=V[:, c, 0:258], start=False, stop=True), reads=[BST, BV], writes=[BpO])
                            for dc in range(2):
                                op("pe", lambda e: e.matmul(pK[:, 0:256], lhsT=XC[:, dc, tsl], rhs=wk[:, dc, :], start=(dc == 0), stop=(dc == 1)),
                                   reads=[BXC, Bwh], writes=[BpK])
                            op("dve", lambda e: e.tensor_scalar(out=KW[:], in0=pK[:, 0:256], scalar1=KWS[d_][:, c, h:h + 1], scalar2=None, op0=ALU.mult), reads=[BpK, BGS], writes=[BKW])
                            op("pe", lambda e: e.matmul(pP0[:, 0:258], lhsT=KW[:, 0:128], rhs=V[:, c, 0:258], start=True, stop=True), reads=[BKW, BV], writes=[BpP0])
                            op("pe", lambda e: e.matmul(pP1[:, 0:258], lhsT=KW[:, 128:256], rhs=V[:, c, 0:258], start=True, stop=True), reads=[BKW, BV], writes=[BpP1])
                            ebc = EB[d_][:, c, h:h + 1]
                            op("dve", lambda e: e.tensor_scalar(out=t1[:], in0=pO[:, 256:257], scalar1=ebc, scalar2=None, op0=ALU.mult), reads=[BpO, BGS], writes=[Bt1])
                            op("dve", lambda e: e.tensor_scalar(out=t2[:], in0=t1[:], scalar1=-1.0, scalar2=None, op0=ALU.mult), reads=[Bt1], writes=[Bt2])
                            op("dve", lambda e: e.tensor_tensor(out=t1[:], in0=t1[:], in1=t2[:], op=ALU.max), reads=[Bt1, Bt2], writes=[Bt1])
                            op("dve", lambda e: e.tensor_scalar(out=t1[:], in0=t1[:], scalar1=1.0, scalar2=None, op0=ALU.max), reads=[Bt1], writes=[Bt1])
                            op("dve", lambda e: e.reciprocal(out=t1[:], in_=t1[:]), reads=[Bt1], writes=[Bt1])
                            op("dve", lambda e: e.tensor_scalar(out=t1[:], in0=t1[:], scalar1=ebc, scalar2=None, op0=ALU.mult), reads=[Bt1, BGS], writes=[Bt1])
                            if d_ == 0:
                                op("dve", lambda e: e.tensor_scalar(out=HFc[:], in0=pO[:, 0:256], scalar1=t1[:], scalar2=None, op0=ALU.mult), reads=[BpO, Bt1], writes=[BHFc])
                                dma("sp", lambda e: e.dma_start(out=hf_scr[tsl, :], in_=HFc[:]), reads=[BHFc], writes=[B_hf[c]])
                            else:
                                dma("sp", lambda e: e.dma_start(out=HFc[:], in_=hf_scr[tsl, :]), reads=[B_hf[c]], writes=[BHFc])
                                op("dve", lambda e: e.scalar_tensor_tensor(out=HS[:], in0=pO[:, 0:256], scalar=t1[:], in1=HFc[:], op0=ALU.mult, op1=ALU.add),
                                   reads=[BpO, Bt1, BHFc], writes=[BHS])
                                rms_rstd("M", HS[:], 256, ssq[:], junk[:], BHS, Bssq, Bjk)
                                op("dve", lambda e: e.tensor_scalar(out=HN[:], in0=HS[:], scalar1=ssq[:], scalar2=None, op0=ALU.mult), reads=[BHS, Bssq], writes=[BHN])
                                for dc in range(2):
                                    op("pe", lambda e: e.transpose(out=pTv[:, dc, :], in_=HN[:, dc * 128:(dc + 1) * 128], identity=ident_b[:]), reads=[BHN, B_const], writes=[BpT])
                                for dc in range(2):
                                    for c8 in range(8):
                                        op("pe", lambda e: e.matmul(pOT[:, dc * 128:(dc + 1) * 128], lhsT=wo[:, c8, dc * 128:(dc + 1) * 128], rhs=hT[:, c8, tsl], start=(c8 == 0), stop=(c8 == 7)),
                                           reads=[Bwh, B_hT[c // 4]], writes=[BpOT])
                                op("act", lambda e: e.activation(out=SG[:].rearrange("p a b -> p (a b)"), in_=pOT[:, 0:256], func=AF.Sigmoid), reads=[BpOT], writes=[BSG])
                                q4 = c % 4
                                for dc in range(2):
                                    op("dve", lambda e: e.scalar_tensor_tensor(out=YM[:, dc, q4 * 128:(q4 + 1) * 128], in0=pTv[:, dc, :], scalar=vcol["mng"](h * 2 + dc), in1=SG[:, dc, :], op0=ALU.mult, op1=ALU.mult),
                                       reads=[BpT, BSG, B_const], writes=[BYM])
                                if q4 == 0:
                                    g = c // 4
                                    dma("sp", lambda e: e.dma_start(out=ymT[h * 256:(h + 1) * 256, g * 512:(g + 1) * 512].rearrange("(a p) t -> p a t", p=128), in_=YM[:]),
                                        reads=[BYM], writes=[B_ymT])
                            egc = EG[d_][:, c, h:h + 1]
                            op("dve", lambda e: e.scalar_tensor_tensor(out=Cst[:, 0, :], in0=Cst[:, 0, :], scalar=egc, in1=pP0[:, 0:258], op0=ALU.mult, op1=ALU.add),
                               reads=[BCst, BGS, BpP0], writes=[BCst])
                            op("dve", lambda e: e.scalar_tensor_tensor(out=Cst[:, 1, :], in0=Cst[:, 1, :], scalar=egc, in1=pP1[:, 0:258], op0=ALU.mult, op1=ALU.add),
                               reads=[BCst, BGS, BpP1], writes=[BCst])
                            op("pool", lambda e: e.tensor_copy(out=Cbf[:], in_=Cst[:]), reads=[BCst], writes=[BCbf])
                    kb.barrier()
            kb.barrier()
         except _Stop:
            kb.barrier()

        if dbg is not None and dbg[0] == "ym":
            with ExitStack() as cs:
                tb = sb("dbgb", [128, T], BF16, cs); t32 = sb("dbg32", [128, T], F32, cs)
                Bt = kb.buf(); Bt2 = kb.buf()
                for n in range(8):
                    dma("sp", lambda e: e.dma_start(out=tb[:], in_=ymT[n * 128:(n + 1) * 128, :]), reads=[B_ymT], writes=[Bt])
                    op("dve", lambda e: e.tensor_copy(out=t32[:], in_=tb[:]), reads=[Bt], writes=[Bt2])
                    dma("sp", lambda e: e.dma_start(out=dbg_out[n * 128:(n + 1) * 128, :], in_=t32[:]), reads=[Bt2])
                kb.barrier()


        B_mg = kb.buf("mgT")
        MP0 = 4112
        if dbg is None or dbg[0] in ("full", "x1", "idx"):
          with ExitStack() as cs:
            wbm_a = sb("wbm_a", [128, 8, D], BF16, cs); wbl_a = sb("wbl_a", [128, 8, D], BF16, cs)
            wgm_a = sb("wgm_a", [128, 8, D], BF16, cs); wgl_a = sb("wgl_a", [128, 8, D], BF16, cs)
            YMt = [sb("YMt%d" % i, [128, 8, 512], BF16, cs) for i in range(2)]; YLt = [sb("YLt%d" % i, [128, 8, 512], BF16, cs) for i in range(2)]
            GMs = [sb("GMs%d" % i, [128, 512], F32, cs) for i in range(2)]; GLs = [sb("GLs%d" % i, [128, 512], F32, cs) for i in range(2)]
            TM = [sb("TM%d" % i, [128, 512], F32, cs) for i in range(2)]
            MGe = [sb("MGe%d" % i, [128, 8, 512], BF16, cs) for i in range(2)]
            pE = [ps("pE%d" % i, [128, 512], F32, cs) for i in range(8)]
            Bwe = kb.buf(); BYM_ = kb.bufs(2); BYL_ = kb.bufs(2); BGM = kb.bufs(2); BGL = kb.bufs(2); BTM = kb.bufs(2); BMGe = kb.bufs(2)
            BpE = kb.bufs(8, "pE", excl=True)
            for c in range(8):
                rs = slice(c * 128, (c + 1) * 128)
                load_w_bf16(wbm_a[:, c, :], w_bm[rs, :], [Bwe])
                load_w_bf16(wbl_a[:, c, :], w_bl[rs, :], [Bwe])
                load_w_bf16(wgm_a[:, c, :], w_in[rs, MP0:MP0 + 1024], [Bwe])
                load_w_bf16(wgl_a[:, c, :], w_in[rs, MP0 + 1024:MP0 + 2048], [Bwe])
            it = 0
            for g in range(NG):
                gs = slice(g * 512, (g + 1) * 512)
                sg_ = g % 2
                dma("sp", lambda e: e.dma_start(out=YMt[sg_][:], in_=ymT[:, gs].rearrange("(c p) t -> p c t", p=128)), reads=[B_ymT], writes=[BYM_[sg_]])
                dma("sp", lambda e: e.dma_start(out=YLt[sg_][:], in_=ylT[:, gs].rearrange("(c p) t -> p c t", p=128)), reads=[B_ylT], writes=[BYL_[sg_]])
                for e_ in range(8):
                    es_ = slice(e_ * 128, (e_ + 1) * 128)
                    pz = (it % 2) * 4; tz = it % 2; it += 1
                    for c in range(8):
                        op("pe", lambda e: e.matmul(pE[pz + 0][:], lhsT=wbm_a[:, c, es_], rhs=YMt[sg_][:, c, :], start=(c == 0), stop=(c == 7)), reads=[Bwe, BYM_[sg_]], writes=[BpE[pz + 0]])
                    for c in range(8):
                        op("pe", lambda e: e.matmul(pE[pz + 1][:], lhsT=wbl_a[:, c, es_], rhs=YLt[sg_][:, c, :], start=(c == 0), stop=(c == 7)), reads=[Bwe, BYL_[sg_]], writes=[BpE[pz + 1]])
                    for c in range(8):
                        op("pe", lambda e: e.matmul(pE[pz + 2][:], lhsT=wgm_a[:, c, es_], rhs=hT[:, c, gs], start=(c == 0), stop=(c == 7)), reads=[Bwe, B_hT[g]], writes=[BpE[pz + 2]])
                    for c in range(8):
                        op("pe", lambda e: e.matmul(pE[pz + 3][:], lhsT=wgl_a[:, c, es_], rhs=hT[:, c, gs], start=(c == 0), stop=(c == 7)), reads=[Bwe, B_hT[g]], writes=[BpE[pz + 3]])
                    op("act", lambda e: e.activation(out=GMs[tz][:], in_=pE[pz + 2][:], func=AF.Sigmoid), reads=[BpE[pz + 2]], writes=[BGM[tz]])
                    op("act", lambda e: e.activation(out=GLs[tz][:], in_=pE[pz + 3][:], func=AF.Sigmoid), reads=[BpE[pz + 3]], writes=[BGL[tz]])
                    op("dve", lambda e: e.tensor_tensor(out=TM[tz][:], in0=GMs[tz][:], in1=pE[pz + 0][:], op=ALU.mult), reads=[BGM[tz], BpE[pz + 0]], writes=[BTM[tz]])
                    op("dve", lambda e: e.tensor_tensor(out=GLs[tz][:], in0=GLs[tz][:], in1=pE[pz + 1][:], op=ALU.mult), reads=[BGL[tz], BpE[pz + 1]], writes=[BGL[tz]])
                    op("pool", lambda e: e.tensor_tensor(out=MGe[sg_][:, e_, :], in0=TM[tz][:], in1=GLs[tz][:], op=ALU.add), reads=[BTM[tz], BGL[tz]], writes=[BMGe[sg_]])
                dma("sp", lambda e: e.dma_start(out=mgT[:, gs].rearrange("(c p) t -> p c t", p=128), in_=MGe[sg_][:]), reads=[BMGe[sg_]], writes=[B_mg])
            kb.barrier()
        hT_scope.close()
        B_uv = kb.buf("uvtab")
        if need_peer:
          with ExitStack() as us:
            UVt = [sb("UVt%d" % i, [128, 4, 2 * D], BF16, us) for i in range(2)]
            BUVu = kb.bufs(2); BUVv = kb.bufs(2)
            for blk in range(32):
                s_ = blk % 2
                rws = slice(blk * 512, (blk + 1) * 512)
                dma("pool", lambda e: e.dma_start(out=UVt[s_][:, :, 0:D], in_=p_u[rws, :].rearrange("(p a) d -> p a d", a=4)), writes=[BUVu[s_]])
                dma("pool", lambda e: e.dma_start(out=UVt[s_][:, :, D:2 * D], in_=p_v[rws, :].rearrange("(p a) d -> p a d", a=4)), writes=[BUVv[s_]])
                dma("sp", lambda e: e.dma_start(out=uv_tab[rws, :].rearrange("(p a) d -> p a d", a=4), in_=UVt[s_][:]), reads=[BUVu[s_], BUVv[s_]], writes=[B_uv])
            kb.barrier()

        if dbg is None or dbg[0] in ("full", "x1", "idx"):
          with ExitStack() as cs:
            do_peer = dbg is None or dbg[0] in ("full", "idx")
            g2_bc = sb("g2_bc", [128, D], F32, cs); gf_bc = sb("gf_bc", [128, D], F32, cs)
            dma("sp", lambda e: e.dma_start(out=g2_bc[:], in_=norm2_g.partition_broadcast(128)), writes=[B_const])
            dma("sp", lambda e: e.dma_start(out=gf_bc[:], in_=fin_g.partition_broadcast(128)), writes=[B_const])
            woutb = sb("woutb", [128, 8, D], BF16, cs)
            Bwo = kb.buf()
            for c in range(8):
                load_w_bf16(woutb[:, c, :], w_out[c * 128:(c + 1) * 128, :], [Bwo])
            pF = [ps("pF%d" % i, [128, 512], F32, cs) if i != 2 else ps("pFb", [128, 1024], BF16, cs) for i in range(8)]
            BpF = kb.bufs(8, "pF", excl=True)
            if do_peer:
                wpq = sb("wpq", [128, 8, 2048], BF16, cs)
                KEYT = sb("KEYT", [128, 16, 128], BF16, cs)
                Bwp = kb.buf()
                for c in range(8):
                    load_w_bf16(wpq[:, c, 0:1024], p_wq[c * 128:(c + 1) * 128, 0:1024], [Bwp])
                    load_w_bf16(wpq[:, c, 1024:2048], p_wq[c * 128:(c + 1) * 128, 1024:2048], [Bwp])
                with ExitStack() as ks:
                    KEYB = sb("KEYB", [128, 16, 128], BF16, ks)
                    Bkf = kb.buf()
                    for hp in range(16):
                        load_w_bf16(KEYB[:, hp, :], p_keys[hp], [Bkf])
                    for q in range(2):
                        for h8 in range(8):
                            hp = q * 8 + h8
                            op("pe", lambda e: e.transpose(out=pF[2][:, h8 * 128:(h8 + 1) * 128], in_=KEYB[:, hp, :], identity=ident_b[:]),
                               reads=[Bkf, B_const], writes=[BpF[2]])
                        op("act", lambda e: e.copy(out=KEYT[:, q * 8:(q + 1) * 8, :].rearrange("p a b -> p (a b)"), in_=pF[2][:]), reads=[BpF[2]], writes=[Bwp])
                    kb.barrier()
            MGg = sb("MGg", [128, 8, 512], BF16, cs); BMGg = kb.buf()
            xin = sb("xinE", [128, D], F32, cs); Bxin = kb.buf()
            x1t = sb("x1t", [128, D], F32, cs); Bx1 = kb.buf()
            ss2 = sb("ss2", [128, 1], F32, cs); Bss2 = kb.buf()
            junkF = sb("junkF", [128, D], BF16, cs); BjF = kb.buf()
            yt = sb("yt", [128, D], F32, cs); Byt = kb.buf()
            if do_peer:
                h2 = sb("h2", [128, D], F32, cs); Bh2 = kb.buf()
                h2b = sb("h2b", [128, D], BF16, cs); Bh2b = kb.buf()
                h2T = sb("h2T", [128, 8, 128], BF16, cs); Bh2T = kb.buf()
                QP = sb("QP", [128, 16, 128], BF16, cs); BQP = kb.buf()
                WK = sb("WK", [128, 16, 128], F32, cs); BWK = kb.buf()
                SCS = sb("SCS", [128, 16, 128], F32, cs); BSCS = kb.buf()
                v8 = sb("v8", [128, 16, 16], F32, cs); Bv8 = kb.buf()
                Bv8a = kb.bufs(16); Bv8b = kb.bufs(16); Bi8a = kb.bufs(16); Bi8b = kb.bufs(16); BWKa = kb.bufs(16)
                Bb8a = kb.bufs(8); Bb8b = kb.bufs(8); Bp8a = kb.bufs(8); Bp8b = kb.bufs(8); BCWa = kb.bufs(8)
                i8 = sb("i8", [128, 16, 16], U32, cs); Bi8 = kb.buf()
                i8f = sb("i8f", [128, 16, 16], F32, cs); Bi8f = kb.buf()
                CAND = sb("CAND", [128, 8, 256], F32, cs); BCAND = kb.buf()
                CW = sb("CW", [128, 8, 256], F32, cs); BCW = kb.buf()
                b8 = sb("b8", [128, 8, 16], F32, cs); Bb8 = kb.buf()
                p8 = sb("p8", [128, 8, 16], U32, cs); Bp8 = kb.buf()
                pff = sb("pff", [128, 8, 16], F32, cs)
                phf = sb("phf", [128, 8, 16], F32, cs); plf = sb("plf", [128, 8, 16], F32, cs); Bph = kb.buf()
                EQ = sb("EQ", [128, 128, 16], F32, cs); BEQ = kb.buf()
                ID0 = sb("ID0", [128, 128], F32, cs); ID1 = sb("ID1", [128, 128], F32, cs); BID = kb.buf()
                IDXu = sb("IDXu", [128, 128], U32, cs); BIDX = kb.buf()
                NB = sb("NB", [128, 8], F32, cs); Zs = sb("Zs", [128, 8], F32, cs); EX = sb("EX", [128, 8, 16], F32, cs); BSM = kb.buf()
                GATE = sb("GATE", [128, 128], F32, cs); BGATE = kb.buf()
                ACTV = sb("ACTV", [128, 128], F32, cs); BACTV = kb.buf()
                WT = sb("WT", [128, 128], F32, cs); BWT = kb.buf()
                NGB = 14
                GB = [sb("GB%d" % i, [128, 2 * D], BF16, cs) for i in range(NGB)]; BGB = kb.bufs(NGB)
                DG = [sb("DG%d" % i, [128, 128], BF16, cs) for i in range(8)]; BDG = kb.bufs(8)
                G1 = sb("G1", [128, 128], F32, cs)
                BAk = kb.bufs(32); BGk = kb.bufs(32)
                dgi = 0
                junkU = sb("junkU", [128, D], BF16, cs); BjU = kb.buf()
                gbi = 0
                pT2 = pF[2]; BpT2 = BpF[2]
                pT2v = pT2[:].rearrange("p (a b) -> p a b", b=128)
            ng_lim = dbg[3] if (dbg is not None and len(dbg) > 3) else NG
            for g in range(ng_lim):
                gs = slice(g * 512, (g + 1) * 512)
                dma("sp", lambda e: e.dma_start(out=MGg[:], in_=mgT[:, gs].rearrange("(c p) t -> p c t", p=128)), reads=[B_mg], writes=[BMGg])
                for tt in range(4):
                    i = g * 4 + tt
                    rows = slice(i * 128, (i + 1) * 128)
                    dma("sp", lambda e: e.dma_start(out=xin[:], in_=x[rows, :]), writes=[Bxin])
                    for half in range(2):
                        hsl = slice(half * 512, (half + 1) * 512)
                        for e_ in range(8):
                            op("pe", lambda e: e.matmul(pF[half][:], lhsT=MGg[:, e_, tt * 128:(tt + 1) * 128], rhs=woutb[:, e_, hsl], start=(e_ == 0), stop=(e_ == 7)),
                               reads=[BMGg, Bwo], writes=[BpF[half]])
                        op("dve", lambda e: e.tensor_tensor(out=x1t[:, hsl], in0=pF[half][:], in1=xin[:, hsl], op=ALU.add), reads=[BpF[half], Bxin], writes=[Bx1])
                    if dbg is not None and dbg[0] == "x1":
                        dma("sp", lambda e: e.dma_start(out=dbg_out[rows, :], in_=x1t[:]), reads=[Bx1])
                        continue
                    rms_rstd("P", x1t[:], D, ss2[:], junkF[:], Bx1, Bss2, BjF)
                    op("dve", lambda e: e.scalar_tensor_tensor(out=h2[:], in0=x1t[:], scalar=ss2[:], in1=g2_bc[:], op0=ALU.mult, op1=ALU.mult),
                       reads=[Bx1, Bss2, B_const], writes=[Bh2])
                    op("pool", lambda e: e.tensor_copy(out=h2b[:], in_=h2[:]), reads=[Bh2], writes=[Bh2b])
                    for c in range(8):
                        op("pe", lambda e: e.transpose(out=pT2v[:, c, :], in_=h2b[:, c * 128:(c + 1) * 128], identity=ident_b[:]), reads=[Bh2b, B_const], writes=[BpT2])
                    op("act", lambda e: e.copy(out=h2T[:], in_=pT2v), reads=[BpT2], writes=[Bh2T])
                    for hp in range(16):
                        bk = 3 + hp // 4
                        for c in range(8):
                            op("pe", lambda e: e.matmul(pF[bk][:, (hp % 4) * 128:(hp % 4 + 1) * 128], lhsT=wpq[:, c, hp * 128:(hp + 1) * 128], rhs=h2T[:, c, :], start=(c == 0), stop=(c == 7)),
                               reads=[Bwp, Bh2T], writes=[BpF[bk]])
                    for q in range(4):
                        op("act", lambda e: e.copy(out=QP[:, q * 4:(q + 1) * 4, :].rearrange("p a b -> p (a b)"), in_=pF[3 + q][:]), reads=[BpF[3 + q]], writes=[BQP])
                    for hp in range(16):
                        bk = 3 + hp // 4
                        op("pe", lambda e: e.matmul(pF[bk][:, (hp % 4) * 128:(hp % 4 + 1) * 128], lhsT=QP[:, hp, :], rhs=KEYT[:, hp, :], start=True, stop=True),
                           reads=[BQP, Bwp], writes=[BpF[bk]])
                    for q in range(4):
                        op("act", lambda e: e.copy(out=SCS[:, q * 4:(q + 1) * 4, :].rearrange("p a b -> p (a b)"), in_=pF[3 + q][:]), reads=[BpF[3 + q]], writes=[BSCS])
                    for hp in range(16):
                        op("dve", lambda e: e.max(out=v8[:, hp, 0:8], in_=SCS[:, hp, :]), reads=[BSCS], writes=[Bv8a[hp]])
                    for hp in range(16):
                        op("dve", lambda e: e.max_index(out=i8[:, hp, 0:8], in_max=v8[:, hp, 0:8], in_values=SCS[:, hp, :]), reads=[BSCS, Bv8a[hp]], writes=[Bi8a[hp]])
                    for hp in range(16):
                        op("dve", lambda e: e.match_replace(out=WK[:, hp, :], in_to_replace=v8[:, hp, 0:8], in_values=SCS[:, hp, :], imm_value=-1e30), reads=[BSCS, Bv8a[hp]], writes=[BWKa[hp]])
                    for hp in range(16):
                        op("dve", lambda e: e.max(out=v8[:, hp, 8:16], in_=WK[:, hp, :]), reads=[BWKa[hp]], writes=[Bv8b[hp]])
                    for hp in range(16):
                        op("dve", lambda e: e.max_index(out=i8[:, hp, 8:16], in_max=v8[:, hp, 8:16], in_values=WK[:, hp, :]), reads=[BWKa[hp], Bv8b[hp]], writes=[Bi8b[hp]])
                    op("dve", lambda e: e.tensor_copy(out=i8f[:], in_=i8[:]), reads=Bi8a + Bi8b, writes=[Bi8f])
                    v8v = v8[:].rearrange("p (h two) k -> p h two k", two=2)
                    i8v = i8f[:].rearrange("p (h two) k -> p h two k", two=2)
                    s0b = v8v[:, :, 0, :].unsqueeze(3).to_broadcast([128, 8, 16, 16])
                    s1b = v8v[:, :, 1, :].unsqueeze(2).to_broadcast([128, 8, 16, 16])
                    op("dve", lambda e: e.tensor_tensor(out=CAND[:].rearrange("p h (i j) -> p h i j", j=16), in0=s0b, in1=s1b, op=ALU.add), reads=Bv8a + Bv8b, writes=[BCAND])
                    for h in range(8):
                        op("dve", lambda e: e.max(out=b8[:, h, 0:8], in_=CAND[:, h, :]), reads=[BCAND], writes=[Bb8a[h]])
                    for h in range(8):
                        op("dve", lambda e: e.max_index(out=p8[:, h, 0:8], in_max=b8[:, h, 0:8], in_values=CAND[:, h, :]), reads=[BCAND, Bb8a[h]], writes=[Bp8a[h]])
                    for h in range(8):
                        op("dve", lambda e: e.match_replace(out=CW[:, h, :], in_to_replace=b8[:, h, 0:8], in_values=CAND[:, h, :], imm_value=-1e30), reads=[BCAND, Bb8a[h]], writes=[BCWa[h]])
                    for h in range(8):
                        op("dve", lambda e: e.max(out=b8[:, h, 8:16], in_=CW[:, h, :]), reads=[BCWa[h]], writes=[Bb8b[h]])
                    for h in range(8):
                        op("dve", lambda e: e.max_index(out=p8[:, h, 8:16], in_max=b8[:, h, 8:16], in_values=CW[:, h, :]), reads=[BCWa[h], Bb8b[h]], writes=[Bp8b[h]])
                    op("dve", lambda e: e.tensor_copy(out=pff[:], in_=p8[:]), reads=Bp8a + Bp8b, writes=[Bph])
                    pffb = pff[:].rearrange("p h k -> p (h k)").unsqueeze(2).to_broadcast([128, 128, 16])
                    io16m = iota16m[:].unsqueeze(1).to_broadcast([128, 128, 16])
                    op("dve", lambda e: e.tensor_tensor(out=EQ[:], in0=pffb, in1=io16m, op=ALU.is_ge), reads=[Bph, B_const], writes=[BEQ])
                    op("dve", lambda e: e.tensor_reduce(out=phf[:].rearrange("p h k -> p (h k)"), in_=EQ[:], axis=mybir.AxisListType.X, op=ALU.add), reads=[BEQ], writes=[Bph])
                    op("dve", lambda e: e.tensor_scalar(out=phf[:], in0=phf[:], scalar1=-1.0, scalar2=None, op0=ALU.add), reads=[Bph], writes=[Bph])
                    op("dve", lambda e: e.scalar_tensor_tensor(out=plf[:], in0=phf[:], scalar=-16.0, in1=pff[:], op0=ALU.mult, op1=ALU.add), reads=[Bph], writes=[Bph])
                    iob = iota16[:].unsqueeze(1).to_broadcast([128, 128, 16])
                    for (pf_, two, IDd) in ((phf, 0, ID0), (plf, 1, ID1)):
                        pfb = pf_[:].rearrange("p h k -> p (h k)").unsqueeze(2).to_broadcast([128, 128, 16])
                        op("dve", lambda e: e.tensor_tensor(out=EQ[:], in0=pfb, in1=iob, op=ALU.is_equal), reads=[Bph, B_const], writes=[BEQ])
                        tabb = i8v[:, :, two, :].unsqueeze(2).to_broadcast([128, 8, 16, 16])
                        op("dve", lambda e: e.tensor_tensor(out=EQ[:].rearrange("p (h k) i -> p h k i", k=16), in0=EQ[:].rearrange("p (h k) i -> p h k i", k=16), in1=tabb, op=ALU.mult),
                           reads=[BEQ, Bi8f], writes=[BEQ])
                        op("dve", lambda e: e.tensor_reduce(out=IDd[:], in_=EQ[:], axis=mybir.AxisListType.X, op=ALU.add), reads=[BEQ], writes=[BID])
                    op("dve", lambda e: e.scalar_tensor_tensor(out=ID0[:], in0=ID0[:], scalar=128.0, in1=ID1[:], op0=ALU.mult, op1=ALU.add), reads=[BID], writes=[BID])
                    op("dve", lambda e: e.tensor_scalar(out=IDXu[:], in0=ID0[:], scalar1=16383.0, scalar2=0.0, op0=ALU.min, op1=ALU.max), reads=[BID], writes=[BIDX])
                    op("dve", lambda e: e.tensor_scalar(out=NB[:], in0=b8[:, :, 0], scalar1=-1.0, scalar2=None, op0=ALU.mult), reads=Bb8a + Bb8b, writes=[BSM])
                    for h in range(8):
                        op("act", lambda e: e.activation(out=EX[:, h, :], in_=b8[:, h, :], func=AF.Exp, bias=NB[:, h:h + 1], scale=1.0, accum_out=Zs[:, h:h + 1]),
                           reads=Bb8a + Bb8b + [BSM], writes=[BSM])
                    op("dve", lambda e: e.reciprocal(out=Zs[:], in_=Zs[:]), reads=[BSM], writes=[BSM])
                    op("dve", lambda e: e.tensor_tensor(out=GATE[:].rearrange("p (h k) -> p h k", k=16), in0=EX[:], in1=Zs[:].unsqueeze(2).to_broadcast([128, 8, 16]), op=ALU.mult),
                       reads=[BSM], writes=[BGATE])
                    if dbg is not None and dbg[0] == "idx":
                        op("dve", lambda e: e.tensor_copy(out=yt[:, 0:128], in_=ID0[:]), reads=[BID], writes=[Byt])
                        op("dve", lambda e: e.tensor_copy(out=yt[:, 128:256], in_=GATE[:]), reads=[BGATE], writes=[Byt])
                        dma("sp", lambda e: e.dma_start(out=dbg_out[rows, :], in_=yt[:, 0:256]), reads=[Byt])
                        continue
                    for kb4 in range(32):
                        sl4 = []
                        for k in range(kb4 * 4, kb4 * 4 + 4):
                            sgb = gbi % NGB; gbi += 1
                            sl4.append(sgb)
                            dma("pool", lambda e: e.indirect_dma_start(out=GB[sgb][:], out_offset=None, in_=uv_tab[:, :],
                                                                       in_offset=bass.IndirectOffsetOnAxis(ap=IDXu[:, k:k + 1], axis=0)),
                                reads=[BIDX, B_uv], writes=[BGB[sgb]])
                            op("dve", lambda e: e.scalar_tensor_tensor(out=junkU[:], in0=GB[sgb][:, 0:D], scalar=1.0, in1=h2b[:], op0=ALU.mult, op1=ALU.mult, accum_out=ACTV[:, k:k + 1]),
                               reads=[BGB[sgb], Bh2b], writes=[BjU, BAk[kb4]])
                        k4 = slice(kb4 * 4, kb4 * 4 + 4)
                        op("act", lambda e: e.activation(out=G1[:, k4], in_=ACTV[:, k4], func=AF.Gelu), reads=[BAk[kb4]], writes=[BGk[kb4]])
                        op("dve", lambda e: e.tensor_tensor(out=WT[:, k4], in0=G1[:, k4], in1=GATE[:, k4], op=ALU.mult), reads=[BGk[kb4], BGATE], writes=[BGk[kb4]])
                        for j4, k in enumerate(range(kb4 * 4, kb4 * 4 + 4)):
                            sgb = sl4[j4]
                            sd = dgi % 8; dgi += 1
                            op("pool", lambda e: e.tensor_scalar(out=DG[sd][:], in0=ident_b[:], scalar1=WT[:, k:k + 1], scalar2=1.0, op0=ALU.mult, op1=ALU.mult), reads=[BGk[kb4], B_const], writes=[BDG[sd]])
                            op("pe", lambda e: e.matmul(pF[0][:], lhsT=DG[sd][:], rhs=GB[sgb][:, D:D + 512], start=(k == 0), stop=(k == 127)), reads=[BDG[sd], BGB[sgb]], writes=[BpF[0]])
                            op("pe", lambda e: e.matmul(pF[1][:], lhsT=DG[sd][:], rhs=GB[sgb][:, D + 512:2 * D], start=(k == 0), stop=(k == 127)), reads=[BDG[sd], BGB[sgb]], writes=[BpF[1]])
                    op("dve", lambda e: e.tensor_tensor(out=x1t[:, 0:512], in0=x1t[:, 0:512], in1=pF[0][:], op=ALU.add), reads=[Bx1, BpF[0]], writes=[Bx1])
                    op("dve", lambda e: e.tensor_tensor(out=x1t[:, 512:D], in0=x1t[:, 512:D], in1=pF[1][:], op=ALU.add), reads=[Bx1, BpF[1]], writes=[Bx1])
                    rms_rstd("F", x1t[:], D, ss2[:], junkF[:], Bx1, Bss2, BjF)
                    op("dve", lambda e: e.scalar_tensor_tensor(out=yt[:], in0=x1t[:], scalar=ss2[:], in1=gf_bc[:], op0=ALU.mult, op1=ALU.mult),
                       reads=[Bx1, Bss2, B_const], writes=[Byt])
                    dma("sp", lambda e: e.dma_start(out=(dbg_out if dbg is not None else y_out)[rows, :], in_=yt[:]), reads=[Byt])
            kb.barrier()

        kb.finish()
    return nc, in_names


_CONSTS = None


def _consts():
    global _CONSTS
    if _CONSTS is None:
        j = np.arange(128)
        _CONSTS = {
            "c_ident": np.eye(128, dtype=np.float32),
            "c_ut": (j[:, None] <= j[None, :]).astype(np.float32),
            "c_lt": (j[:, None] >= j[None, :]).astype(np.float32),
            "c_iota": np.tile(np.concatenate([np.arange(16), np.arange(16) * 16, np.tile([1.0, 0.0], NT)]).astype(np.float32)[None, :], (128, 1)),
        }
    return _CONSTS


def make_in_maps(inputs):
    f = lambda a: np.ascontiguousarray(np.asarray(a, dtype=np.float32))
    shared = {
        "norm1_g": f(inputs["norm1_g"][0]), "w_in": f(inputs["w_in"][0]), "b_gates": f(inputs["b_gates"][0]),
        "mlstm_conv_w": f(inputs["mlstm_conv_w"][0]), "mlstm_conv_b": f(inputs["mlstm_conv_b"][0]),
        "mlstm_w_q": f(inputs["mlstm_w_q"][0]), "mlstm_w_k": f(inputs["mlstm_w_k"][0]), "mlstm_w_v": f(inputs["mlstm_w_v"][0]),
        "mlstm_norm_g": f(inputs["mlstm_norm_g"][0]), "lru_conv_w": f(inputs["lru_conv_w"][0]), "lru_conv_b": f(inputs["lru_conv_b"][0]),
        "lru_w_r": f(inputs["lru_w_r"][0]), "lru_b_r": f(inputs["lru_b_r"][0]), "lru_w_i": f(inputs["lru_w_i"][0]),
        "lru_b_i": f(inputs["lru_b_i"][0]), "lru_lambda": f(inputs["lru_lambda"][0]),
        "w_branch_mlstm": f(inputs["w_branch_mlstm"][0]), "w_branch_lru": f(inputs["w_branch_lru"][0]), "w_out": f(inputs["w_out"][0]),
        "norm2_g": f(inputs["norm2_g"][0]), "peer_w_q": f(inputs["peer_w_q"][0]),
        "peer_sub_keys": f(inputs["peer_sub_keys"][0]).reshape(16, 128, 128),
        "peer_u": f(inputs["peer_u"][0]), "peer_v": f(inputs["peer_v"][0]), "final_norm_g": f(inputs["final_norm_g"]),
    }
    shared.update(_consts())
    xs = f(inputs["x"])
    return [dict(shared, x=xs[b]) for b in range(8)]


def kernel(**inputs):
    nc, names = build_program()
    in_maps = [{k: m[k] for k in names} for m in make_in_maps(inputs)]
    res = run_bass_kernel_spmd(nc, in_maps, core_ids=list(range(8)))
    return np.stack([np.asarray(r["y"], dtype=np.float32) for r in res.results], axis=0)
```

```python
import numpy as np
from contextlib import ExitStack
import concourse.bass as bass
import concourse.mybir as mybir
from concourse.bass_utils import run_bass_kernel_spmd

F32 = mybir.dt.float32
BF16 = mybir.dt.bfloat16
U32 = mybir.dt.uint32
I32 = mybir.dt.int32
AF = mybir.ActivationFunctionType
ALU = mybir.AluOpType

T = 4096
D = 1024
NT = T // 128
NG = T // 512
DIN = 6160
LN16 = float(np.log(16.0))
EPS = 1e-6


class Buf:
    __slots__ = ("name", "w", "r", "excl")

    def __init__(self, name, excl=False):
        self.name = name
        self.w = None
        self.r = {}
        self.excl = excl


class _Stop(Exception):
    pass


class KB:
    def __init__(self, nc, es, n_dma_sems=32):
        self.nc = nc
        self.es = es
        self.engs = {"pe": nc.tensor, "act": nc.scalar, "dve": nc.vector, "pool": nc.gpsimd, "sp": nc.sync}
        self.sem = {}
        self.cnt = {}
        self.semobj = {}
        for e in self.engs:
            s = es.enter_context(nc.semaphore("sem_" + e))
            self.semobj["E" + e] = s
            self.cnt["E" + e] = 0
        self.dma_keys = {"sp": [], "pool": []}
        for q, n in (("sp", n_dma_sems), ("pool", n_dma_sems)):
            for i in range(n):
                s = es.enter_context(nc.semaphore("sem_dma_%s%d" % (q, i)))
                k = "D%s%d" % (q, i)
                self.semobj[k] = s
                self.cnt[k] = 0
                self.dma_keys[q].append(k)
        self.dma_rr = {"sp": 0, "pool": 0}
        self.waited = {e: {} for e in self.engs}
        self.nbuf = 0
        self.all_dma_events = {}

    def buf(self, name=None, excl=False):
        self.nbuf += 1
        return Buf(name or "b%d" % self.nbuf, excl)

    def bufs(self, n, name="b", excl=False):
        return [self.buf("%s%d" % (name, i), excl) for i in range(n)]

    def _wait(self, eng, deps):
        wd = self.waited[eng]
        best = {}
        for (k, v) in deps:
            if wd.get(k, 0) >= v:
                continue
            if eng == "pe" and k == "Epe":
                continue
            if best.get(k, 0) < v:
                best[k] = v
        for k, v in best.items():
            self.engs[eng].wait_ge(self.semobj[k], v)
            wd[k] = v

    def _deps(self, reads, writes):
        deps = []
        for b in reads:
            if b.w is not None:
                deps.append(b.w)
            if b.excl:
                deps.extend(b.r.items())
        for b in writes:
            if b.w is not None:
                deps.append(b.w)
            deps.extend(b.r.items())
        return deps

    def _record(self, ev, reads, writes):
        k, v = ev
        for b in reads:
            if b.r.get(k, 0) < v:
                b.r[k] = v
        for b in writes:
            b.w = ev
            b.r = {}

    def op(self, eng, fn, reads=(), writes=()):
        self._wait(eng, self._deps(reads, writes))
        inst = fn(self.engs[eng])
        k = "E" + eng
        self.cnt[k] += 1
        inst.then_inc(self.semobj[k], 1)
        self._record((k, self.cnt[k]), reads, writes)

    def dma(self, eng, fn, reads=(), writes=()):
        k = self.dma_keys[eng][self.dma_rr[eng]]
        self.dma_rr[eng] = (self.dma_rr[eng] + 1) % len(self.dma_keys[eng])
        deps = self._deps(reads, writes)
        if self.cnt[k] > 0:
            deps.append((k, self.cnt[k]))
        self._wait(eng, deps)
        inst = fn(self.engs[eng])
        self.cnt[k] += 16
        inst.then_inc(self.semobj[k], 16)
        ev = (k, self.cnt[k])
        self._record(ev, reads, writes)
        self.all_dma_events[k] = self.cnt[k]
        return ev

    def barrier(self):
        deps = [(k, v) for k, v in self.cnt.items() if v > 0]
        for e in self.engs:
            self._wait(e, deps)

    def finish(self):
        deps = [(k, v) for k, v in self.cnt.items() if v > 0]
        self._wait("sp", deps)


def build_program(dbg=None):
    nc = bass.Bass("TRN2", target_bir_lowering=False)

    in_names = []
    need_peer = dbg is None or dbg[0] == "full"

    def din(name, shape, dt=F32):
        if name in ("peer_u", "peer_v") and not need_peer:
            return None
        in_names.append(name)
        return nc.dram_tensor(name, list(shape), dt, kind="ExternalInput").ap()

    x = din("x", [T, D])
    norm1_g = din("norm1_g", [D])
    w_in = din("w_in", [D, DIN])
    b_gates = din("b_gates", [16])
    m_conv_w = din("mlstm_conv_w", [4, D])
    m_conv_b = din("mlstm_conv_b", [D])
    m_wq = din("mlstm_w_q", [4, 256, 256])
    m_wk = din("mlstm_w_k", [4, 256, 256])
    m_wv = din("mlstm_w_v", [4, 256, 256])
    m_norm_g = din("mlstm_norm_g", [D])
    l_conv_w = din("lru_conv_w", [4, D])
    l_conv_b = din("lru_conv_b", [D])
    l_wr = din("lru_w_r", [2, 8, 128, 128])
    l_br = din("lru_b_r", [2, D])
    l_wi = din("lru_w_i", [2, 8, 128, 128])
    l_bi = din("lru_b_i", [2, D])
    l_lam = din("lru_lambda", [2, D])
    w_bm = din("w_branch_mlstm", [D, D])
    w_bl = din("w_branch_lru", [D, D])
    w_out = din("w_out", [D, D])
    norm2_g = din("norm2_g", [D])
    p_wq = din("peer_w_q", [D, 2048])
    p_keys = din("peer_sub_keys", [16, 128, 128])
    p_u = din("peer_u", [16384, D])
    p_v = din("peer_v", [16384, D])
    fin_g = din("final_norm_g", [D])
    c_ident = din("c_ident", [128, 128])
    c_ut = din("c_ut", [128, 128])
    c_lt = din("c_lt", [128, 128])
    c_iota = din("c_iota", [128, 32 + 2 * NT])

    y_out = nc.dram_tensor("y", [T, D], F32, kind="ExternalOutput").ap()
    ymT = nc.dram_tensor("scr_ymT", [D, T], BF16, kind="Internal").ap()
    ylT = nc.dram_tensor("scr_ylT", [D, T], BF16, kind="Internal").ap()
    hf_scr = nc.dram_tensor("scr_hf", [T, 256], F32, kind="Internal").ap()
    mgT = nc.dram_tensor("scr_mgT", [D, T], BF16, kind="Internal").ap()
    uv_tab = nc.dram_tensor("scr_uv", [16384, 2 * D], BF16, kind="Internal").ap()
    dbg_out = None
    if dbg is not None:
        dbg_out = nc.dram_tensor("dbg", list(dbg[1]), F32, kind="ExternalOutput").ap()

    es = ExitStack()
    with es:
        E = es.enter_context
        kb = KB(nc, es)
        op, dma = kb.op, kb.dma

        uid = [0]

        def sb(name, shape, dt=F32, ctx=None):
            uid[0] += 1
            return (ctx or es).enter_context(nc.sbuf_tensor("%s_%d" % (name, uid[0]), list(shape), dt))

        def ps(name, shape, dt=F32, ctx=None):
            uid[0] += 1
            return (ctx or es).enter_context(nc.psum_tensor("%s_%d" % (name, uid[0]), list(shape), dt))

        ident_f = sb("ident_f", [128, 128]); ident_b = sb("ident_b", [128, 128], BF16)
        ut_f = sb("ut_f", [128, 128]); lt_f = sb("lt_f", [128, 128])
        ones_f = sb("ones_f", [128, 128])
        iota16 = sb("iota16", [128, 16]); iota16m = sb("iota16m", [128, 16])
        onezero = sb("onezero", [128, NT, 2])
        B_const = kb.buf("const")
        dma("sp", lambda e: e.dma_start(out=ident_f[:], in_=c_ident[:, :]), writes=[B_const])
        dma("sp", lambda e: e.dma_start(out=ut_f[:], in_=c_ut[:, :]), writes=[B_const])
        dma("sp", lambda e: e.dma_start(out=lt_f[:], in_=c_lt[:, :]), writes=[B_const])
        dma("sp", lambda e: e.dma_start(out=iota16[:], in_=c_iota[:, 0:16]), writes=[B_const])
        dma("sp", lambda e: e.dma_start(out=iota16m[:], in_=c_iota[:, 16:32]), writes=[B_const])
        dma("sp", lambda e: e.dma_start(out=onezero[:].rearrange("p a b -> p (a b)"), in_=c_iota[:, 32:32 + 2 * NT]), writes=[B_const])
        op("dve", lambda e: e.tensor_copy(out=ident_b[:], in_=ident_f[:]), reads=[B_const], writes=[B_const])
        op("dve", lambda e: e.memset(ones_f[:], 1.0), writes=[B_const])
        NV = 0
        vec_specs = [("g1", norm1_g, None), ("mcb", m_conv_b, None), ("mng", m_norm_g, None), ("lcb", l_conv_b, None)]
        for j in range(4):
            vec_specs.append(("mcw%d" % j, m_conv_w, j))
            vec_specs.append(("lcw%d" % j, l_conv_w, j))
        for d_ in range(2):
            vec_specs.append(("lbr%d" % d_, l_br, d_))
            vec_specs.append(("lbi%d" % d_, l_bi, d_))
            vec_specs.append(("lam%d" % d_, l_lam, d_))
        vecs = sb("vecs", [128, len(vec_specs), 8])
        vcol = {}
        for i, (nm, ap_, row) in enumerate(vec_specs):
            src = ap_ if row is None else ap_[row]
            src = src.rearrange("(c p) -> p c", p=128)
            dma("sp", lambda e, i=i, src=src: e.dma_start(out=vecs[:, i, :], in_=src, allow_slow_non_contiguous=True),
                writes=[B_const])
            vcol[nm] = (lambda i: (lambda c: vecs[:, i, c:c + 1]))(i)
        cdec = sb("cdec", [128, 2, 8])
        for d_ in range(2):
            li = [i for i, s in enumerate(vec_specs) if s[0] == "lam%d" % d_][0]
            op("act", lambda e: e.activation(out=cdec[:, d_, :], in_=vecs[:, li, :], func=AF.Exp, scale=-1.0),
               reads=[B_const], writes=[B_const])
            op("act", lambda e: e.activation(out=cdec[:, d_, :], in_=cdec[:, d_, :], func=AF.Ln, bias=1.0, scale=1.0),
               reads=[B_const], writes=[B_const])
            op("dve", lambda e: e.tensor_scalar(out=cdec[:, d_, :], in0=cdec[:, d_, :], scalar1=-8.0, scalar2=None,
                                                op0=ALU.mult), reads=[B_const], writes=[B_const])
        bg_bc = sb("bg_bc", [128, 16])
        dma("sp", lambda e: e.dma_start(out=bg_bc[:], in_=b_gates.partition_broadcast(128)), writes=[B_const])
        epsc = sb("epsc", [128, 4])
        op("dve", lambda e: e.memset(epsc[:, 0:1], EPS), writes=[B_const])
        op("dve", lambda e: e.memset(epsc[:, 1:2], -LN16), writes=[B_const])
        op("dve", lambda e: e.memset(epsc[:, 2:3], 1.0), writes=[B_const])
        op("dve", lambda e: e.memset(epsc[:, 3:4], 0.0), writes=[B_const])
        eps_col, nln16_col, one_col, zero_col = epsc[:, 0:1], epsc[:, 1:2], epsc[:, 2:3], epsc[:, 3:4]

        hT_scope = ExitStack()
        hT = sb("hT", [128, 8, T], BF16, hT_scope)
        B_hT = kb.bufs(NG, "hT")

        def load_w_bf16(dst, src_rows, bufs_w, reads=()):
            dma("pool", lambda e: e.dma_start(out=dst, in_=src_rows), reads=list(reads), writes=list(bufs_w))

        def rms_rstd(ctx_name, src, n, ss, tmp_junk, B_in, B_ss, B_junk):
            op("act", lambda e: e.activation(out=tmp_junk, in_=src, func=AF.Square, accum_out=ss),
               reads=[B_in], writes=[B_ss, B_junk])
            op("act", lambda e: e.activation(out=ss, in_=ss, func=AF.Sqrt, scale=1.0 / n, bias=eps_col),
               reads=[B_ss, B_const], writes=[B_ss])
            op("dve", lambda e: e.reciprocal(out=ss, in_=ss), reads=[B_ss], writes=[B_ss])

        with ExitStack() as cs:
            xin = [sb("xa%d" % i, [128, D], F32, cs) for i in range(2)]
            xsb = [sb("xsb%d" % i, [128, D], BF16, cs) for i in range(2)]
            junk = sb("junkA", [128, D], BF16, cs)
            ssA = [sb("ssA%d" % i, [128, 1], F32, cs) for i in range(2)]
            tpA = [ps("tpA%d" % i, [128, 8, 128], BF16, cs) for i in range(2)]
            Bx = kb.bufs(2, "xa"); Bxs = kb.bufs(2, "xsb"); Bj = kb.buf("junkA"); Bss = kb.bufs(2, "ssA"); Btp = kb.bufs(2, "tpA", excl=True)
            for i in range(NT):
                s = i % 2
                dma("sp", lambda e: e.dma_start(out=xin[s][:], in_=x[i * 128:(i + 1) * 128, :]), writes=[Bx[s]])
                rms_rstd("A", xin[s][:], D, ssA[s][:], junk[:], Bx[s], Bss[s], Bj)
                op("dve", lambda e: e.tensor_scalar(out=xsb[s][:], in0=xin[s][:], scalar1=ssA[s][:], scalar2=None, op0=ALU.mult),
                   reads=[Bx[s], Bss[s]], writes=[Bxs[s]])
                for c in range(8):
                    op("pe", lambda e: e.transpose(out=tpA[s][:, c, :], in_=xsb[s][:, c * 128:(c + 1) * 128], identity=ident_b[:]),
                       reads=[Bxs[s], B_const], writes=[Btp[s]])
                g1b = vecs[:, 0, :].unsqueeze(2).to_broadcast([128, 8, 128])
                op("dve", lambda e: e.tensor_tensor(out=hT[:, :, i * 128:(i + 1) * 128], in0=tpA[s][:], in1=g1b, op=ALU.mult),
                   reads=[Btp[s], B_const], writes=[B_hT[i // 4]])
            kb.barrier()

        if dbg is not None and dbg[0] == "hT":
            with ExitStack() as cs:
                t32 = sb("dbg32", [128, 8, 512], F32, cs)
                Bt = kb.buf()
                for g in range(NG):
                    op("dve", lambda e: e.tensor_copy(out=t32[:], in_=hT[:, :, g * 512:(g + 1) * 512]), reads=[B_hT[g]], writes=[Bt])
                    dma("sp", lambda e: e.dma_start(out=dbg_out.rearrange("(c p) t -> p c t", p=128)[:, :, g * 512:(g + 1) * 512], in_=t32[:]), reads=[Bt])
                kb.barrier()

        B_ylT = kb.buf("ylT")
        B_ymT = kb.buf("ymT")
        XL0 = 2064
        LG0 = 3088
        if dbg is None or dbg[0] in ("yl", "full", "x1", "idx"):
          with ExitStack() as cs:
            wxl = sb("wxl", [128, 8, 128], BF16, cs); wlg = sb("wlg", [128, 8, 128], BF16, cs)
            wgate = sb("wgate", [128, 4, 128], BF16, cs)
            XL = sb("XL", [128, T + 4], F32, cs)
            XC = sb("XC", [128, T], F32, cs)
            XCB = sb("XCB", [128, T], BF16, cs)
            LG = sb("LG", [128, T], BF16, cs)
            HF = sb("HF", [128, T], F32, cs)
            NTMP = 2
            R_ = [sb("R%d" % i, [128, 512], F32, cs) for i in range(NTMP)]
            IG_ = [sb("IG%d" % i, [128, 512], F32, cs) for i in range(NTMP)]
            T_ = [sb("T%d" % i, [128, 512], F32, cs) for i in range(NTMP)]
            pL = [ps("pL%d" % i, [128, 512], F32, cs) for i in range(4)]
            Bw = kb.buf("wL"); BXL = kb.buf("XL"); BXC = kb.buf("XC"); BXCB = kb.buf("XCB"); BLG = kb.buf("LG"); BHF = kb.buf("HF")
            BR = kb.bufs(NTMP, "R"); BIG = kb.bufs(NTMP, "IG"); BT = kb.bufs(NTMP, "T"); BpL = kb.bufs(4, "pL", excl=True)
            HB = XL
            pli = 0
            for n in range(8):
                for c in range(8):
                    load_w_bf16(wxl[:, c, :], w_in[c * 128:(c + 1) * 128, XL0 + n * 128: XL0 + (n + 1) * 128], [Bw])
                    load_w_bf16(wlg[:, c, :], w_in[c * 128:(c + 1) * 128, LG0 + n * 128: LG0 + (n + 1) * 128], [Bw])
                for d_ in range(2):
                    load_w_bf16(wgate[:, d_ * 2 + 0, :], l_wr[d_, n], [Bw])
                    load_w_bf16(wgate[:, d_ * 2 + 1, :], l_wi[d_, n], [Bw])
                op("pool", lambda e: e.memset(XL[:, 0:2], 0.0), writes=[BXL])
                op("pool", lambda e: e.memset(XL[:, T + 2:T + 4], 0.0), writes=[BXL])
                for g in range(NG):
                    p1 = pli % 4; pli += 1
                    for c in range(8):
                        op("pe", lambda e: e.matmul(pL[p1][:], lhsT=wxl[:, c, :], rhs=hT[:, c, g * 512:(g + 1) * 512], start=(c == 0), stop=(c == 7)),
                           reads=[Bw, B_hT[g]], writes=[BpL[p1]])
                    op("act", lambda e: e.copy(out=XL[:, 2 + g * 512: 2 + (g + 1) * 512], in_=pL[p1][:]), reads=[BpL[p1]], writes=[BXL])
                    p2 = pli % 4; pli += 1
                    for c in range(8):
                        op("pe", lambda e: e.matmul(pL[p2][:], lhsT=wlg[:, c, :], rhs=hT[:, c, g * 512:(g + 1) * 512], start=(c == 0), stop=(c == 7)),
                           reads=[Bw, B_hT[g]], writes=[BpL[p2]])
                    op("act", lambda e: e.activation(out=LG[:, g * 512:(g + 1) * 512], in_=pL[p2][:], func=AF.Gelu), reads=[BpL[p2]], writes=[BLG])
                op("dve", lambda e: e.tensor_scalar(out=XC[:], in0=XL[:, 0:T], scalar1=vcol["lcw0"](n), scalar2=vcol["lcb"](n), op0=ALU.mult, op1=ALU.add),
                   reads=[BXL, B_const], writes=[BXC])
                for j in range(1, 4):
                    op("dve", lambda e: e.scalar_tensor_tensor(out=XC[:], in0=XL[:, j:j + T], scalar=vcol["lcw%d" % j](n), in1=XC[:], op0=ALU.mult, op1=ALU.add),
                       reads=[BXL, BXC, B_const], writes=[BXC])
                op("pool", lambda e: e.tensor_copy(out=XCB[:], in_=XC[:]), reads=[BXC], writes=[BXCB])
                ti = 0
                for d_ in range(2):
                    groups = list(range(NG)) if d_ == 0 else list(range(NG - 1, -1, -1))
                    Hd = HF if d_ == 0 else HB
                    BHd = BHF if d_ == 0 else BXL
                    hoff = 0 if d_ == 0 else 2
                    prev = None
                    for g in groups:
                        s = ti % NTMP; ti += 1
                        sl = slice(g * 512, (g + 1) * 512)
                        p1 = pli % 4; pli += 1
                        op("pe", lambda e: e.matmul(pL[p1][:], lhsT=wgate[:, d_ * 2, :], rhs=XCB[:, sl], start=True, stop=True),
                           reads=[Bw, BXCB], writes=[BpL[p1]])
                        op("act", lambda e: e.activation(out=R_[s][:], in_=pL[p1][:], func=AF.Sigmoid, bias=vcol["lbr%d" % d_](n)),
                           reads=[BpL[p1], B_const], writes=[BR[s]])
                        p2 = pli % 4; pli += 1
                        op("pe", lambda e: e.matmul(pL[p2][:], lhsT=wgate[:, d_ * 2 + 1, :], rhs=XCB[:, sl], start=True, stop=True),
                           reads=[Bw, BXCB], writes=[BpL[p2]])
                        op("act", lambda e: e.activation(out=IG_[s][:], in_=pL[p2][:], func=AF.Sigmoid, bias=vcol["lbi%d" % d_](n)),
                           reads=[BpL[p2], B_const], writes=[BIG[s]])
                        op("act", lambda e: e.activation(out=R_[s][:], in_=R_[s][:], func=AF.Exp, scale=cdec[:, d_, n:n + 1]),
                           reads=[BR[s], B_const], writes=[BR[s]])
                        op("pool", lambda e: e.tensor_tensor(out=T_[s][:], in0=R_[s][:], in1=R_[s][:], op=ALU.mult), reads=[BR[s]], writes=[BT[s]])
                        op("act", lambda e: e.activation(out=T_[s][:], in_=T_[s][:], func=AF.Sqrt, scale=-1.0, bias=one_col),
                           reads=[BT[s], B_const], writes=[BT[s]])
                        op("pool", lambda e: e.tensor_tensor(out=IG_[s][:], in0=IG_[s][:], in1=XC[:, sl], op=ALU.mult), reads=[BIG[s], BXC], writes=[BIG[s]])
                        op("pool", lambda e: e.tensor_tensor(out=T_[s][:], in0=T_[s][:], in1=IG_[s][:], op=ALU.mult), reads=[BT[s], BIG[s]], writes=[BT[s]])
                        if d_ == 0:
                            init = 0.0 if prev is None else HF[:, prev * 512 + 511: prev * 512 + 512]
                            op("dve", lambda e: e.tensor_tensor_scan(out=HF[:, sl], data0=R_[s][:], data1=T_[s][:], initial=init, op0=ALU.mult, op1=ALU.add),
                               reads=[BR[s], BT[s], BHF], writes=[BHF])
                        else:
                            init = 0.0 if prev is None else HB[:, 2 + prev * 512: 2 + prev * 512 + 1]
                            op("dve", lambda e: e.tensor_tensor_scan(out=HB[:, 2 + g * 512: 2 + (g + 1) * 512][:, ::-1], data0=R_[s][:, ::-1], data1=T_[s][:, ::-1],
                                                                    initial=init, op0=ALU.mult, op1=ALU.add),
                               reads=[BR[s], BT[s], BXL], writes=[BXL])
                        prev = g
                op("dve", lambda e: e.tensor_tensor(out=HF[:], in0=HF[:], in1=HB[:, 2:2 + T], op=ALU.add), reads=[BHF, BXL], writes=[BHF])
                op("dve", lambda e: e.tensor_tensor(out=XCB[:], in0=HF[:], in1=LG[:], op=ALU.mult), reads=[BHF, BLG], writes=[BXCB])
                dma("sp", lambda e: e.dma_start(out=ylT[n * 128:(n + 1) * 128, :], in_=XCB[:]), reads=[BXCB], writes=[B_ylT])
            kb.barrier()

        if dbg is not None and dbg[0] == "yl":
            with ExitStack() as cs:
                tb = sb("dbgb", [128, T], BF16, cs); t32 = sb("dbg32", [128, T], F32, cs)
                Bt = kb.buf(); Bt2 = kb.buf()
                for n in range(8):
                    dma("sp", lambda e: e.dma_start(out=tb[:], in_=ylT[n * 128:(n + 1) * 128, :]), reads=[B_ylT], writes=[Bt])
                    op("dve", lambda e: e.tensor_copy(out=t32[:], in_=tb[:]), reads=[Bt], writes=[Bt2])
                    dma("sp", lambda e: e.dma_start(out=dbg_out[n * 128:(n + 1) * 128, :], in_=t32[:]), reads=[Bt2])
                kb.barrier()


        B_hf = kb.bufs(NT, "hfscr")
        stopM = dbg[2] if (dbg is not None and len(dbg) > 2) else 0
        if dbg is None or dbg[0] in ("ym", "full", "x1", "idx"):
         try:
          with ExitStack() as ms:
            wg = sb("wg", [128, 8, 16], BF16, ms)
            G = sb("G", [128, NT, 16], F32, ms)
            LF = [sb("LF%d" % d_, [128, NT, 4], F32, ms) for d_ in range(2)]
            SC1 = [sb("SC1%d" % d_, [128, NT, 4], F32, ms) for d_ in range(2)]
            EB = [sb("EB%d" % d_, [128, NT, 4], F32, ms) for d_ in range(2)]
            EG = [sb("EG%d" % d_, [128, NT, 4], F32, ms) for d_ in range(2)]
            KWS = [sb("KWS%d" % d_, [128, NT, 4], F32, ms) for d_ in range(2)]
            pb = [ps("pM%d" % i, [128, 512], F32, ms) for i in range(7)]
            pTb = ps("pMT", [128, 1024], BF16, ms)
            Bpb = kb.bufs(8, "pM", excl=True)
            ut_b = sb("ut_b", [128, 128], BF16, ms); lt_b = sb("lt_b", [128, 128], BF16, ms); ones_b = sb("ones_b", [128, 128], BF16, ms)
            op("dve", lambda e: e.tensor_copy(out=ut_b[:], in_=ut_f[:]), reads=[B_const], writes=[B_const])
            op("dve", lambda e: e.tensor_copy(out=lt_b[:], in_=lt_f[:]), reads=[B_const], writes=[B_const])
            op("dve", lambda e: e.memset(ones_b[:], 1.0), writes=[B_const])
            LFh = sb("LFh", [128, NT * 4], BF16, ms); LFl = sb("LFl", [128, NT * 4], BF16, ms); LFr = sb("LFr", [128, NT * 4], F32, ms)
            Bwg = kb.buf("wg"); BG = kb.buf("G"); BGS = kb.buf("Gscal")
            wgf = sb("wgf", [128, 8, 16], F32, ms)
            Bwgf = kb.buf("wgf")
            dma("sp", lambda e: e.dma_start(out=wgf[:], in_=w_in[:, 2048:2064].rearrange("(c p) n -> p c n", p=128)), writes=[Bwgf])
            op("dve", lambda e: e.tensor_copy(out=wg[:], in_=wgf[:]), reads=[Bwgf], writes=[Bwg])
            for i in range(NT):
                p = i % 2
                for c in range(8):
                    op("pe", lambda e: e.matmul(pb[p][:, 0:16], lhsT=hT[:, c, i * 128:(i + 1) * 128], rhs=wg[:, c, :], start=(c == 0), stop=(c == 7)),
                       reads=[Bwg, B_hT[i // 4]], writes=[Bpb[p]])
                op("dve", lambda e: e.tensor_tensor(out=G[:, i, :], in0=pb[p][:, 0:16], in1=bg_bc[:], op=ALU.add), reads=[Bpb[p], B_const], writes=[BG])
            mask = [ut_f, lt_f]
            for d_ in range(2):
                fcol = (2 * d_ + 1) * 4
                icol = (2 * d_) * 4
                op("act", lambda e: e.activation(out=LF[d_][:], in_=G[:, :, fcol:fcol + 4], func=AF.Exp, scale=-1.0), reads=[BG], writes=[BGS])
                op("act", lambda e: e.activation(out=LF[d_][:], in_=LF[d_][:], func=AF.Ln, bias=one_col, scale=1.0), reads=[BGS, B_const], writes=[BGS])
                op("dve", lambda e: e.tensor_scalar(out=LF[d_][:], in0=LF[d_][:], scalar1=-1.0, scalar2=None, op0=ALU.mult), reads=[BGS], writes=[BGS])
                pB = pb[2 + d_ * 2]; pGt = pb[3 + d_ * 2]
                maskb = [ut_b, lt_b]
                lf2 = LF[d_][:].rearrange("p a b -> p (a b)")
                op("dve", lambda e: e.tensor_copy(out=LFh[:], in_=lf2), reads=[BGS], writes=[BGS])
                op("dve", lambda e: e.tensor_tensor(out=LFr[:], in0=lf2, in1=LFh[:], op=ALU.subtract), reads=[BGS], writes=[BGS])
                op("dve", lambda e: e.tensor_copy(out=LFl[:], in_=LFr[:]), reads=[BGS], writes=[BGS])
                op("pe", lambda e: e.matmul(pB[:, 0:128], lhsT=maskb[d_][:], rhs=LFh[:], start=True, stop=False), reads=[BGS, B_const], writes=[Bpb[2 + d_ * 2]])
                op("pe", lambda e: e.matmul(pB[:, 0:128], lhsT=maskb[d_][:], rhs=LFl[:], start=False, stop=True), reads=[BGS, B_const], writes=[Bpb[2 + d_ * 2]])
                op("pe", lambda e: e.matmul(pGt[:, 0:128], lhsT=ones_b[:], rhs=LFh[:], start=True, stop=False), reads=[BGS, B_const], writes=[Bpb[3 + d_ * 2]])
                op("pe", lambda e: e.matmul(pGt[:, 0:128], lhsT=ones_b[:], rhs=LFl[:], start=False, stop=True), reads=[BGS, B_const], writes=[Bpb[3 + d_ * 2]])
                pBv = pB[:, 0:128].rearrange("p (a b) -> p a b", b=4)
                pGv = pGt[:, 0:128].rearrange("p (a b) -> p a b", b=4)
                op("dve", lambda e: e.tensor_tensor(out=SC1[d_][:], in0=G[:, :, icol:icol + 4], in1=pBv, op=ALU.subtract), reads=[BG, Bpb[2 + d_ * 2]], writes=[BGS])
                op("act", lambda e: e.activation(out=SC1[d_][:], in_=SC1[d_][:], func=AF.Exp, bias=nln16_col, scale=1.0), reads=[BGS, B_const], writes=[BGS])
                op("act", lambda e: e.activation(out=EB[d_][:], in_=pBv, func=AF.Exp), reads=[Bpb[2 + d_ * 2]], writes=[BGS])
                op("act", lambda e: e.activation(out=EG[d_][:], in_=pGv, func=AF.Exp), reads=[Bpb[3 + d_ * 2]], writes=[BGS])
                op("dve", lambda e: e.tensor_tensor(out=KWS[d_][:], in0=SC1[d_][:], in1=EG[d_][:], op=ALU.mult), reads=[BGS], writes=[BGS])

            for h in range(4 if stopM == 0 else (0 if stopM == 1 else 1)):
              with ExitStack() as hs:
                wxm = sb("wxm", [128, 8, 256], BF16, hs); wo = sb("wo", [128, 8, 256], BF16, hs)
                wq = sb("wq", [128, 2, 256], BF16, hs); wk = sb("wk", [128, 2, 256], BF16, hs); wv = sb("wv", [128, 2, 256], BF16, hs)
                XC = sb("mXC", [128, 2, T], BF16, hs)
                V = sb("mV", [128, NT, 260], BF16, hs)
                Bwh = kb.buf("wh"); BXC = kb.buf("mXC"); BV = kb.buf("mV")
                for c in range(8):
                    load_w_bf16(wxm[:, c, :], w_in[c * 128:(c + 1) * 128, h * 256:(h + 1) * 256], [Bwh])
                    load_w_bf16(wo[:, c, :], w_in[c * 128:(c + 1) * 128, 1024 + h * 256: 1024 + (h + 1) * 256], [Bwh])
                for dc in range(2):
                    load_w_bf16(wq[:, dc, :], m_wq[h, dc * 128:(dc + 1) * 128, :], [Bwh])
                    load_w_bf16(wk[:, dc, :], m_wk[h, dc * 128:(dc + 1) * 128, :], [Bwh])
                    load_w_bf16(wv[:, dc, :], m_wv[h, dc * 128:(dc + 1) * 128, :], [Bwh])
                op("dve", lambda e: e.tensor_copy(out=V[:, :, 256:258], in_=onezero[:]), reads=[B_const], writes=[BV])
                pi = 0
                with ExitStack() as s1:
                    XM = sb("mXM", [128, T + 4], F32, s1)
                    XMB = sb("mXMB", [128, 2, T], BF16, s1)
                    XCF = [sb("mXCF%d" % i, [128, 1024], F32, s1) for i in range(2)]
                    XSG = [sb("mXSG%d" % i, [128, 1024], F32, s1) for i in range(2)]; BXSG = kb.bufs(2, "mXSG")
                    BXM = kb.buf("mXM"); BXMB = kb.buf("mXMB"); BXCF = kb.bufs(2, "mXCF")
                    op("pool", lambda e: e.memset(XM[:, 0:2], 0.0), writes=[BXM])
                    op("pool", lambda e: e.memset(XM[:, T + 2:T + 4], 0.0), writes=[BXM])
                    for cc in range(2):
                        ch = h * 2 + cc
                        for g in range(NG):
                            p = pi % 4; pi += 1
                            for c in range(8):
                                op("pe", lambda e: e.matmul(pb[p][:], lhsT=wxm[:, c, cc * 128:(cc + 1) * 128], rhs=hT[:, c, g * 512:(g + 1) * 512], start=(c == 0), stop=(c == 7)),
                                   reads=[Bwh, B_hT[g]], writes=[Bpb[p]])
                            op("act", lambda e: e.copy(out=XM[:, 2 + g * 512:2 + (g + 1) * 512], in_=pb[p][:]), reads=[Bpb[p]], writes=[BXM])
                            op("dve", lambda e: e.tensor_copy(out=XMB[:, cc, g * 512:(g + 1) * 512], in_=pb[p][:]), reads=[Bpb[p]], writes=[BXMB])
                        for q4 in range(4):
                            s = q4 % 2
                            o0 = q4 * 1024
                            op("dve", lambda e: e.tensor_scalar(out=XCF[s][:], in0=XM[:, o0:o0 + 1024], scalar1=vcol["mcw0"](ch), scalar2=vcol["mcb"](ch), op0=ALU.mult, op1=ALU.add),
                               reads=[BXM, B_const], writes=[BXCF[s]])
                            for j in range(1, 4):
                                op("dve", lambda e: e.scalar_tensor_tensor(out=XCF[s][:], in0=XM[:, o0 + j:o0 + j + 1024], scalar=vcol["mcw%d" % j](ch), in1=XCF[s][:], op0=ALU.mult, op1=ALU.add),
                                   reads=[BXM, BXCF[s], B_const], writes=[BXCF[s]])
                            op("act", lambda e: e.activation(out=XSG[s][:], in_=XCF[s][:], func=AF.Sigmoid), reads=[BXCF[s]], writes=[BXSG[s]])
                            op("dve", lambda e: e.tensor_tensor(out=XC[:, cc, o0:o0 + 1024], in0=XCF[s][:], in1=XSG[s][:], op=ALU.mult), reads=[BXCF[s], BXSG[s]], writes=[BXC])
                    for i in range(NT):
                        p = 4 + (i % 2)
                        for dc in range(2):
                            op("pe", lambda e: e.matmul(pb[p][:, 0:256], lhsT=XMB[:, dc, i * 128:(i + 1) * 128], rhs=wv[:, dc, :], start=(dc == 0), stop=(dc == 1)),
                               reads=[BXMB, Bwh], writes=[Bpb[p]])
                        op("act", lambda e: e.copy(out=V[:, i, 0:256], in_=pb[p][:, 0:256]), reads=[Bpb[p]], writes=[BV])
                    kb.barrier()
                if stopM == 2:
                    continue
                with ExitStack() as s2:
                    QT = sb("mQT", [128, 2, T], BF16, s2); KT = sb("mKT", [128, 2, T], BF16, s2)
                    BQT = kb.buf("mQT"); BKT = kb.buf("mKT")
                    for (dst, Bd, wmat) in ((QT, BQT, wq), (KT, BKT, wk)):
                        for ec in range(2):
                            for g in range(NG):
                                p = pi % 4; pi += 1
                                for dc in range(2):
                                    op("pe", lambda e: e.matmul(pb[p][:], lhsT=wmat[:, dc, ec * 128:(ec + 1) * 128], rhs=XC[:, dc, g * 512:(g + 1) * 512], start=(dc == 0), stop=(dc == 1)),
                                       reads=[Bwh, BXC], writes=[Bpb[p]])
                                op("act", lambda e: e.copy(out=dst[:, ec, g * 512:(g + 1) * 512], in_=pb[p][:]), reads=[Bpb[p]], writes=[Bd])
                    ST = sb("mST", [128, 128], BF16, s2); KW = sb("mKW", [128, 256], BF16, s2)
                    Cst = sb("mCst", [128, 2, 258], F32, s2); Cbf = sb("mCbf", [128, 2, 258], BF16, s2)
                    t2 = sb("mt2", [128, 1], F32, s2); Bt2 = kb.buf()
                    t1 = sb("mt1", [128, 1], F32, s2); HFc = sb("mHFc", [128, 256], F32, s2); HS = sb("mHS", [128, 256], F32, s2)
                    junk = sb("mjunk", [128, 256], BF16, s2); ssq = sb("mssq", [128, 1], F32, s2)
                    HN = sb("mHN", [128, 256], BF16, s2); SG = sb("mSG", [128, 2, 128], F32, s2); YM = sb("mYM", [128, 2, 512], BF16, s2)
                    BST = kb.buf(); BKW = kb.buf(); BCst = kb.buf(); BCbf = kb.buf(); Bt1 = kb.buf(); BHFc = kb.buf(); BHS = kb.buf()
                    Bjk = kb.buf(); Bssq = kb.buf(); BHN = kb.buf(); BSG = kb.buf(); BYM = kb.buf()
                    pS, pO, pK, pP0, pP1, pOT = pb[0], pb[1], pb[2], pb[3], pb[4], pb[6]
                    BpS, BpO, BpK, BpP0, BpP1, BpT, BpOT = Bpb[0], Bpb[1], Bpb[2], Bpb[3], Bpb[4], Bpb[5], Bpb[6]
                    pTv = pTb[:, 0:256].rearrange("p (a b) -> p a b", b=128)
                    for d_ in range(2 if stopM == 0 else (0 if stopM == 3 else 1)):
                        op("dve", lambda e: e.memset(Cst[:], 0.0), writes=[BCst])
                        op("dve", lambda e: e.memset(Cbf[:], 0.0), writes=[BCbf])
                        chunks = list(range(NT)) if d_ == 0 else list(range(NT - 1, -1, -1))
                        if stopM == 4:
                            chunks = chunks[:4]
                        for c in chunks:
                            tsl = slice(c * 128, (c + 1) * 128)
                            for dc in range(2):
                                op("pe", lambda e: e.matmul(pS[:, 0:128], lhsT=KT[:, dc, tsl], rhs=QT[:, dc, tsl], start=(dc == 0), stop=(dc == 1)),
                                   reads=[BKT, BQT], writes=[BpS])
                            op("dve", lambda e: e.scalar_tensor_tensor(out=ST[:], in0=pS[:, 0:128], scalar=SC1[d_][:, c, h:h + 1], in1=mask[d_][:], op0=ALU.mult, op1=ALU.mult),
                               reads=[BpS, BGS, B_const], writes=[BST])
                            for dc in range(2):
                                op("pe", lambda e: e.matmul(pO[:, 0:258], lhsT=QT[:, dc, tsl], rhs=Cbf[:, dc, :], start=(dc == 0), stop=False),
                                   reads=[BQT, BCbf], writes=[BpO])
                            op("pe", lambda e: e.matmul(pO[:, 0:258], lhsT=ST[:], rhs=V[:, c, 0:258], start=False, stop=True), reads=[BST, BV], writes=[BpO])
                            for dc in range(2):
                                op("pe", lambda e: e.matmul(pK[:, 0:256], lhsT=XC[:, dc, tsl], rhs=wk[:, dc, :], start=(dc == 0), stop=(dc == 1)),
                                   reads=[BXC, Bwh], writes=[BpK])
                            op("dve", lambda e: e.tensor_scalar(out=KW[:], in0=pK[:, 0:256], scalar1=KWS[d_][:, c, h:h + 1], scalar2=None, op0=ALU.mult), reads=[BpK, BGS], writes=[BKW])
                            op("pe", lambda e: e.matmul(pP0[:, 0:258], lhsT=KW[:, 0:128], rhs=V[:, c, 0:258], start=True, stop=True), reads=[BKW, BV], writes=[BpP0])
                            op("pe", lambda e: e.matmul(pP1[:, 0:258], lhsT=KW[:, 128:256], rhs=V[:, c, 0:258], start=True, stop=True), reads=[BKW, BV], writes=[BpP1])
                            ebc = EB[d_][:, c, h:h + 1]
                            op("dve", lambda e: e.tensor_scalar(out=t1[:], in0=pO[:, 256:257], scalar1=ebc, scalar2=None, op0=ALU.mult), reads=[BpO, BGS], writes=[Bt1])
                            op("dve", lambda e: e.tensor_scalar(out=t2[:], in0=t1[:], scalar1=-1.0, scalar2=None, op0=ALU.mult), reads=[Bt1], writes=[Bt2])
                            op("dve", lambda e: e.tensor_tensor(out=t1[:], in0=t1[:], in1=t2[:], op=ALU.max), reads=[Bt1, Bt2], writes=[Bt1])
                            op("dve", lambda e: e.tensor_scalar(out=t1[:], in0=t1[:], scalar1=1.0, scalar2=None, op0=ALU.max), reads=[Bt1], writes=[Bt1])
                            op("dve", lambda e: e.reciprocal(out=t1[:], in_=t1[:]), reads=[Bt1], writes=[Bt1])
                            op("dve", lambda e: e.tensor_scalar(out=t1[:], in0=t1[:], scalar1=ebc, scalar2=None, op0=ALU.mult), reads=[Bt1, BGS], writes=[Bt1])
                            if d_ == 0:
                                op("dve", lambda e: e.tensor_scalar(out=HFc[:], in0=pO[:, 0:256], scalar1=t1[:], scalar2=None, op0=ALU.mult), reads=[BpO, Bt1], writes=[BHFc])
                                dma("sp", lambda e: e.dma_start(out=hf_scr[tsl, :], in_=HFc[:]), reads=[BHFc], writes=[B_hf[c]])
                            else:
                                dma("sp", lambda e: e.dma_start(out=HFc[:], in_=hf_scr[tsl, :]), reads=[B_hf[c]], writes=[BHFc])
                                op("dve", lambda e: e.scalar_tensor_tensor(out=HS[:], in0=pO[:, 0:256], scalar=t1[:], in1=HFc[:], op0=ALU.mult, op1=ALU.add),
                                   reads=[BpO, Bt1, BHFc], writes=[BHS])
                                rms_rstd("M", HS[:], 256, ssq[:], junk[:], BHS, Bssq, Bjk)
                                op("dve", lambda e: e.tensor_scalar(out=HN[:], in0=HS[:], scalar1=ssq[:], scalar2=None, op0=ALU.mult), reads=[BHS, Bssq], writes=[BHN])
                                for dc in range(2):
                                    op("pe", lambda e: e.transpose(out=pTv[:, dc, :], in_=HN[:, dc * 128:(dc + 1) * 128], identity=ident_b[:]), reads=[BHN, B_const], writes=[BpT])
                                for dc in range(2):
                                    for c8 in range(8):
                                        op("pe", lambda e: e.matmul(pOT[:, dc * 128:(dc + 1) * 128], lhsT=wo[:, c8, dc * 128:(dc + 1) * 128], rhs=hT[:, c8, tsl], start=(c8 == 0), stop=(c8 == 7)),
                                           reads=[Bwh, B_hT[c // 4]], writes=[BpOT])
                                op("act", lambda e: e.activation(out=SG[:].rearrange("p a b -> p (a b)"), in_=pOT[:, 0:256], func=AF.Sigmoid), reads=[BpOT], writes=[BSG])
                                q4 = c % 4
                                for dc in range(2):
                                    op("dve", lambda e: e.scalar_tensor_tensor(out=YM[:, dc, q4 * 128:(q4 + 1) * 128], in0=pTv[:, dc, :], scalar=vcol["mng"](h * 2 + dc), in1=SG[:, dc, :], op0=ALU.mult, op1=ALU.mult),
                                       reads=[BpT, BSG, B_const], writes=[BYM])
                                if q4 == 0:
                                    g = c // 4
                                    dma("sp", lambda e: e.dma_start(out=ymT[h * 256:(h + 1) * 256, g * 512:(g + 1) * 512].rearrange("(a p) t -> p a t", p=128), in_=YM[:]),
                                        reads=[BYM], writes=[B_ymT])
                            egc = EG[d_][:, c, h:h + 1]
                            op("dve", lambda e: e.scalar_tensor_tensor(out=Cst[:, 0, :], in0=Cst[:, 0, :], scalar=egc, in1=pP0[:, 0:258], op0=ALU.mult, op1=ALU.add),
                               reads=[BCst, BGS, BpP0], writes=[BCst])
                            op("dve", lambda e: e.scalar_tensor_tensor(out=Cst[:, 1, :], in0=Cst[:, 1, :], scalar=egc, in1=pP1[:, 0:258], op0=ALU.mult, op1=ALU.add),
                               reads=[BCst, BGS, BpP1], writes=[BCst])
                            op("pool", lambda e: e.tensor_copy(out=Cbf[:], in_=Cst[:]), reads=[BCst], writes=[BCbf])
                    kb.barrier()
            kb.barrier()
         except _Stop:
            kb.barrier()

        if dbg is not None and dbg[0] == "ym":
            with ExitStack() as cs:
                tb = sb("dbgb", [128, T], BF16, cs); t32 = sb("dbg32", [128, T], F32, cs)
                Bt = kb.buf(); Bt2 = kb.buf()
                for n in range(8):
                    dma("sp", lambda e: e.dma_start(out=tb[:], in_=ymT[n * 128:(n + 1) * 128, :]), reads=[B_ymT], writes=[Bt])
                    op("dve", lambda e: e.tensor_copy(out=t32[:], in_=tb[:]), reads=[Bt], writes=[Bt2])
                    dma("sp", lambda e: e.dma_start(out=dbg_out[n * 128:(n + 1) * 128, :], in_=t32[:]), reads=[Bt2])
                kb.barrier()


        B_mg = kb.buf("mgT")
        MP0 = 4112
        if dbg is None or dbg[0] in ("full", "x1", "idx"):
          with ExitStack() as cs:
            wbm_a = sb("wbm_a", [128, 8, D], BF16, cs); wbl_a = sb("wbl_a", [128, 8, D], BF16, cs)
            wgm_a = sb("wgm_a", [128, 8, D], BF16, cs); wgl_a = sb("wgl_a", [128, 8, D], BF16, cs)
            YMt = [sb("YMt%d" % i, [128, 8, 512], BF16, cs) for i in range(2)]; YLt = [sb("YLt%d" % i, [128, 8, 512], BF16, cs) for i in range(2)]
            GMs = [sb("GMs%d" % i, [128, 512], F32, cs) for i in range(2)]; GLs = [sb("GLs%d" % i, [128, 512], F32, cs) for i in range(2)]
            TM = [sb("TM%d" % i, [128, 512], F32, cs) for i in range(2)]
            MGe = [sb("MGe%d" % i, [128, 8, 512], BF16, cs) for i in range(2)]
            pE = [ps("pE%d" % i, [128, 512], F32, cs) for i in range(8)]
            Bwe = kb.buf(); BYM_ = kb.bufs(2); BYL_ = kb.bufs(2); BGM = kb.bufs(2); BGL = kb.bufs(2); BTM = kb.bufs(2); BMGe = kb.bufs(2)
            BpE = kb.bufs(8, "pE", excl=True)
            for c in range(8):
                rs = slice(c * 128, (c + 1) * 128)
                load_w_bf16(wbm_a[:, c, :], w_bm[rs, :], [Bwe])
                load_w_bf16(wbl_a[:, c, :], w_bl[rs, :], [Bwe])
                load_w_bf16(wgm_a[:, c, :], w_in[rs, MP0:MP0 + 1024], [Bwe])
                load_w_bf16(wgl_a[:, c, :], w_in[rs, MP0 + 1024:MP0 + 2048], [Bwe])
            it = 0
            for g in range(NG):
                gs = slice(g * 512, (g + 1) * 512)
                sg_ = g % 2
                dma("sp", lambda e: e.dma_start(out=YMt[sg_][:], in_=ymT[:, gs].rearrange("(c p) t -> p c t", p=128)), reads=[B_ymT], writes=[BYM_[sg_]])
                dma("sp", lambda e: e.dma_start(out=YLt[sg_][:], in_=ylT[:, gs].rearrange("(c p) t -> p c t", p=128)), reads=[B_ylT], writes=[BYL_[sg_]])
                for e_ in range(8):
                    es_ = slice(e_ * 128, (e_ + 1) * 128)
                    pz = (it % 2) * 4; tz = it % 2; it += 1
                    for c in range(8):
                        op("pe", lambda e: e.matmul(pE[pz + 0][:], lhsT=wbm_a[:, c, es_], rhs=YMt[sg_][:, c, :], start=(c == 0), stop=(c == 7)), reads=[Bwe, BYM_[sg_]], writes=[BpE[pz + 0]])
                    for c in range(8):
                        op("pe", lambda e: e.matmul(pE[pz + 1][:], lhsT=wbl_a[:, c, es_], rhs=YLt[sg_][:, c, :], start=(c == 0), stop=(c == 7)), reads=[Bwe, BYL_[sg_]], writes=[BpE[pz + 1]])
                    for c in range(8):
                        op("pe", lambda e: e.matmul(pE[pz + 2][:], lhsT=wgm_a[:, c, es_], rhs=hT[:, c, gs], start=(c == 0), stop=(c == 7)), reads=[Bwe, B_hT[g]], writes=[BpE[pz + 2]])
                    for c in range(8):
                        op("pe", lambda e: e.matmul(pE[pz + 3][:], lhsT=wgl_a[:, c, es_], rhs=hT[:, c, gs], start=(c == 0), stop=(c == 7)), reads=[Bwe, B_hT[g]], writes=[BpE[pz + 3]])
                    op("act", lambda e: e.activation(out=GMs[tz][:], in_=pE[pz + 2][:], func=AF.Sigmoid), reads=[BpE[pz + 2]], writes=[BGM[tz]])
                    op("act", lambda e: e.activation(out=GLs[tz][:], in_=pE[pz + 3][:], func=AF.Sigmoid), reads=[BpE[pz + 3]], writes=[BGL[tz]])
                    op("dve", lambda e: e.tensor_tensor(out=TM[tz][:], in0=GMs[tz][:], in1=pE[pz + 0][:], op=ALU.mult), reads=[BGM[tz], BpE[pz + 0]], writes=[BTM[tz]])
                    op("dve", lambda e: e.tensor_tensor(out=GLs[tz][:], in0=GLs[tz][:], in1=pE[pz + 1][:], op=ALU.mult), reads=[BGL[tz], BpE[pz + 1]], writes=[BGL[tz]])
                    op("pool", lambda e: e.tensor_tensor(out=MGe[sg_][:, e_, :], in0=TM[tz][:], in1=GLs[tz][:], op=ALU.add), reads=[BTM[tz], BGL[tz]], writes=[BMGe[sg_]])
                dma("sp", lambda e: e.dma_start(out=mgT[:, gs].rearrange("(c p) t -> p c t", p=128), in_=MGe[sg_][:]), reads=[BMGe[sg_]], writes=[B_mg])
            kb.barrier()
        hT_scope.close()
        B_uv = kb.buf("uvtab")
        if need_peer:
          with ExitStack() as us:
            UVt = [sb("UVt%d" % i, [128, 4, 2 * D], BF16, us) for i in range(2)]
            BUVu = kb.bufs(2); BUVv = kb.bufs(2)
            for blk in range(32):
                s_ = blk % 2
                rws = slice(blk * 512, (blk + 1) * 512)
                dma("pool", lambda e: e.dma_start(out=UVt[s_][:, :, 0:D], in_=p_u[rws, :].rearrange("(p a) d -> p a d", a=4)), writes=[BUVu[s_]])
                dma("pool", lambda e: e.dma_start(out=UVt[s_][:, :, D:2 * D], in_=p_v[rws, :].rearrange("(p a) d -> p a d", a=4)), writes=[BUVv[s_]])
                dma("sp", lambda e: e.dma_start(out=uv_tab[rws, :].rearrange("(p a) d -> p a d", a=4), in_=UVt[s_][:]), reads=[BUVu[s_], BUVv[s_]], writes=[B_uv])
            kb.barrier()

        if dbg is None or dbg[0] in ("full", "x1", "idx"):
          with ExitStack() as cs:
            do_peer = dbg is None or dbg[0] in ("full", "idx")
            g2_bc = sb("g2_bc", [128, D], F32, cs); gf_bc = sb("gf_bc", [128, D], F32, cs)
            dma("sp", lambda e: e.dma_start(out=g2_bc[:], in_=norm2_g.partition_broadcast(128)), writes=[B_const])
            dma("sp", lambda e: e.dma_start(out=gf_bc[:], in_=fin_g.partition_broadcast(128)), writes=[B_const])
            woutb = sb("woutb", [128, 8, D], BF16, cs)
            Bwo = kb.buf()
            for c in range(8):
                load_w_bf16(woutb[:, c, :], w_out[c * 128:(c + 1) * 128, :], [Bwo])
            pF = [ps("pF%d" % i, [128, 512], F32, cs) if i != 2 else ps("pFb", [128, 1024], BF16, cs) for i in range(8)]
            BpF = kb.bufs(8, "pF", excl=True)
            if do_peer:
                wpq = sb("wpq", [128, 8, 2048], BF16, cs)
                KEYT = sb("KEYT", [128, 16, 128], BF16, cs)
                Bwp = kb.buf()
                for c in range(8):
                    load_w_bf16(wpq[:, c, 0:1024], p_wq[c * 128:(c + 1) * 128, 0:1024], [Bwp])
                    load_w_bf16(wpq[:, c, 1024:2048], p_wq[c * 128:(c + 1) * 128, 1024:2048], [Bwp])
                with ExitStack() as ks:
                    KEYB = sb("KEYB", [128, 16, 128], BF16, ks)
                    Bkf = kb.buf()
                    for hp in range(16):
                        load_w_bf16(KEYB[:, hp, :], p_keys[hp], [Bkf])
                    for q in range(2):
                        for h8 in range(8):
                            hp = q * 8 + h8
                            op("pe", lambda e: e.transpose(out=pF[2][:, h8 * 128:(h8 + 1) * 128], in_=KEYB[:, hp, :], identity=ident_b[:]),
                               reads=[Bkf, B_const], writes=[BpF[2]])
                        op("act", lambda e: e.copy(out=KEYT[:, q * 8:(q + 1) * 8, :].rearrange("p a b -> p (a b)"), in_=pF[2][:]), reads=[BpF[2]], writes=[Bwp])
                    kb.barrier()
            MGg = sb("MGg", [128, 8, 512], BF16, cs); BMGg = kb.buf()
            xin = sb("xinE", [128, D], F32, cs); Bxin = kb.buf()
            x1t = sb("x1t", [128, D], F32, cs); Bx1 = kb.buf()
            ss2 = sb("ss2", [128, 1], F32, cs); Bss2 = kb.buf()
            junkF = sb("junkF", [128, D], BF16, cs); BjF = kb.buf()
            yt = sb("yt", [128, D], F32, cs); Byt = kb.buf()
            if do_peer:
                h2 = sb("h2", [128, D], F32, cs); Bh2 = kb.buf()
                h2b = sb("h2b", [128, D], BF16, cs); Bh2b = kb.buf()
                h2T = sb("h2T", [128, 8, 128], BF16, cs); Bh2T = kb.buf()
                QP = sb("QP", [128, 16, 128], BF16, cs); BQP = kb.buf()
                WK = sb("WK", [128, 16, 128], F32, cs); BWK = kb.buf()
                SCS = sb("SCS", [128, 16, 128], F32, cs); BSCS = kb.buf()
                v8 = sb("v8", [128, 16, 16], F32, cs); Bv8 = kb.buf()
                Bv8a = kb.bufs(16); Bv8b = kb.bufs(16); Bi8a = kb.bufs(16); Bi8b = kb.bufs(16); BWKa = kb.bufs(16)
                Bb8a = kb.bufs(8); Bb8b = kb.bufs(8); Bp8a = kb.bufs(8); Bp8b = kb.bufs(8); BCWa = kb.bufs(8)
                i8 = sb("i8", [128, 16, 16], U32, cs); Bi8 = kb.buf()
                i8f = sb("i8f", [128, 16, 16], F32, cs); Bi8f = kb.buf()
                CAND = sb("CAND", [128, 8, 256], F32, cs); BCAND = kb.buf()
                CW = sb("CW", [128, 8, 256], F32, cs); BCW = kb.buf()
                b8 = sb("b8", [128, 8, 16], F32, cs); Bb8 = kb.buf()
                p8 = sb("p8", [128, 8, 16], U32, cs); Bp8 = kb.buf()
                pff = sb("pff", [128, 8, 16], F32, cs)
                phf = sb("phf", [128, 8, 16], F32, cs); plf = sb("plf", [128, 8, 16], F32, cs); Bph = kb.buf()
                EQ = sb("EQ", [128, 128, 16], F32, cs); BEQ = kb.buf()
                ID0 = sb("ID0", [128, 128], F32, cs); ID1 = sb("ID1", [128, 128], F32, cs); BID = kb.buf()
                IDXu = sb("IDXu", [128, 128], U32, cs); BIDX = kb.buf()
                NB = sb("NB", [128, 8], F32, cs); Zs = sb("Zs", [128, 8], F32, cs); EX = sb("EX", [128, 8, 16], F32, cs); BSM = kb.buf()
                GATE = sb("GATE", [128, 128], F32, cs); BGATE = kb.buf()
                ACTV = sb("ACTV", [128, 128], F32, cs); BACTV = kb.buf()
                WT = sb("WT", [128, 128], F32, cs); BWT = kb.buf()
                NGB = 14
                GB = [sb("GB%d" % i, [128, 2 * D], BF16, cs) for i in range(NGB)]; BGB = kb.bufs(NGB)
                DG = [sb("DG%d" % i, [128, 128], BF16, cs) for i in range(8)]; BDG = kb.bufs(8)
                G1 = sb("G1", [128, 128], F32, cs)
                BAk = kb.bufs(64); BGk = kb.bufs(64)
                dgi = 0
                junkU = sb("junkU", [128, D], BF16, cs); BjU = kb.buf()
                gbi = 0
                pT2 = pF[2]; BpT2 = BpF[2]
                pT2v = pT2[:].rearrange("p (a b) -> p a b", b=128)
            ng_lim = dbg[3] if (dbg is not None and len(dbg) > 3) else NG
            for g in range(ng_lim):
                gs = slice(g * 512, (g + 1) * 512)
                dma("sp", lambda e: e.dma_start(out=MGg[:], in_=mgT[:, gs].rearrange("(c p) t -> p c t", p=128)), reads=[B_mg], writes=[BMGg])
                for tt in range(4):
                    i = g * 4 + tt
                    rows = slice(i * 128, (i + 1) * 128)
                    dma("sp", lambda e: e.dma_start(out=xin[:], in_=x[rows, :]), writes=[Bxin])
                    for half in range(2):
                        hsl = slice(half * 512, (half + 1) * 512)
                        for e_ in range(8):
                            op("pe", lambda e: e.matmul(pF[half][:], lhsT=MGg[:, e_, tt * 128:(tt + 1) * 128], rhs=woutb[:, e_, hsl], start=(e_ == 0), stop=(e_ == 7)),
                               reads=[BMGg, Bwo], writes=[BpF[half]])
                        op("dve", lambda e: e.tensor_tensor(out=x1t[:, hsl], in0=pF[half][:], in1=xin[:, hsl], op=ALU.add), reads=[BpF[half], Bxin], writes=[Bx1])
                    if dbg is not None and dbg[0] == "x1":
                        dma("sp", lambda e: e.dma_start(out=dbg_out[rows, :], in_=x1t[:]), reads=[Bx1])
                        continue
                    rms_rstd("P", x1t[:], D, ss2[:], junkF[:], Bx1, Bss2, BjF)
                    op("dve", lambda e: e.scalar_tensor_tensor(out=h2[:], in0=x1t[:], scalar=ss2[:], in1=g2_bc[:], op0=ALU.mult, op1=ALU.mult),
                       reads=[Bx1, Bss2, B_const], writes=[Bh2])
                    op("pool", lambda e: e.tensor_copy(out=h2b[:], in_=h2[:]), reads=[Bh2], writes=[Bh2b])
                    for c in range(8):
                        op("pe", lambda e: e.transpose(out=pT2v[:, c, :], in_=h2b[:, c * 128:(c + 1) * 128], identity=ident_b[:]), reads=[Bh2b, B_const], writes=[BpT2])
                    op("act", lambda e: e.copy(out=h2T[:], in_=pT2v), reads=[BpT2], writes=[Bh2T])
                    for hp in range(16):
                        bk = 3 + hp // 4
                        for c in range(8):
                            op("pe", lambda e: e.matmul(pF[bk][:, (hp % 4) * 128:(hp % 4 + 1) * 128], lhsT=wpq[:, c, hp * 128:(hp + 1) * 128], rhs=h2T[:, c, :], start=(c == 0), stop=(c == 7)),
                               reads=[Bwp, Bh2T], writes=[BpF[bk]])
                    for q in range(4):
                        op("act", lambda e: e.copy(out=QP[:, q * 4:(q + 1) * 4, :].rearrange("p a b -> p (a b)"), in_=pF[3 + q][:]), reads=[BpF[3 + q]], writes=[BQP])
                    for hp in range(16):
                        bk = 3 + hp // 4
                        op("pe", lambda e: e.matmul(pF[bk][:, (hp % 4) * 128:(hp % 4 + 1) * 128], lhsT=QP[:, hp, :], rhs=KEYT[:, hp, :], start=True, stop=True),
                           reads=[BQP, Bwp], writes=[BpF[bk]])
                    for q in range(4):
                        op("act", lambda e: e.copy(out=SCS[:, q * 4:(q + 1) * 4, :].rearrange("p a b -> p (a b)"), in_=pF[3 + q][:]), reads=[BpF[3 + q]], writes=[BSCS])
                    for hp in range(16):
                        op("dve", lambda e: e.max(out=v8[:, hp, 0:8], in_=SCS[:, hp, :]), reads=[BSCS], writes=[Bv8a[hp]])
                    for hp in range(16):
                        op("dve", lambda e: e.max_index(out=i8[:, hp, 0:8], in_max=v8[:, hp, 0:8], in_values=SCS[:, hp, :]), reads=[BSCS, Bv8a[hp]], writes=[Bi8a[hp]])
                    for hp in range(16):
                        op("dve", lambda e: e.match_replace(out=WK[:, hp, :], in_to_replace=v8[:, hp, 0:8], in_values=SCS[:, hp, :], imm_value=-1e30), reads=[BSCS, Bv8a[hp]], writes=[BWKa[hp]])
                    for hp in range(16):
                        op("dve", lambda e: e.max(out=v8[:, hp, 8:16], in_=WK[:, hp, :]), reads=[BWKa[hp]], writes=[Bv8b[hp]])
                    for hp in range(16):
                        op("dve", lambda e: e.max_index(out=i8[:, hp, 8:16], in_max=v8[:, hp, 8:16], in_values=WK[:, hp, :]), reads=[BWKa[hp], Bv8b[hp]], writes=[Bi8b[hp]])
                    op("dve", lambda e: e.tensor_copy(out=i8f[:], in_=i8[:]), reads=Bi8a + Bi8b, writes=[Bi8f])
                    v8v = v8[:].rearrange("p (h two) k -> p h two k", two=2)
                    i8v = i8f[:].rearrange("p (h two) k -> p h two k", two=2)
                    s0b = v8v[:, :, 0, :].unsqueeze(3).to_broadcast([128, 8, 16, 16])
                    s1b = v8v[:, :, 1, :].unsqueeze(2).to_broadcast([128, 8, 16, 16])
                    op("dve", lambda e: e.tensor_tensor(out=CAND[:].rearrange("p h (i j) -> p h i j", j=16), in0=s0b, in1=s1b, op=ALU.add), reads=Bv8a + Bv8b, writes=[BCAND])
                    for h in range(8):
                        op("dve", lambda e: e.max(out=b8[:, h, 0:8], in_=CAND[:, h, :]), reads=[BCAND], writes=[Bb8a[h]])
                    for h in range(8):
                        op("dve", lambda e: e.max_index(out=p8[:, h, 0:8], in_max=b8[:, h, 0:8], in_values=CAND[:, h, :]), reads=[BCAND, Bb8a[h]], writes=[Bp8a[h]])
                    for h in range(8):
                        op("dve", lambda e: e.match_replace(out=CW[:, h, :], in_to_replace=b8[:, h, 0:8], in_values=CAND[:, h, :], imm_value=-1e30), reads=[BCAND, Bb8a[h]], writes=[BCWa[h]])
                    for h in range(8):
                        op("dve", lambda e: e.max(out=b8[:, h, 8:16], in_=CW[:, h, :]), reads=[BCWa[h]], writes=[Bb8b[h]])
                    for h in range(8):
                        op("dve", lambda e: e.max_index(out=p8[:, h, 8:16], in_max=b8[:, h, 8:16], in_values=CW[:, h, :]), reads=[BCWa[h], Bb8b[h]], writes=[Bp8b[h]])
                    op("dve", lambda e: e.tensor_copy(out=pff[:], in_=p8[:]), reads=Bp8a + Bp8b, writes=[Bph])
                    pffb = pff[:].rearrange("p h k -> p (h k)").unsqueeze(2).to_broadcast([128, 128, 16])
                    io16m = iota16m[:].unsqueeze(1).to_broadcast([128, 128, 16])
                    op("dve", lambda e: e.tensor_tensor(out=EQ[:], in0=pffb, in1=io16m, op=ALU.is_ge), reads=[Bph, B_const], writes=[BEQ])
                    op("dve", lambda e: e.tensor_reduce(out=phf[:].rearrange("p h k -> p (h k)"), in_=EQ[:], axis=mybir.AxisListType.X, op=ALU.add), reads=[BEQ], writes=[Bph])
                    op("dve", lambda e: e.tensor_scalar(out=phf[:], in0=phf[:], scalar1=-1.0, scalar2=None, op0=ALU.add), reads=[Bph], writes=[Bph])
                    op("dve", lambda e: e.scalar_tensor_tensor(out=plf[:], in0=phf[:], scalar=-16.0, in1=pff[:], op0=ALU.mult, op1=ALU.add), reads=[Bph], writes=[Bph])
                    iob = iota16[:].unsqueeze(1).to_broadcast([128, 128, 16])
                    for (pf_, two, IDd) in ((phf, 0, ID0), (plf, 1, ID1)):
                        pfb = pf_[:].rearrange("p h k -> p (h k)").unsqueeze(2).to_broadcast([128, 128, 16])
                        op("dve", lambda e: e.tensor_tensor(out=EQ[:], in0=pfb, in1=iob, op=ALU.is_equal), reads=[Bph, B_const], writes=[BEQ])
                        tabb = i8v[:, :, two, :].unsqueeze(2).to_broadcast([128, 8, 16, 16])
                        op("dve", lambda e: e.tensor_tensor(out=EQ[:].rearrange("p (h k) i -> p h k i", k=16), in0=EQ[:].rearrange("p (h k) i -> p h k i", k=16), in1=tabb, op=ALU.mult),
                           reads=[BEQ, Bi8f], writes=[BEQ])
                        op("dve", lambda e: e.tensor_reduce(out=IDd[:], in_=EQ[:], axis=mybir.AxisListType.X, op=ALU.add), reads=[BEQ], writes=[BID])
                    op("dve", lambda e: e.scalar_tensor_tensor(out=ID0[:], in0=ID0[:], scalar=128.0, in1=ID1[:], op0=ALU.mult, op1=ALU.add), reads=[BID], writes=[BID])
                    op("dve", lambda e: e.tensor_scalar(out=IDXu[:], in0=ID0[:], scalar1=16383.0, scalar2=0.0, op0=ALU.min, op1=ALU.max), reads=[BID], writes=[BIDX])
                    op("dve", lambda e: e.tensor_scalar(out=NB[:], in0=b8[:, :, 0], scalar1=-1.0, scalar2=None, op0=ALU.mult), reads=Bb8a + Bb8b, writes=[BSM])
                    for h in range(8):
                        op("act", lambda e: e.activation(out=EX[:, h, :], in_=b8[:, h, :], func=AF.Exp, bias=NB[:, h:h + 1], scale=1.0, accum_out=Zs[:, h:h + 1]),
                           reads=Bb8a + Bb8b + [BSM], writes=[BSM])
                    op("dve", lambda e: e.reciprocal(out=Zs[:], in_=Zs[:]), reads=[BSM], writes=[BSM])
                    op("dve", lambda e: e.tensor_tensor(out=GATE[:].rearrange("p (h k) -> p h k", k=16), in0=EX[:], in1=Zs[:].unsqueeze(2).to_broadcast([128, 8, 16]), op=ALU.mult),
                       reads=[BSM], writes=[BGATE])
                    if dbg is not None and dbg[0] == "idx":
                        op("dve", lambda e: e.tensor_copy(out=yt[:, 0:128], in_=ID0[:]), reads=[BID], writes=[Byt])
                        op("dve", lambda e: e.tensor_copy(out=yt[:, 128:256], in_=GATE[:]), reads=[BGATE], writes=[Byt])
                        dma("sp", lambda e: e.dma_start(out=dbg_out[rows, :], in_=yt[:, 0:256]), reads=[Byt])
                        continue
                    for kb4 in range(64):
                        sl4 = []
                        for k in range(kb4 * 2, kb4 * 2 + 2):
                            sgb = gbi % NGB; gbi += 1
                            sl4.append(sgb)
                            dma("pool", lambda e: e.indirect_dma_start(out=GB[sgb][:], out_offset=None, in_=uv_tab[:, :],
                                                                       in_offset=bass.IndirectOffsetOnAxis(ap=IDXu[:, k:k + 1], axis=0)),
                                reads=[BIDX, B_uv], writes=[BGB[sgb]])
                            op("dve", lambda e: e.scalar_tensor_tensor(out=junkU[:], in0=GB[sgb][:, 0:D], scalar=1.0, in1=h2b[:], op0=ALU.mult, op1=ALU.mult, accum_out=ACTV[:, k:k + 1]),
                               reads=[BGB[sgb], Bh2b], writes=[BjU, BAk[kb4]])
                        k4 = slice(kb4 * 2, kb4 * 2 + 2)
                        op("act", lambda e: e.activation(out=G1[:, k4], in_=ACTV[:, k4], func=AF.Gelu), reads=[BAk[kb4]], writes=[BGk[kb4]])
                        op("dve", lambda e: e.tensor_tensor(out=WT[:, k4], in0=G1[:, k4], in1=GATE[:, k4], op=ALU.mult), reads=[BGk[kb4], BGATE], writes=[BGk[kb4]])
                        for j4, k in enumerate(range(kb4 * 2, kb4 * 2 + 2)):
                            sgb = sl4[j4]
                            sd = dgi % 8; dgi += 1
                            op("dve", lambda e: e.tensor_scalar(out=DG[sd][:], in0=ident_b[:], scalar1=WT[:, k:k + 1], scalar2=None, op0=ALU.mult), reads=[BGk[kb4], B_const], writes=[BDG[sd]])
                            op("pe", lambda e: e.matmul(pF[0][:], lhsT=DG[sd][:], rhs=GB[sgb][:, D:D + 512], start=(k == 0), stop=(k == 127)), reads=[BDG[sd], BGB[sgb]], writes=[BpF[0]])
                            op("pe", lambda e: e.matmul(pF[1][:], lhsT=DG[sd][:], rhs=GB[sgb][:, D + 512:2 * D], start=(k == 0), stop=(k == 127)), reads=[BDG[sd], BGB[sgb]], writes=[BpF[1]])
                    op("dve", lambda e: e.tensor_tensor(out=x1t[:, 0:512], in0=x1t[:, 0:512], in1=pF[0][:], op=ALU.add), reads=[Bx1, BpF[0]], writes=[Bx1])
                    op("dve", lambda e: e.tensor_tensor(out=x1t[:, 512:D], in0=x1t[:, 512:D], in1=pF[1][:], op=ALU.add), reads=[Bx1, BpF[1]], writes=[Bx1])
                    rms_rstd("F", x1t[:], D, ss2[:], junkF[:], Bx1, Bss2, BjF)
                    op("dve", lambda e: e.scalar_tensor_tensor(out=yt[:], in0=x1t[:], scalar=ss2[:], in1=gf_bc[:], op0=ALU.mult, op1=ALU.mult),
                       reads=[Bx1, Bss2, B_const], writes=[Byt])
                    dma("sp", lambda e: e.dma_start(out=(dbg_out if dbg is not None else y_out)[rows, :], in_=yt[:]), reads=[Byt])
            kb.barrier()

        kb.finish()
    return nc, in_names


_CONSTS = None


def _consts():
    global _CONSTS
    if _CONSTS is None:
        j = np.arange(128)
        _CONSTS = {
            "c_ident": np.eye(128, dtype=np.float32),
            "c_ut": (j[:, None] <= j[None, :]).astype(np.float32),
            "c_lt": (j[:, None] >= j[None, :]).astype(np.float32),
            "c_iota": np.tile(np.concatenate([np.arange(16), np.arange(16) * 16, np.tile([1.0, 0.0], NT)]).astype(np.float32)[None, :], (128, 1)),
        }
    return _CONSTS


def make_in_maps(inputs):
    f = lambda a: np.ascontiguousarray(np.asarray(a, dtype=np.float32))
    shared = {
        "norm1_g": f(inputs["norm1_g"][0]), "w_in": f(inputs["w_in"][0]), "b_gates": f(inputs["b_gates"][0]),
        "mlstm_conv_w": f(inputs["mlstm_conv_w"][0]), "mlstm_conv_b": f(inputs["mlstm_conv_b"][0]),
        "mlstm_w_q": f(inputs["mlstm_w_q"][0]), "mlstm_w_k": f(inputs["mlstm_w_k"][0]), "mlstm_w_v": f(inputs["mlstm_w_v"][0]),
        "mlstm_norm_g": f(inputs["mlstm_norm_g"][0]), "lru_conv_w": f(inputs["lru_conv_w"][0]), "lru_conv_b": f(inputs["lru_conv_b"][0]),
        "lru_w_r": f(inputs["lru_w_r"][0]), "lru_b_r": f(inputs["lru_b_r"][0]), "lru_w_i": f(inputs["lru_w_i"][0]),
        "lru_b_i": f(inputs["lru_b_i"][0]), "lru_lambda": f(inputs["lru_lambda"][0]),
        "w_branch_mlstm": f(inputs["w_branch_mlstm"][0]), "w_branch_lru": f(inputs["w_branch_lru"][0]), "w_out": f(inputs["w_out"][0]),
        "norm2_g": f(inputs["norm2_g"][0]), "peer_w_q": f(inputs["peer_w_q"][0]),
        "peer_sub_keys": f(inputs["peer_sub_keys"][0]).reshape(16, 128, 128),
        "peer_u": f(inputs["peer_u"][0]), "peer_v": f(inputs["peer_v"][0]), "final_norm_g": f(inputs["final_norm_g"]),
    }
    shared.update(_consts())
    xs = f(inputs["x"])
    return [dict(shared, x=xs[b]) for b in range(8)]


def kernel(**inputs):
    nc, names = build_program()
    in_maps = [{k: m[k] for k in names} for m in make_in_maps(inputs)]
    res = run_bass_kernel_spmd(nc, in_maps, core_ids=list(range(8)))
    return np.stack([np.asarray(r["y"], dtype=np.float32) for r in res.results], axis=0)
```

```python
import numpy as np
from contextlib import ExitStack
import concourse.bass as bass
import concourse.mybir as mybir
from concourse.bass_utils import run_bass_kernel_spmd

F32 = mybir.dt.float32
BF16 = mybir.dt.bfloat16
U32 = mybir.dt.uint32
I32 = mybir.dt.int32
AF = mybir.ActivationFunctionType
ALU = mybir.AluOpType

T = 4096
D = 1024
NT = T // 128
NG = T // 512
DIN = 6160
LN16 = float(np.log(16.0))
EPS = 1e-6


class Buf:
    __slots__ = ("name", "w", "r", "excl")

    def __init__(self, name, excl=False):
        self.name = name
        self.w = None
        self.r = {}
        self.excl = excl


class _Stop(Exception):
    pass


class KB:
    def __init__(self, nc, es, n_dma_sems=32):
        self.nc = nc
        self.es = es
        self.engs = {"pe": nc.tensor, "act": nc.scalar, "dve": nc.vector, "pool": nc.gpsimd, "sp": nc.sync}
        self.sem = {}
        self.cnt = {}
        self.semobj = {}
        for e in self.engs:
            s = es.enter_context(nc.semaphore("sem_" + e))
            self.semobj["E" + e] = s
            self.cnt["E" + e] = 0
        self.dma_keys = {"sp": [], "pool": []}
        for q, n in (("sp", n_dma_sems), ("pool", n_dma_sems)):
            for i in range(n):
                s = es.enter_context(nc.semaphore("sem_dma_%s%d" % (q, i)))
                k = "D%s%d" % (q, i)
                self.semobj[k] = s
                self.cnt[k] = 0
                self.dma_keys[q].append(k)
        self.dma_rr = {"sp": 0, "pool": 0}
        self.waited = {e: {} for e in self.engs}
        self.nbuf = 0
        self.all_dma_events = {}

    def buf(self, name=None, excl=False):
        self.nbuf += 1
        return Buf(name or "b%d" % self.nbuf, excl)

    def bufs(self, n, name="b", excl=False):
        return [self.buf("%s%d" % (name, i), excl) for i in range(n)]

    def _wait(self, eng, deps):
        wd = self.waited[eng]
        best = {}
        for (k, v) in deps:
            if wd.get(k, 0) >= v:
                continue
            if eng == "pe" and k == "Epe":
                continue
            if best.get(k, 0) < v:
                best[k] = v
        for k, v in best.items():
            self.engs[eng].wait_ge(self.semobj[k], v)
            wd[k] = v

    def _deps(self, reads, writes):
        deps = []
        for b in reads:
            if b.w is not None:
                deps.append(b.w)
            if b.excl:
                deps.extend(b.r.items())
        for b in writes:
            if b.w is not None:
                deps.append(b.w)
            deps.extend(b.r.items())
        return deps

    def _record(self, ev, reads, writes):
        k, v = ev
        for b in reads:
            if b.r.get(k, 0) < v:
                b.r[k] = v
        for b in writes:
            b.w = ev
            b.r = {}

    def op(self, eng, fn, reads=(), writes=()):
        self._wait(eng, self._deps(reads, writes))
        inst = fn(self.engs[eng])
        k = "E" + eng
        self.cnt[k] += 1
        inst.then_inc(self.semobj[k], 1)
        self._record((k, self.cnt[k]), reads, writes)

    def dma(self, eng, fn, reads=(), writes=()):
        k = self.dma_keys[eng][self.dma_rr[eng]]
        self.dma_rr[eng] = (self.dma_rr[eng] + 1) % len(self.dma_keys[eng])
        deps = self._deps(reads, writes)
        if self.cnt[k] > 0:
            deps.append((k, self.cnt[k]))
        self._wait(eng, deps)
        inst = fn(self.engs[eng])
        self.cnt[k] += 16
        inst.then_inc(self.semobj[k], 16)
        ev = (k, self.cnt[k])
        self._record(ev, reads, writes)
        self.all_dma_events[k] = self.cnt[k]
        return ev

    def barrier(self):
        deps = [(k, v) for k, v in self.cnt.items() if v > 0]
        for e in self.engs:
            self._wait(e, deps)

    def finish(self):
        deps = [(k, v) for k, v in self.cnt.items() if v > 0]
        self._wait("sp", deps)


def build_program(dbg=None):
    nc = bass.Bass("TRN2", target_bir_lowering=False)

    in_names = []
    need_peer = dbg is None or dbg[0] == "full"

    def din(name, shape, dt=F32):
        if name in ("peer_u", "peer_v") and not need_peer:
            return None
        in_names.append(name)
        return nc.dram_tensor(name, list(shape), dt, kind="ExternalInput").ap()

    x = din("x", [T, D])
    norm1_g = din("norm1_g", [D])
    w_in = din("w_in", [D, DIN])
    b_gates = din("b_gates", [16])
    m_conv_w = din("mlstm_conv_w", [4, D])
    m_conv_b = din("mlstm_conv_b", [D])
    m_wq = din("mlstm_w_q", [4, 256, 256])
    m_wk = din("mlstm_w_k", [4, 256, 256])
    m_wv = din("mlstm_w_v", [4, 256, 256])
    m_norm_g = din("mlstm_norm_g", [D])
    l_conv_w = din("lru_conv_w", [4, D])
    l_conv_b = din("lru_conv_b", [D])
    l_wr = din("lru_w_r", [2, 8, 128, 128])
    l_br = din("lru_b_r", [2, D])
    l_wi = din("lru_w_i", [2, 8, 128, 128])
    l_bi = din("lru_b_i", [2, D])
    l_lam = din("lru_lambda", [2, D])
    w_bm = din("w_branch_mlstm", [D, D])
    w_bl = din("w_branch_lru", [D, D])
    w_out = din("w_out", [D, D])
    norm2_g = din("norm2_g", [D])
    p_wq = din("peer_w_q", [D, 2048])
    p_keys = din("peer_sub_keys", [16, 128, 128])
    p_u = din("peer_u", [16384, D])
    p_v = din("peer_v", [16384, D])
    fin_g = din("final_norm_g", [D])
    c_ident = din("c_ident", [128, 128])
    c_ut = din("c_ut", [128, 128])
    c_lt = din("c_lt", [128, 128])
    c_iota = din("c_iota", [128, 32 + 2 * NT])

    y_out = nc.dram_tensor("y", [T, D], F32, kind="ExternalOutput").ap()
    ymT = nc.dram_tensor("scr_ymT", [D, T], BF16, kind="Internal").ap()
    ylT = nc.dram_tensor("scr_ylT", [D, T], BF16, kind="Internal").ap()
    hf_scr = nc.dram_tensor("scr_hf", [T, 256], F32, kind="Internal").ap()
    mgT = nc.dram_tensor("scr_mgT", [D, T], BF16, kind="Internal").ap()
    uv_tab = nc.dram_tensor("scr_uv", [16384, 2 * D], BF16, kind="Internal").ap()
    dbg_out = None
    if dbg is not None:
        dbg_out = nc.dram_tensor("dbg", list(dbg[1]), F32, kind="ExternalOutput").ap()

    es = ExitStack()
    with es:
        E = es.enter_context
        kb = KB(nc, es)
        op, dma = kb.op, kb.dma

        uid = [0]

        def sb(name, shape, dt=F32, ctx=None):
            uid[0] += 1
            return (ctx or es).enter_context(nc.sbuf_tensor("%s_%d" % (name, uid[0]), list(shape), dt))

        def ps(name, shape, dt=F32, ctx=None):
            uid[0] += 1
            return (ctx or es).enter_context(nc.psum_tensor("%s_%d" % (name, uid[0]), list(shape), dt))

        ident_f = sb("ident_f", [128, 128]); ident_b = sb("ident_b", [128, 128], BF16)
        ut_f = sb("ut_f", [128, 128]); lt_f = sb("lt_f", [128, 128])
        ones_f = sb("ones_f", [128, 128])
        iota16 = sb("iota16", [128, 16]); iota16m = sb("iota16m", [128, 16])
        onezero = sb("onezero", [128, NT, 2])
        B_const = kb.buf("const")
        dma("sp", lambda e: e.dma_start(out=ident_f[:], in_=c_ident[:, :]), writes=[B_const])
        dma("sp", lambda e: e.dma_start(out=ut_f[:], in_=c_ut[:, :]), writes=[B_const])
        dma("sp", lambda e: e.dma_start(out=lt_f[:], in_=c_lt[:, :]), writes=[B_const])
        dma("sp", lambda e: e.dma_start(out=iota16[:], in_=c_iota[:, 0:16]), writes=[B_const])
        dma("sp", lambda e: e.dma_start(out=iota16m[:], in_=c_iota[:, 16:32]), writes=[B_const])
        dma("sp", lambda e: e.dma_start(out=onezero[:].rearrange("p a b -> p (a b)"), in_=c_iota[:, 32:32 + 2 * NT]), writes=[B_const])
        op("dve", lambda e: e.tensor_copy(out=ident_b[:], in_=ident_f[:]), reads=[B_const], writes=[B_const])
        op("dve", lambda e: e.memset(ones_f[:], 1.0), writes=[B_const])
        NV = 0
        vec_specs = [("g1", norm1_g, None), ("mcb", m_conv_b, None), ("mng", m_norm_g, None), ("lcb", l_conv_b, None)]
        for j in range(4):
            vec_specs.append(("mcw%d" % j, m_conv_w, j))
            vec_specs.append(("lcw%d" % j, l_conv_w, j))
        for d_ in range(2):
            vec_specs.append(("lbr%d" % d_, l_br, d_))
            vec_specs.append(("lbi%d" % d_, l_bi, d_))
            vec_specs.append(("lam%d" % d_, l_lam, d_))
        vecs = sb("vecs", [128, len(vec_specs), 8])
        vcol = {}
        for i, (nm, ap_, row) in enumerate(vec_specs):
            src = ap_ if row is None else ap_[row]
            src = src.rearrange("(c p) -> p c", p=128)
            dma("sp", lambda e, i=i, src=src: e.dma_start(out=vecs[:, i, :], in_=src, allow_slow_non_contiguous=True),
                writes=[B_const])
            vcol[nm] = (lambda i: (lambda c: vecs[:, i, c:c + 1]))(i)
        cdec = sb("cdec", [128, 2, 8])
        for d_ in range(2):
            li = [i for i, s in enumerate(vec_specs) if s[0] == "lam%d" % d_][0]
            op("act", lambda e: e.activation(out=cdec[:, d_, :], in_=vecs[:, li, :], func=AF.Exp, scale=-1.0),
               reads=[B_const], writes=[B_const])
            op("act", lambda e: e.activation(out=cdec[:, d_, :], in_=cdec[:, d_, :], func=AF.Ln, bias=1.0, scale=1.0),
               reads=[B_const], writes=[B_const])
            op("dve", lambda e: e.tensor_scalar(out=cdec[:, d_, :], in0=cdec[:, d_, :], scalar1=-8.0, scalar2=None,
                                                op0=ALU.mult), reads=[B_const], writes=[B_const])
        bg_bc = sb("bg_bc", [128, 16])
        dma("sp", lambda e: e.dma_start(out=bg_bc[:], in_=b_gates.partition_broadcast(128)), writes=[B_const])
        epsc = sb("epsc", [128, 4])
        op("dve", lambda e: e.memset(epsc[:, 0:1], EPS), writes=[B_const])
        op("dve", lambda e: e.memset(epsc[:, 1:2], -LN16), writes=[B_const])
        op("dve", lambda e: e.memset(epsc[:, 2:3], 1.0), writes=[B_const])
        op("dve", lambda e: e.memset(epsc[:, 3:4], 0.0), writes=[B_const])
        eps_col, nln16_col, one_col, zero_col = epsc[:, 0:1], epsc[:, 1:2], epsc[:, 2:3], epsc[:, 3:4]

        hT_scope = ExitStack()
        hT = sb("hT", [128, 8, T], BF16, hT_scope)
        B_hT = kb.bufs(NG, "hT")

        def load_w_bf16(dst, src_rows, bufs_w, reads=()):
            dma("pool", lambda e: e.dma_start(out=dst, in_=src_rows), reads=list(reads), writes=list(bufs_w))

        def rms_rstd(ctx_name, src, n, ss, tmp_junk, B_in, B_ss, B_junk):
            op("act", lambda e: e.activation(out=tmp_junk, in_=src, func=AF.Square, accum_out=ss),
               reads=[B_in], writes=[B_ss, B_junk])
            op("act", lambda e: e.activation(out=ss, in_=ss, func=AF.Sqrt, scale=1.0 / n, bias=eps_col),
               reads=[B_ss, B_const], writes=[B_ss])
            op("dve", lambda e: e.reciprocal(out=ss, in_=ss), reads=[B_ss], writes=[B_ss])

        with ExitStack() as cs:
            xin = [sb("xa%d" % i, [128, D], F32, cs) for i in range(2)]
            xsb = [sb("xsb%d" % i, [128, D], BF16, cs) for i in range(2)]
            junk = sb("junkA", [128, D], BF16, cs)
            ssA = [sb("ssA%d" % i, [128, 1], F32, cs) for i in range(2)]
            tpA = [ps("tpA%d" % i, [128, 8, 128], BF16, cs) for i in range(2)]
            Bx = kb.bufs(2, "xa"); Bxs = kb.bufs(2, "xsb"); Bj = kb.buf("junkA"); Bss = kb.bufs(2, "ssA"); Btp = kb.bufs(2, "tpA", excl=True)
            for i in range(NT):
                s = i % 2
                dma("sp", lambda e: e.dma_start(out=xin[s][:], in_=x[i * 128:(i + 1) * 128, :]), writes=[Bx[s]])
                rms_rstd("A", xin[s][:], D, ssA[s][:], junk[:], Bx[s], Bss[s], Bj)
                op("dve", lambda e: e.tensor_scalar(out=xsb[s][:], in0=xin[s][:], scalar1=ssA[s][:], scalar2=None, op0=ALU.mult),
                   reads=[Bx[s], Bss[s]], writes=[Bxs[s]])
                for c in range(8):
                    op("pe", lambda e: e.transpose(out=tpA[s][:, c, :], in_=xsb[s][:, c * 128:(c + 1) * 128], identity=ident_b[:]),
                       reads=[Bxs[s], B_const], writes=[Btp[s]])
                g1b = vecs[:, 0, :].unsqueeze(2).to_broadcast([128, 8, 128])
                op("dve", lambda e: e.tensor_tensor(out=hT[:, :, i * 128:(i + 1) * 128], in0=tpA[s][:], in1=g1b, op=ALU.mult),
                   reads=[Btp[s], B_const], writes=[B_hT[i // 4]])
            kb.barrier()

        if dbg is not None and dbg[0] == "hT":
            with ExitStack() as cs:
                t32 = sb("dbg32", [128, 8, 512], F32, cs)
                Bt = kb.buf()
                for g in range(NG):
                    op("dve", lambda e: e.tensor_copy(out=t32[:], in_=hT[:, :, g * 512:(g + 1) * 512]), reads=[B_hT[g]], writes=[Bt])
                    dma("sp", lambda e: e.dma_start(out=dbg_out.rearrange("(c p) t -> p c t", p=128)[:, :, g * 512:(g + 1) * 512], in_=t32[:]), reads=[Bt])
                kb.barrier()

        B_ylT = kb.buf("ylT")
        B_ymT = kb.buf("ymT")
        XL0 = 2064
        LG0 = 3088
        if dbg is None or dbg[0] in ("yl", "full", "x1", "idx"):
          with ExitStack() as cs:
            wxl = sb("wxl", [128, 8, 128], BF16, cs); wlg = sb("wlg", [128, 8, 128], BF16, cs)
            wgate = sb("wgate", [128, 4, 128], BF16, cs)
            XL = sb("XL", [128, T + 4], F32, cs)
            XC = sb("XC", [128, T], F32, cs)
            XCB = sb("XCB", [128, T], BF16, cs)
            LG = sb("LG", [128, T], BF16, cs)
            HF = sb("HF", [128, T], F32, cs)
            NTMP = 2
            R_ = [sb("R%d" % i, [128, 512], F32, cs) for i in range(NTMP)]
            IG_ = [sb("IG%d" % i, [128, 512], F32, cs) for i in range(NTMP)]
            T_ = [sb("T%d" % i, [128, 512], F32, cs) for i in range(NTMP)]
            pL = [ps("pL%d" % i, [128, 512], F32, cs) for i in range(4)]
            Bw = kb.buf("wL"); BXL = kb.buf("XL"); BXC = kb.buf("XC"); BXCB = kb.buf("XCB"); BLG = kb.buf("LG"); BHF = kb.buf("HF")
            BR = kb.bufs(NTMP, "R"); BIG = kb.bufs(NTMP, "IG"); BT = kb.bufs(NTMP, "T"); BpL = kb.bufs(4, "pL", excl=True)
            HB = XL
            pli = 0
            for n in range(8):
                for c in range(8):
                    load_w_bf16(wxl[:, c, :], w_in[c * 128:(c + 1) * 128, XL0 + n * 128: XL0 + (n + 1) * 128], [Bw])
                    load_w_bf16(wlg[:, c, :], w_in[c * 128:(c + 1) * 128, LG0 + n * 128: LG0 + (n + 1) * 128], [Bw])
                for d_ in range(2):
                    load_w_bf16(wgate[:, d_ * 2 + 0, :], l_wr[d_, n], [Bw])
                    load_w_bf16(wgate[:, d_ * 2 + 1, :], l_wi[d_, n], [Bw])
                op("pool", lambda e: e.memset(XL[:, 0:2], 0.0), writes=[BXL])
                op("pool", lambda e: e.memset(XL[:, T + 2:T + 4], 0.0), writes=[BXL])
                for g in range(NG):
                    p1 = pli % 4; pli += 1
                    for c in range(8):
                        op("pe", lambda e: e.matmul(pL[p1][:], lhsT=wxl[:, c, :], rhs=hT[:, c, g * 512:(g + 1) * 512], start=(c == 0), stop=(c == 7)),
                           reads=[Bw, B_hT[g]], writes=[BpL[p1]])
                    op("act", lambda e: e.copy(out=XL[:, 2 + g * 512: 2 + (g + 1) * 512], in_=pL[p1][:]), reads=[BpL[p1]], writes=[BXL])
                    p2 = pli % 4; pli += 1
                    for c in range(8):
                        op("pe", lambda e: e.matmul(pL[p2][:], lhsT=wlg[:, c, :], rhs=hT[:, c, g * 512:(g + 1) * 512], start=(c == 0), stop=(c == 7)),
                           reads=[Bw, B_hT[g]], writes=[BpL[p2]])
                    op("act", lambda e: e.activation(out=LG[:, g * 512:(g + 1) * 512], in_=pL[p2][:], func=AF.Gelu), reads=[BpL[p2]], writes=[BLG])
                op("dve", lambda e: e.tensor_scalar(out=XC[:], in0=XL[:, 0:T], scalar1=vcol["lcw0"](n), scalar2=vcol["lcb"](n), op0=ALU.mult, op1=ALU.add),
                   reads=[BXL, B_const], writes=[BXC])
                for j in range(1, 4):
                    op("dve", lambda e: e.scalar_tensor_tensor(out=XC[:], in0=XL[:, j:j + T], scalar=vcol["lcw%d" % j](n), in1=XC[:], op0=ALU.mult, op1=ALU.add),
                       reads=[BXL, BXC, B_const], writes=[BXC])
                op("pool", lambda e: e.tensor_copy(out=XCB[:], in_=XC[:]), reads=[BXC], writes=[BXCB])
                ti = 0
                for d_ in range(2):
                    groups = list(range(NG)) if d_ == 0 else list(range(NG - 1, -1, -1))
                    Hd = HF if d_ == 0 else HB
                    BHd = BHF if d_ == 0 else BXL
                    hoff = 0 if d_ == 0 else 2
                    prev = None
                    for g in groups:
                        s = ti % NTMP; ti += 1
                        sl = slice(g * 512, (g + 1) * 512)
                        p1 = pli % 4; pli += 1
                        op("pe", lambda e: e.matmul(pL[p1][:], lhsT=wgate[:, d_ * 2, :], rhs=XCB[:, sl], start=True, stop=True),
                           reads=[Bw, BXCB], writes=[BpL[p1]])
                        op("act", lambda e: e.activation(out=R_[s][:], in_=pL[p1][:], func=AF.Sigmoid, bias=vcol["lbr%d" % d_](n)),
                           reads=[BpL[p1], B_const], writes=[BR[s]])
                        p2 = pli % 4; pli += 1
                        op("pe", lambda e: e.matmul(pL[p2][:], lhsT=wgate[:, d_ * 2 + 1, :], rhs=XCB[:, sl], start=True, stop=True),
                           reads=[Bw, BXCB], writes=[BpL[p2]])
                        op("act", lambda e: e.activation(out=IG_[s][:], in_=pL[p2][:], func=AF.Sigmoid, bias=vcol["lbi%d" % d_](n)),
                           reads=[BpL[p2], B_const], writes=[BIG[s]])
                        op("act", lambda e: e.activation(out=R_[s][:], in_=R_[s][:], func=AF.Exp, scale=cdec[:, d_, n:n + 1]),
                           reads=[BR[s], B_const], writes=[BR[s]])
                        op("pool", lambda e: e.tensor_tensor(out=T_[s][:], in0=R_[s][:], in1=R_[s][:], op=ALU.mult), reads=[BR[s]], writes=[BT[s]])
                        op("act", lambda e: e.activation(out=T_[s][:], in_=T_[s][:], func=AF.Sqrt, scale=-1.0, bias=one_col),
                           reads=[BT[s], B_const], writes=[BT[s]])
                        op("pool", lambda e: e.tensor_tensor(out=IG_[s][:], in0=IG_[s][:], in1=XC[:, sl], op=ALU.mult), reads=[BIG[s], BXC], writes=[BIG[s]])
                        op("pool", lambda e: e.tensor_tensor(out=T_[s][:], in0=T_[s][:], in1=IG_[s][:], op=ALU.mult), reads=[BT[s], BIG[s]], writes=[BT[s]])
                        if d_ == 0:
                            init = 0.0 if prev is None else HF[:, prev * 512 + 511: prev * 512 + 512]
                            op("dve", lambda e: e.tensor_tensor_scan(out=HF[:, sl], data0=R_[s][:], data1=T_[s][:], initial=init, op0=ALU.mult, op1=ALU.add),
                               reads=[BR[s], BT[s], BHF], writes=[BHF])
                        else:
                            init = 0.0 if prev is None else HB[:, 2 + prev * 512: 2 + prev * 512 + 1]
                            op("dve", lambda e: e.tensor_tensor_scan(out=HB[:, 2 + g * 512: 2 + (g + 1) * 512][:, ::-1], data0=R_[s][:, ::-1], data1=T_[s][:, ::-1],
                                                                    initial=init, op0=ALU.mult, op1=ALU.add),
                               reads=[BR[s], BT[s], BXL], writes=[BXL])
                        prev = g
                op("dve", lambda e: e.tensor_tensor(out=HF[:], in0=HF[:], in1=HB[:, 2:2 + T], op=ALU.add), reads=[BHF, BXL], writes=[BHF])
                op("dve", lambda e: e.tensor_tensor(out=XCB[:], in0=HF[:], in1=LG[:], op=ALU.mult), reads=[BHF, BLG], writes=[BXCB])
                dma("sp", lambda e: e.dma_start(out=ylT[n * 128:(n + 1) * 128, :], in_=XCB[:]), reads=[BXCB], writes=[B_ylT])
            kb.barrier()

        if dbg is not None and dbg[0] == "yl":
            with ExitStack() as cs:
                tb = sb("dbgb", [128, T], BF16, cs); t32 = sb("dbg32", [128, T], F32, cs)
                Bt = kb.buf(); Bt2 = kb.buf()
                for n in range(8):
                    dma("sp", lambda e: e.dma_start(out=tb[:], in_=ylT[n * 128:(n + 1) * 128, :]), reads=[B_ylT], writes=[Bt])
                    op("dve", lambda e: e.tensor_copy(out=t32[:], in_=tb[:]), reads=[Bt], writes=[Bt2])
                    dma("sp", lambda e: e.dma_start(out=dbg_out[n * 128:(n + 1) * 128, :], in_=t32[:]), reads=[Bt2])
                kb.barrier()


        B_hf = kb.bufs(NT, "hfscr")
        stopM = dbg[2] if (dbg is not None and len(dbg) > 2) else 0
        if dbg is None or dbg[0] in ("ym", "full", "x1", "idx"):
         try:
          with ExitStack() as ms:
            wg = sb("wg", [128, 8, 16], BF16, ms)
            G = sb("G", [128, NT, 16], F32, ms)
            LF = [sb("LF%d" % d_, [128, NT, 4], F32, ms) for d_ in range(2)]
            SC1 = [sb("SC1%d" % d_, [128, NT, 4], F32, ms) for d_ in range(2)]
            EB = [sb("EB%d" % d_, [128, NT, 4], F32, ms) for d_ in range(2)]
            EG = [sb("EG%d" % d_, [128, NT, 4], F32, ms) for d_ in range(2)]
            KWS = [sb("KWS%d" % d_, [128, NT, 4], F32, ms) for d_ in range(2)]
            pb = [ps("pM%d" % i, [128, 512], F32, ms) for i in range(7)]
            pTb = ps("pMT", [128, 1024], BF16, ms)
            Bpb = kb.bufs(8, "pM", excl=True)
            ut_b = sb("ut_b", [128, 128], BF16, ms); lt_b = sb("lt_b", [128, 128], BF16, ms); ones_b = sb("ones_b", [128, 128], BF16, ms)
            op("dve", lambda e: e.tensor_copy(out=ut_b[:], in_=ut_f[:]), reads=[B_const], writes=[B_const])
            op("dve", lambda e: e.tensor_copy(out=lt_b[:], in_=lt_f[:]), reads=[B_const], writes=[B_const])
            op("dve", lambda e: e.memset(ones_b[:], 1.0), writes=[B_const])
            LFh = sb("LFh", [128, NT * 4], BF16, ms); LFl = sb("LFl", [128, NT * 4], BF16, ms); LFr = sb("LFr", [128, NT * 4], F32, ms)
            Bwg = kb.buf("wg"); BG = kb.buf("G"); BGS = kb.buf("Gscal")
            wgf = sb("wgf", [128, 8, 16], F32, ms)
            Bwgf = kb.buf("wgf")
            dma("sp", lambda e: e.dma_start(out=wgf[:], in_=w_in[:, 2048:2064].rearrange("(c p) n -> p c n", p=128)), writes=[Bwgf])
            op("dve", lambda e: e.tensor_copy(out=wg[:], in_=wgf[:]), reads=[Bwgf], writes=[Bwg])
            for i in range(NT):
                p = i % 2
                for c in range(8):
                    op("pe", lambda e: e.matmul(pb[p][:, 0:16], lhsT=hT[:, c, i * 128:(i + 1) * 128], rhs=wg[:, c, :], start=(c == 0), stop=(c == 7)),
                       reads=[Bwg, B_hT[i // 4]], writes=[Bpb[p]])
                op("dve", lambda e: e.tensor_tensor(out=G[:, i, :], in0=pb[p][:, 0:16], in1=bg_bc[:], op=ALU.add), reads=[Bpb[p], B_const], writes=[BG])
            mask = [ut_f, lt_f]
            for d_ in range(2):
                fcol = (2 * d_ + 1) * 4
                icol = (2 * d_) * 4
                op("act", lambda e: e.activation(out=LF[d_][:], in_=G[:, :, fcol:fcol + 4], func=AF.Exp, scale=-1.0), reads=[BG], writes=[BGS])
                op("act", lambda e: e.activation(out=LF[d_][:], in_=LF[d_][:], func=AF.Ln, bias=one_col, scale=1.0), reads=[BGS, B_const], writes=[BGS])
                op("dve", lambda e: e.tensor_scalar(out=LF[d_][:], in0=LF[d_][:], scalar1=-1.0, scalar2=None, op0=ALU.mult), reads=[BGS], writes=[BGS])
                pB = pb[2 + d_ * 2]; pGt = pb[3 + d_ * 2]
                maskb = [ut_b, lt_b]
                lf2 = LF[d_][:].rearrange("p a b -> p (a b)")
                op("dve", lambda e: e.tensor_copy(out=LFh[:], in_=lf2), reads=[BGS], writes=[BGS])
                op("dve", lambda e: e.tensor_tensor(out=LFr[:], in0=lf2, in1=LFh[:], op=ALU.subtract), reads=[BGS], writes=[BGS])
                op("dve", lambda e: e.tensor_copy(out=LFl[:], in_=LFr[:]), reads=[BGS], writes=[BGS])
                op("pe", lambda e: e.matmul(pB[:, 0:128], lhsT=maskb[d_][:], rhs=LFh[:], start=True, stop=False), reads=[BGS, B_const], writes=[Bpb[2 + d_ * 2]])
                op("pe", lambda e: e.matmul(pB[:, 0:128], lhsT=maskb[d_][:], rhs=LFl[:], start=False, stop=True), reads=[BGS, B_const], writes=[Bpb[2 + d_ * 2]])
                op("pe", lambda e: e.matmul(pGt[:, 0:128], lhsT=ones_b[:], rhs=LFh[:], start=True, stop=False), reads=[BGS, B_const], writes=[Bpb[3 + d_ * 2]])
                op("pe", lambda e: e.matmul(pGt[:, 0:128], lhsT=ones_b[:], rhs=LFl[:], start=False, stop=True), reads=[BGS, B_const], writes=[Bpb[3 + d_ * 2]])
                pBv = pB[:, 0:128].rearrange("p (a b) -> p a b", b=4)
                pGv = pGt[:, 0:128].rearrange("p (a b) -> p a b", b=4)
                op("dve", lambda e: e.tensor_tensor(out=SC1[d_][:], in0=G[:, :, icol:icol + 4], in1=pBv, op=ALU.subtract), reads=[BG, Bpb[2 + d_ * 2]], writes=[BGS])
                op("act", lambda e: e.activation(out=SC1[d_][:], in_=SC1[d_][:], func=AF.Exp, bias=nln16_col, scale=1.0), reads=[BGS, B_const], writes=[BGS])
                op("act", lambda e: e.activation(out=EB[d_][:], in_=pBv, func=AF.Exp), reads=[Bpb[2 + d_ * 2]], writes=[BGS])
                op("act", lambda e: e.activation(out=EG[d_][:], in_=pGv, func=AF.Exp), reads=[Bpb[3 + d_ * 2]], writes=[BGS])
                op("dve", lambda e: e.tensor_tensor(out=KWS[d_][:], in0=SC1[d_][:], in1=EG[d_][:], op=ALU.mult), reads=[BGS], writes=[BGS])

            for h in range(4 if stopM == 0 else (0 if stopM == 1 else 1)):
              with ExitStack() as hs:
                wxm = sb("wxm", [128, 8, 256], BF16, hs); wo = sb("wo", [128, 8, 256], BF16, hs)
                wq = sb("wq", [128, 2, 256], BF16, hs); wk = sb("wk", [128, 2, 256], BF16, hs); wv = sb("wv", [128, 2, 256], BF16, hs)
                XC = sb("mXC", [128, 2, T], BF16, hs)
                V = sb("mV", [128, NT, 260], BF16, hs)
                Bwh = kb.buf("wh"); BXC = kb.buf("mXC"); BV = kb.buf("mV")
                for c in range(8):
                    load_w_bf16(wxm[:, c, :], w_in[c * 128:(c + 1) * 128, h * 256:(h + 1) * 256], [Bwh])
                    load_w_bf16(wo[:, c, :], w_in[c * 128:(c + 1) * 128, 1024 + h * 256: 1024 + (h + 1) * 256], [Bwh])
                for dc in range(2):
                    load_w_bf16(wq[:, dc, :], m_wq[h, dc * 128:(dc + 1) * 128, :], [Bwh])
                    load_w_bf16(wk[:, dc, :], m_wk[h, dc * 128:(dc + 1) * 128, :], [Bwh])
                    load_w_bf16(wv[:, dc, :], m_wv[h, dc * 128:(dc + 1) * 128, :], [Bwh])
                op("dve", lambda e: e.tensor_copy(out=V[:, :, 256:258], in_=onezero[:]), reads=[B_const], writes=[BV])
                pi = 0
                with ExitStack() as s1:
                    XM = sb("mXM", [128, T + 4], F32, s1)
                    XMB = sb("mXMB", [128, 2, T], BF16, s1)
                    XCF = [sb("mXCF%d" % i, [128, 1024], F32, s1) for i in range(2)]
                    XSG = [sb("mXSG%d" % i, [128, 1024], F32, s1) for i in range(2)]; BXSG = kb.bufs(2, "mXSG")
                    BXM = kb.buf("mXM"); BXMB = kb.buf("mXMB"); BXCF = kb.bufs(2, "mXCF")
                    op("pool", lambda e: e.memset(XM[:, 0:2], 0.0), writes=[BXM])
                    op("pool", lambda e: e.memset(XM[:, T + 2:T + 4], 0.0), writes=[BXM])
                    for cc in range(2):
                        ch = h * 2 + cc
                        for g in range(NG):
                            p = pi % 4; pi += 1
                            for c in range(8):
                                op("pe", lambda e: e.matmul(pb[p][:], lhsT=wxm[:, c, cc * 128:(cc + 1) * 128], rhs=hT[:, c, g * 512:(g + 1) * 512], start=(c == 0), stop=(c == 7)),
                                   reads=[Bwh, B_hT[g]], writes=[Bpb[p]])
                            op("act", lambda e: e.copy(out=XM[:, 2 + g * 512:2 + (g + 1) * 512], in_=pb[p][:]), reads=[Bpb[p]], writes=[BXM])
                            op("dve", lambda e: e.tensor_copy(out=XMB[:, cc, g * 512:(g + 1) * 512], in_=pb[p][:]), reads=[Bpb[p]], writes=[BXMB])
                        for q4 in range(4):
                            s = q4 % 2
                            o0 = q4 * 1024
                            op("dve", lambda e: e.tensor_scalar(out=XCF[s][:], in0=XM[:, o0:o0 + 1024], scalar1=vcol["mcw0"](ch), scalar2=vcol["mcb"](ch), op0=ALU.mult, op1=ALU.add),
                               reads=[BXM, B_const], writes=[BXCF[s]])
                            for j in range(1, 4):
                                op("dve", lambda e: e.scalar_tensor_tensor(out=XCF[s][:], in0=XM[:, o0 + j:o0 + j + 1024], scalar=vcol["mcw%d" % j](ch), in1=XCF[s][:], op0=ALU.mult, op1=ALU.add),
                                   reads=[BXM, BXCF[s], B_const], writes=[BXCF[s]])
                            op("act", lambda e: e.activation(out=XSG[s][:], in_=XCF[s][:], func=AF.Sigmoid), reads=[BXCF[s]], writes=[BXSG[s]])
                            op("dve", lambda e: e.tensor_tensor(out=XC[:, cc, o0:o0 + 1024], in0=XCF[s][:], in1=XSG[s][:], op=ALU.mult), reads=[BXCF[s], BXSG[s]], writes=[BXC])
                    for i in range(NT):
                        p = 4 + (i % 2)
                        for dc in range(2):
                            op("pe", lambda e: e.matmul(pb[p][:, 0:256], lhsT=XMB[:, dc, i * 128:(i + 1) * 128], rhs=wv[:, dc, :], start=(dc == 0), stop=(dc == 1)),
                               reads=[BXMB, Bwh], writes=[Bpb[p]])
                        op("act", lambda e: e.copy(out=V[:, i, 0:256], in_=pb[p][:, 0:256]), reads=[Bpb[p]], writes=[BV])
                    kb.barrier()
                if stopM == 2:
                    continue
                with ExitStack() as s2:
                    QT = sb("mQT", [128, 2, T], BF16, s2); KT = sb("mKT", [128, 2, T], BF16, s2)
                    BQT = kb.buf("mQT"); BKT = kb.buf("mKT")
                    for (dst, Bd, wmat) in ((QT, BQT, wq), (KT, BKT, wk)):
                        for ec in range(2):
                            for g in range(NG):
                                p = pi % 4; pi += 1
                                for dc in range(2):
                                    op("pe", lambda e: e.matmul(pb[p][:], lhsT=wmat[:, dc, ec * 128:(ec + 1) * 128], rhs=XC[:, dc, g * 512:(g + 1) * 512], start=(dc == 0), stop=(dc == 1)),
                                       reads=[Bwh, BXC], writes=[Bpb[p]])
                                op("act", lambda e: e.copy(out=dst[:, ec, g * 512:(g + 1) * 512], in_=pb[p][:]), reads=[Bpb[p]], writes=[Bd])
                    ST = sb("mST", [128, 128], BF16, s2); KW = sb("mKW", [128, 256], BF16, s2)
                    Cst = sb("mCst", [128, 2, 258], F32, s2); Cbf = sb("mCbf", [128, 2, 258], BF16, s2)
                    t2 = sb("mt2", [128, 1], F32, s2); Bt2 = kb.buf()
                    t1 = sb("mt1", [128, 1], F32, s2); HFc = sb("mHFc", [128, 256], F32, s2); HS = sb("mHS", [128, 256], F32, s2)
                    junk = sb("mjunk", [128, 256], BF16, s2); ssq = sb("mssq", [128, 1], F32, s2)
                    HN = sb("mHN", [128, 256], BF16, s2); SG = sb("mSG", [128, 2, 128], F32, s2); YM = sb("mYM", [128, 2, 512], BF16, s2)
                    BST = kb.buf(); BKW = kb.buf(); BCst = kb.buf(); BCbf = kb.buf(); Bt1 = kb.buf(); BHFc = kb.buf(); BHS = kb.buf()
                    Bjk = kb.buf(); Bssq = kb.buf(); BHN = kb.buf(); BSG = kb.buf(); BYM = kb.buf()
                    pS, pO, pK, pP0, pP1, pOT = pb[0], pb[1], pb[2], pb[3], pb[4], pb[6]
                    BpS, BpO, BpK, BpP0, BpP1, BpT, BpOT = Bpb[0], Bpb[1], Bpb[2], Bpb[3], Bpb[4], Bpb[5], Bpb[6]
                    pTv = pTb[:, 0:256].rearrange("p (a b) -> p a b", b=128)
                    for d_ in range(2 if stopM == 0 else (0 if stopM == 3 else 1)):
                        op("dve", lambda e: e.memset(Cst[:], 0.0), writes=[BCst])
                        op("dve", lambda e: e.memset(Cbf[:], 0.0), writes=[BCbf])
                        chunks = list(range(NT)) if d_ == 0 else list(range(NT - 1, -1, -1))
                        if stopM == 4:
                            chunks = chunks[:4]
                        for c in chunks:
                            tsl = slice(c * 128, (c + 1) * 128)
                            for dc in range(2):
                                op("pe", lambda e: e.matmul(pS[:, 0:128], lhsT=KT[:, dc, tsl], rhs=QT[:, dc, tsl], start=(dc == 0), stop=(dc == 1)),
                                   reads=[BKT, BQT], writes=[BpS])
                            op("dve", lambda e: e.scalar_tensor_tensor(out=ST[:], in0=pS[:, 0:128], scalar=SC1[d_][:, c, h:h + 1], in1=mask[d_][:], op0=ALU.mult, op1=ALU.mult),
                               reads=[BpS, BGS, B_const], writes=[BST])
                            for dc in range(2):
                                op("pe", lambda e: e.matmul(pO[:, 0:258], lhsT=QT[:, dc, tsl], rhs=Cbf[:, dc, :], start=(dc == 0), stop=False),
                                   reads=[BQT, BCbf], writes=[BpO])
                            op("pe", lambda e: e.matmul(pO[:, 0:258], lhsT=ST[:], rhs=V[:, c, 0:258], start=False, stop=True), reads=[BST, BV], writes=[BpO])
                            for dc in range(2):
                                op("pe", lambda e: e.matmul(pK[:, 0:256], lhsT=XC[:, dc, tsl], rhs=wk[:, dc, :], start=(dc == 0), stop=(dc == 1)),
                                   reads=[BXC, Bwh], writes=[BpK])
                            op("dve", lambda e: e.tensor_scalar(out=KW[:], in0=pK[:, 0:256], scalar1=KWS[d_][:, c, h:h + 1], scalar2=None, op0=ALU.mult), reads=[BpK, BGS], writes=[BKW])
                            op("pe", lambda e: e.matmul(pP0[:, 0:258], lhsT=KW[:, 0:128], rhs=V[:, c, 0:258], start=True, stop=True), reads=[BKW, BV], writes=[BpP0])
                            op("pe", lambda e: e.matmul(pP1[:, 0:258], lhsT=KW[:, 128:256], rhs=V[:, c, 0:258], start=True, stop=True), reads=[BKW, BV], writes=[BpP1])
                            ebc = EB[d_][:, c, h:h + 1]
                            op("dve", lambda e: e.tensor_scalar(out=t1[:], in0=pO[:, 256:257], scalar1=ebc, scalar2=None, op0=ALU.mult), reads=[BpO, BGS], writes=[Bt1])
                            op("dve", lambda e: e.tensor_scalar(out=t2[:], in0=t1[:], scalar1=-1.0, scalar2=None, op0=ALU.mult), reads=[Bt1], writes=[Bt2])
                            op("dve", lambda e: e.tensor_tensor(out=t1[:], in0=t1[:], in1=t2[:], op=ALU.max), reads=[Bt1, Bt2], writes=[Bt1])
                            op("dve", lambda e: e.tensor_scalar(out=t1[:], in0=t1[:], scalar1=1.0, scalar2=None, op0=ALU.max), reads=[Bt1], writes=[Bt1])
                            op("dve", lambda e: e.reciprocal(out=t1[:], in_=t1[:]), reads=[Bt1], writes=[Bt1])
                            op("dve", lambda e: e.tensor_scalar(out=t1[:], in0=t1[:], scalar1=ebc, scalar2=None, op0=ALU.mult), reads=[Bt1, BGS], writes=[Bt1])
                            if d_ == 0:
                                op("dve", lambda e: e.tensor_scalar(out=HFc[:], in0=pO[:, 0:256], scalar1=t1[:], scalar2=None, op0=ALU.mult), reads=[BpO, Bt1], writes=[BHFc])
                                dma("sp", lambda e: e.dma_start(out=hf_scr[tsl, :], in_=HFc[:]), reads=[BHFc], writes=[B_hf[c]])
                            else:
                                dma("sp", lambda e: e.dma_start(out=HFc[:], in_=hf_scr[tsl, :]), reads=[B_hf[c]], writes=[BHFc])
                                op("dve", lambda e: e.scalar_tensor_tensor(out=HS[:], in0=pO[:, 0:256], scalar=t1[:], in1=HFc[:], op0=ALU.mult, op1=ALU.add),
                                   reads=[BpO, Bt1, BHFc], writes=[BHS])
                                rms_rstd("M", HS[:], 256, ssq[:], junk[:], BHS, Bssq, Bjk)
                                op("dve", lambda e: e.tensor_scalar(out=HN[:], in0=HS[:], scalar1=ssq[:], scalar2=None, op0=ALU.mult), reads=[BHS, Bssq], writes=[BHN])
                                for dc in range(2):
                                    op("pe", lambda e: e.transpose(out=pTv[:, dc, :], in_=HN[:, dc * 128:(dc + 1) * 128], identity=ident_b[:]), reads=[BHN, B_const], writes=[BpT])
                                for dc in range(2):
                                    for c8 in range(8):
                                        op("pe", lambda e: e.matmul(pOT[:, dc * 128:(dc + 1) * 128], lhsT=wo[:, c8, dc * 128:(dc + 1) * 128], rhs=hT[:, c8, tsl], start=(c8 == 0), stop=(c8 == 7)),
                                           reads=[Bwh, B_hT[c // 4]], writes=[BpOT])
                                op("act", lambda e: e.activation(out=SG[:].rearrange("p a b -> p (a b)"), in_=pOT[:, 0:256], func=AF.Sigmoid), reads=[BpOT], writes=[BSG])
                                q4 = c % 4
                                for dc in range(2):
                                    op("dve", lambda e: e.scalar_tensor_tensor(out=YM[:, dc, q4 * 128:(q4 + 1) * 128], in0=pTv[:, dc, :], scalar=vcol["mng"](h * 2 + dc), in1=SG[:, dc, :], op0=ALU.mult, op1=ALU.mult),
                                       reads=[BpT, BSG, B_const], writes=[BYM])
                                if q4 == 0:
                                    g = c // 4
                                    dma("sp", lambda e: e.dma_start(out=ymT[h * 256:(h + 1) * 256, g * 512:(g + 1) * 512].rearrange("(a p) t -> p a t", p=128), in_=YM[:]),
                                        reads=[BYM], writes=[B_ymT])
                            egc = EG[d_][:, c, h:h + 1]
                            op("dve", lambda e: e.scalar_tensor_tensor(out=Cst[:, 0, :], in0=Cst[:, 0, :], scalar=egc, in1=pP0[:, 0:258], op0=ALU.mult, op1=ALU.add),
                               reads=[BCst, BGS, BpP0], writes=[BCst])
                            op("dve", lambda e: e.scalar_tensor_tensor(out=Cst[:, 1, :], in0=Cst[:, 1, :], scalar=egc, in1=pP1[:, 0:258], op0=ALU.mult, op1=ALU.add),
                               reads=[BCst, BGS, BpP1], writes=[BCst])
                            op("pool", lambda e: e.tensor_copy(out=Cbf[:], in_=Cst[:]), reads=[BCst], writes=[BCbf])
                    kb.barrier()
            kb.barrier()
         except _Stop:
            kb.barrier()

        if dbg is not None and dbg[0] == "ym":
            with ExitStack() as cs:
                tb = sb("dbgb", [128, T], BF16, cs); t32 = sb("dbg32", [128, T], F32, cs)
                Bt = kb.buf(); Bt2 = kb.buf()
                for n in range(8):
                    dma("sp", lambda e: e.dma_start(out=tb[:], in_=ymT[n * 128:(n + 1) * 128, :]), reads=[B_ymT], writes=[Bt])
                    op("dve", lambda e: e.tensor_copy(out=t32[:], in_=tb[:]), reads=[Bt], writes=[Bt2])
                    dma("sp", lambda e: e.dma_start(out=dbg_out[n * 128:(n + 1) * 128, :], in_=t32[:]), reads=[Bt2])
                kb.barrier()


        B_mg = kb.buf("mgT")
        MP0 = 4112
        if dbg is None or dbg[0] in ("full", "x1", "idx"):
          with ExitStack() as cs:
            wbm_a = sb("wbm_a", [128, 8, D], BF16, cs); wbl_a = sb("wbl_a", [128, 8, D], BF16, cs)
            wgm_a = sb("wgm_a", [128, 8, D], BF16, cs); wgl_a = sb("wgl_a", [128, 8, D], BF16, cs)
            YMt = [sb("YMt%d" % i, [128, 8, 512], BF16, cs) for i in range(2)]; YLt = [sb("YLt%d" % i, [128, 8, 512], BF16, cs) for i in range(2)]
            GMs = [sb("GMs%d" % i, [128, 512], F32, cs) for i in range(2)]; GLs = [sb("GLs%d" % i, [128, 512], F32, cs) for i in range(2)]
            TM = [sb("TM%d" % i, [128, 512], F32, cs) for i in range(2)]
            MGe = [sb("MGe%d" % i, [128, 8, 512], BF16, cs) for i in range(2)]
            pE = [ps("pE%d" % i, [128, 512], F32, cs) for i in range(8)]
            Bwe = kb.buf(); BYM_ = kb.bufs(2); BYL_ = kb.bufs(2); BGM = kb.bufs(2); BGL = kb.bufs(2); BTM = kb.bufs(2); BMGe = kb.bufs(2)
            BpE = kb.bufs(8, "pE", excl=True)
            for c in range(8):
                rs = slice(c * 128, (c + 1) * 128)
                load_w_bf16(wbm_a[:, c, :], w_bm[rs, :], [Bwe])
                load_w_bf16(wbl_a[:, c, :], w_bl[rs, :], [Bwe])
                load_w_bf16(wgm_a[:, c, :], w_in[rs, MP0:MP0 + 1024], [Bwe])
                load_w_bf16(wgl_a[:, c, :], w_in[rs, MP0 + 1024:MP0 + 2048], [Bwe])
            it = 0
            for g in range(NG):
                gs = slice(g * 512, (g + 1) * 512)
                sg_ = g % 2
                dma("sp", lambda e: e.dma_start(out=YMt[sg_][:], in_=ymT[:, gs].rearrange("(c p) t -> p c t", p=128)), reads=[B_ymT], writes=[BYM_[sg_]])
                dma("sp", lambda e: e.dma_start(out=YLt[sg_][:], in_=ylT[:, gs].rearrange("(c p) t -> p c t", p=128)), reads=[B_ylT], writes=[BYL_[sg_]])
                for e_ in range(8):
                    es_ = slice(e_ * 128, (e_ + 1) * 128)
                    pz = (it % 2) * 4; tz = it % 2; it += 1
                    for c in range(8):
                        op("pe", lambda e: e.matmul(pE[pz + 0][:], lhsT=wbm_a[:, c, es_], rhs=YMt[sg_][:, c, :], start=(c == 0), stop=(c == 7)), reads=[Bwe, BYM_[sg_]], writes=[BpE[pz + 0]])
                    for c in range(8):
                        op("pe", lambda e: e.matmul(pE[pz + 1][:], lhsT=wbl_a[:, c, es_], rhs=YLt[sg_][:, c, :], start=(c == 0), stop=(c == 7)), reads=[Bwe, BYL_[sg_]], writes=[BpE[pz + 1]])
                    for c in range(8):
                        op("pe", lambda e: e.matmul(pE[pz + 2][:], lhsT=wgm_a[:, c, es_], rhs=hT[:, c, gs], start=(c == 0), stop=(c == 7)), reads=[Bwe, B_hT[g]], writes=[BpE[pz + 2]])
                    for c in range(8):
                        op("pe", lambda e: e.matmul(pE[pz + 3][:], lhsT=wgl_a[:, c, es_], rhs=hT[:, c, gs], start=(c == 0), stop=(c == 7)), reads=[Bwe, B_hT[g]], writes=[BpE[pz + 3]])
                    op("act", lambda e: e.activation(out=GMs[tz][:], in_=pE[pz + 2][:], func=AF.Sigmoid), reads=[BpE[pz + 2]], writes=[BGM[tz]])
                    op("act", lambda e: e.activation(out=GLs[tz][:], in_=pE[pz + 3][:], func=AF.Sigmoid), reads=[BpE[pz + 3]], writes=[BGL[tz]])
                    op("dve", lambda e: e.tensor_tensor(out=TM[tz][:], in0=GMs[tz][:], in1=pE[pz + 0][:], op=ALU.mult), reads=[BGM[tz], BpE[pz + 0]], writes=[BTM[tz]])
                    op("dve", lambda e: e.tensor_tensor(out=GLs[tz][:], in0=GLs[tz][:], in1=pE[pz + 1][:], op=ALU.mult), reads=[BGL[tz], BpE[pz + 1]], writes=[BGL[tz]])
                    op("pool", lambda e: e.tensor_tensor(out=MGe[sg_][:, e_, :], in0=TM[tz][:], in1=GLs[tz][:], op=ALU.add), reads=[BTM[tz], BGL[tz]], writes=[BMGe[sg_]])
                dma("sp", lambda e: e.dma_start(out=mgT[:, gs].rearrange("(c p) t -> p c t", p=128), in_=MGe[sg_][:]), reads=[BMGe[sg_]], writes=[B_mg])
            kb.barrier()
        hT_scope.close()
        B_uv = kb.buf("uvtab")
        if need_peer:
          with ExitStack() as us:
            UVt = [sb("UVt%d" % i, [128, 4, 2 * D], BF16, us) for i in range(2)]
            BUVu = kb.bufs(2); BUVv = kb.bufs(2)
            for blk in range(32):
                s_ = blk % 2
                rws = slice(blk * 512, (blk + 1) * 512)
                dma("pool", lambda e: e.dma_start(out=UVt[s_][:, :, 0:D], in_=p_u[rws, :].rearrange("(p a) d -> p a d", a=4)), writes=[BUVu[s_]])
                dma("pool", lambda e: e.dma_start(out=UVt[s_][:, :, D:2 * D], in_=p_v[rws, :].rearrange("(p a) d -> p a d", a=4)), writes=[BUVv[s_]])
                dma("sp", lambda e: e.dma_start(out=uv_tab[rws, :].rearrange("(p a) d -> p a d", a=4), in_=UVt[s_][:]), reads=[BUVu[s_], BUVv[s_]], writes=[B_uv])
            kb.barrier()

        if dbg is None or dbg[0] in ("full", "x1", "idx"):
          with ExitStack() as cs:
            do_peer = dbg is None or dbg[0] in ("full", "idx")
            g2_bc = sb("g2_bc", [128, D], F32, cs); gf_bc = sb("gf_bc", [128, D], F32, cs)
            dma("sp", lambda e: e.dma_start(out=g2_bc[:], in_=norm2_g.partition_broadcast(128)), writes=[B_const])
            dma("sp", lambda e: e.dma_start(out=gf_bc[:], in_=fin_g.partition_broadcast(128)), writes=[B_const])
            woutb = sb("woutb", [128, 8, D], BF16, cs)
            Bwo = kb.buf()
            for c in range(8):
                load_w_bf16(woutb[:, c, :], w_out[c * 128:(c + 1) * 128, :], [Bwo])
            pF = [ps("pF%d" % i, [128, 512], F32, cs) if i != 2 else ps("pFb", [128, 1024], BF16, cs) for i in range(8)]
            BpF = kb.bufs(8, "pF", excl=True)
            if do_peer:
                wpq = sb("wpq", [128, 8, 2048], BF16, cs)
                KEYT = sb("KEYT", [128, 16, 128], BF16, cs)
                Bwp = kb.buf()
                for c in range(8):
                    load_w_bf16(wpq[:, c, 0:1024], p_wq[c * 128:(c + 1) * 128, 0:1024], [Bwp])
                    load_w_bf16(wpq[:, c, 1024:2048], p_wq[c * 128:(c + 1) * 128, 1024:2048], [Bwp])
                with ExitStack() as ks:
                    KEYB = sb("KEYB", [128, 16, 128], BF16, ks)
                    Bkf = kb.buf()
                    for hp in range(16):
                        load_w_bf16(KEYB[:, hp, :], p_keys[hp], [Bkf])
                    for q in range(2):
                        for h8 in range(8):
                            hp = q * 8 + h8
                            op("pe", lambda e: e.transpose(out=pF[2][:, h8 * 128:(h8 + 1) * 128], in_=KEYB[:, hp, :], identity=ident_b[:]),
                               reads=[Bkf, B_const], writes=[BpF[2]])
                        op("act", lambda e: e.copy(out=KEYT[:, q * 8:(q + 1) * 8, :].rearrange("p a b -> p (a b)"), in_=pF[2][:]), reads=[BpF[2]], writes=[Bwp])
                    kb.barrier()
            MGg = sb("MGg", [128, 8, 512], BF16, cs); BMGg = kb.buf()
            xin = sb("xinE", [128, D], F32, cs); Bxin = kb.buf()
            x1t = sb("x1t", [128, D], F32, cs); Bx1 = kb.buf()
            ss2 = sb("ss2", [128, 1], F32, cs); Bss2 = kb.buf()
            junkF = sb("junkF", [128, D], BF16, cs); BjF = kb.buf()
            yt = sb("yt", [128, D], F32, cs); Byt = kb.buf()
            if do_peer:
                h2 = sb("h2", [128, D], F32, cs); Bh2 = kb.buf()
                h2b = sb("h2b", [128, D], BF16, cs); Bh2b = kb.buf()
                h2T = sb("h2T", [128, 8, 128], BF16, cs); Bh2T = kb.buf()
                QP = sb("QP", [128, 16, 128], BF16, cs); BQP = kb.buf()
                WK = sb("WK", [128, 16, 128], F32, cs); BWK = kb.buf()
                SCS = sb("SCS", [128, 16, 128], F32, cs); BSCS = kb.buf()
                v8 = sb("v8", [128, 16, 16], F32, cs); Bv8 = kb.buf()
                Bv8a = kb.bufs(16); Bv8b = kb.bufs(16); Bi8a = kb.bufs(16); Bi8b = kb.bufs(16); BWKa = kb.bufs(16)
                Bb8a = kb.bufs(8); Bb8b = kb.bufs(8); Bp8a = kb.bufs(8); Bp8b = kb.bufs(8); BCWa = kb.bufs(8)
                i8 = sb("i8", [128, 16, 16], U32, cs); Bi8 = kb.buf()
                i8f = sb("i8f", [128, 16, 16], F32, cs); Bi8f = kb.buf()
                CAND = sb("CAND", [128, 8, 256], F32, cs); BCAND = kb.buf()
                CW = sb("CW", [128, 8, 256], F32, cs); BCW = kb.buf()
                b8 = sb("b8", [128, 8, 16], F32, cs); Bb8 = kb.buf()
                p8 = sb("p8", [128, 8, 16], U32, cs); Bp8 = kb.buf()
                pff = sb("pff", [128, 8, 16], F32, cs)
                phf = sb("phf", [128, 8, 16], F32, cs); plf = sb("plf", [128, 8, 16], F32, cs); Bph = kb.buf()
                EQ = sb("EQ", [128, 128, 16], F32, cs); BEQ = kb.buf()
                ID0 = sb("ID0", [128, 128], F32, cs); ID1 = sb("ID1", [128, 128], F32, cs); BID = kb.buf()
                IDXu = sb("IDXu", [128, 128], U32, cs); BIDX = kb.buf()
                NB = sb("NB", [128, 8], F32, cs); Zs = sb("Zs", [128, 8], F32, cs); EX = sb("EX", [128, 8, 16], F32, cs); BSM = kb.buf()
                GATE = sb("GATE", [128, 128], F32, cs); BGATE = kb.buf()
                ACTV = sb("ACTV", [128, 128], F32, cs); BACTV = kb.buf()
                WT = sb("WT", [128, 128], F32, cs); BWT = kb.buf()
                NGB = 14
                GB = [sb("GB%d" % i, [128, 2 * D], BF16, cs) for i in range(NGB)]; BGB = kb.bufs(NGB)
                DG = [sb("DG%d" % i, [128, 128], BF16, cs) for i in range(8)]; BDG = kb.bufs(8)
                G1 = sb("G1", [128, 128], F32, cs)
                BAk = kb.bufs(32); BGk = kb.bufs(32)
                dgi = 0
                junkU = sb("junkU", [128, D], BF16, cs); BjU = kb.buf()
                gbi = 0
                pT2 = pF[2]; BpT2 = BpF[2]
                pT2v = pT2[:].rearrange("p (a b) -> p a b", b=128)
            ng_lim = dbg[3] if (dbg is not None and len(dbg) > 3) else NG
            for g in range(ng_lim):
                gs = slice(g * 512, (g + 1) * 512)
                dma("sp", lambda e: e.dma_start(out=MGg[:], in_=mgT[:, gs].rearrange("(c p) t -> p c t", p=128)), reads=[B_mg], writes=[BMGg])
                for tt in range(4):
                    i = g * 4 + tt
                    rows = slice(i * 128, (i + 1) * 128)
                    dma("sp", lambda e: e.dma_start(out=xin[:], in_=x[rows, :]), writes=[Bxin])
                    for half in range(2):
                        hsl = slice(half * 512, (half + 1) * 512)
                        for e_ in range(8):
                            op("pe", lambda e: e.matmul(pF[half][:], lhsT=MGg[:, e_, tt * 128:(tt + 1) * 128], rhs=woutb[:, e_, hsl], start=(e_ == 0), stop=(e_ == 7)),
                               reads=[BMGg, Bwo], writes=[BpF[half]])
                        op("dve", lambda e: e.tensor_tensor(out=x1t[:, hsl], in0=pF[half][:], in1=xin[:, hsl], op=ALU.add), reads=[BpF[half], Bxin], writes=[Bx1])
                    if dbg is not None and dbg[0] == "x1":
                        dma("sp", lambda e: e.dma_start(out=dbg_out[rows, :], in_=x1t[:]), reads=[Bx1])
                        continue
                    rms_rstd("P", x1t[:], D, ss2[:], junkF[:], Bx1, Bss2, BjF)
                    op("dve", lambda e: e.scalar_tensor_tensor(out=h2[:], in0=x1t[:], scalar=ss2[:], in1=g2_bc[:], op0=ALU.mult, op1=ALU.mult),
                       reads=[Bx1, Bss2, B_const], writes=[Bh2])
                    op("pool", lambda e: e.tensor_copy(out=h2b[:], in_=h2[:]), reads=[Bh2], writes=[Bh2b])
                    for c in range(8):
                        op("pe", lambda e: e.transpose(out=pT2v[:, c, :], in_=h2b[:, c * 128:(c + 1) * 128], identity=ident_b[:]), reads=[Bh2b, B_const], writes=[BpT2])
                    op("act", lambda e: e.copy(out=h2T[:], in_=pT2v), reads=[BpT2], writes=[Bh2T])
                    for hp in range(16):
                        bk = 3 + hp // 4
                        for c in range(8):
                            op("pe", lambda e: e.matmul(pF[bk][:, (hp % 4) * 128:(hp % 4 + 1) * 128], lhsT=wpq[:, c, hp * 128:(hp + 1) * 128], rhs=h2T[:, c, :], start=(c == 0), stop=(c == 7)),
                               reads=[Bwp, Bh2T], writes=[BpF[bk]])
                    for q in range(4):
                        op("act", lambda e: e.copy(out=QP[:, q * 4:(q + 1) * 4, :].rearrange("p a b -> p (a b)"), in_=pF[3 + q][:]), reads=[BpF[3 + q]], writes=[BQP])
                    for hp in range(16):
                        bk = 3 + hp // 4
                        op("pe", lambda e: e.matmul(pF[bk][:, (hp % 4) * 128:(hp % 4 + 1) * 128], lhsT=QP[:, hp, :], rhs=KEYT[:, hp, :], start=True, stop=True),
                           reads=[BQP, Bwp], writes=[BpF[bk]])
                    for q in range(4):
                        op("act", lambda e: e.copy(out=SCS[:, q * 4:(q + 1) * 4, :].rearrange("p a b -> p (a b)"), in_=pF[3 + q][:]), reads=[BpF[3 + q]], writes=[BSCS])
                    for hp in range(16):
                        op("dve", lambda e: e.max(out=v8[:, hp, 0:8], in_=SCS[:, hp, :]), reads=[BSCS], writes=[Bv8a[hp]])
                    for hp in range(16):
                        op("dve", lambda e: e.max_index(out=i8[:, hp, 0:8], in_max=v8[:, hp, 0:8], in_values=SCS[:, hp, :]), reads=[BSCS, Bv8a[hp]], writes=[Bi8a[hp]])
                    for hp in range(16):
                        op("dve", lambda e: e.match_replace(out=WK[:, hp, :], in_to_replace=v8[:, hp, 0:8], in_values=SCS[:, hp, :], imm_value=-1e30), reads=[BSCS, Bv8a[hp]], writes=[BWKa[hp]])
                    for hp in range(16):
                        op("dve", lambda e: e.max(out=v8[:, hp, 8:16], in_=WK[:, hp, :]), reads=[BWKa[hp]], writes=[Bv8b[hp]])
                    for hp in range(16):
                        op("dve", lambda e: e.max_index(out=i8[:, hp, 8:16], in_max=v8[:, hp, 8:16], in_values=WK[:, hp, :]), reads=[BWKa[hp], Bv8b[hp]], writes=[Bi8b[hp]])
                    op("dve", lambda e: e.tensor_copy(out=i8f[:], in_=i8[:]), reads=Bi8a + Bi8b, writes=[Bi8f])
                    v8v = v8[:].rearrange("p (h two) k -> p h two k", two=2)
                    i8v = i8f[:].rearrange("p (h two) k -> p h two k", two=2)
                    s0b = v8v[:, :, 0, :].unsqueeze(3).to_broadcast([128, 8, 16, 16])
                    s1b = v8v[:, :, 1, :].unsqueeze(2).to_broadcast([128, 8, 16, 16])
                    op("dve", lambda e: e.tensor_tensor(out=CAND[:].rearrange("p h (i j) -> p h i j", j=16), in0=s0b, in1=s1b, op=ALU.add), reads=Bv8a + Bv8b, writes=[BCAND])
                    for h in range(8):
                        op("dve", lambda e: e.max(out=b8[:, h, 0:8], in_=CAND[:, h, :]), reads=[BCAND], writes=[Bb8a[h]])
                    for h in range(8):
                        op("dve", lambda e: e.max_index(out=p8[:, h, 0:8], in_max=b8[:, h, 0:8], in_values=CAND[:, h, :]), reads=[BCAND, Bb8a[h]], writes=[Bp8a[h]])
                    for h in range(8):
                        op("dve", lambda e: e.match_replace(out=CW[:, h, :], in_to_replace=b8[:, h, 0:8], in_values=CAND[:, h, :], imm_value=-1e30), reads=[BCAND, Bb8a[h]], writes=[BCWa[h]])
                    for h in range(8):
                        op("dve", lambda e: e.max(out=b8[:, h, 8:16], in_=CW[:, h, :]), reads=[BCWa[h]], writes=[Bb8b[h]])
                    for h in range(8):
                        op("dve", lambda e: e.max_index(out=p8[:, h, 8:16], in_max=b8[:, h, 8:16], in_values=CW[:, h, :]), reads=[BCWa[h], Bb8b[h]], writes=[Bp8b[h]])
                    op("dve", lambda e: e.tensor_copy(out=pff[:], in_=p8[:]), reads=Bp8a + Bp8b, writes=[Bph])
                    pffb = pff[:].rearrange("p h k -> p (h k)").unsqueeze(2).to_broadcast([128, 128, 16])
                    io16m = iota16m[:].unsqueeze(1).to_broadcast([128, 128, 16])
                    op("dve", lambda e: e.tensor_tensor(out=EQ[:], in0=pffb, in1=io16m, op=ALU.is_ge), reads=[Bph, B_const], writes=[BEQ])
                    op("dve", lambda e: e.tensor_reduce(out=phf[:].rearrange("p h k -> p (h k)"), in_=EQ[:], axis=mybir.AxisListType.X, op=ALU.add), reads=[BEQ], writes=[Bph])
                    op("dve", lambda e: e.tensor_scalar(out=phf[:], in0=phf[:], scalar1=-1.0, scalar2=None, op0=ALU.add), reads=[Bph], writes=[Bph])
                    op("dve", lambda e: e.scalar_tensor_tensor(out=plf[:], in0=phf[:], scalar=-16.0, in1=pff[:], op0=ALU.mult, op1=ALU.add), reads=[Bph], writes=[Bph])
                    iob = iota16[:].unsqueeze(1).to_broadcast([128, 128, 16])
                    for (pf_, two, IDd) in ((phf, 0, ID0), (plf, 1, ID1)):
                        pfb = pf_[:].rearrange("p h k -> p (h k)").unsqueeze(2).to_broadcast([128, 128, 16])
                        op("dve", lambda e: e.tensor_tensor(out=EQ[:], in0=pfb, in1=iob, op=ALU.is_equal), reads=[Bph, B_const], writes=[BEQ])
                        tabb = i8v[:, :, two, :].unsqueeze(2).to_broadcast([128, 8, 16, 16])
                        op("dve", lambda e: e.tensor_tensor(out=EQ[:].rearrange("p (h k) i -> p h k i", k=16), in0=EQ[:].rearrange("p (h k) i -> p h k i", k=16), in1=tabb, op=ALU.mult),
                           reads=[BEQ, Bi8f], writes=[BEQ])
                        op("dve", lambda e: e.tensor_reduce(out=IDd[:], in_=EQ[:], axis=mybir.AxisListType.X, op=ALU.add), reads=[BEQ], writes=[BID])
                    op("dve", lambda e: e.scalar_tensor_tensor(out=ID0[:], in0=ID0[:], scalar=128.0, in1=ID1[:], op0=ALU.mult, op1=ALU.add), reads=[BID], writes=[BID])
                    op("dve", lambda e: e.tensor_scalar(out=IDXu[:], in0=ID0[:], scalar1=16383.0, scalar2=0.0, op0=ALU.min, op1=ALU.max), reads=[BID], writes=[BIDX])
                    op("dve", lambda e: e.tensor_scalar(out=NB[:], in0=b8[:, :, 0], scalar1=-1.0, scalar2=None, op0=ALU.mult), reads=Bb8a + Bb8b, writes=[BSM])
                    for h in range(8):
                        op("act", lambda e: e.activation(out=EX[:, h, :], in_=b8[:, h, :], func=AF.Exp, bias=NB[:, h:h + 1], scale=1.0, accum_out=Zs[:, h:h + 1]),
                           reads=Bb8a + Bb8b + [BSM], writes=[BSM])
                    op("dve", lambda e: e.reciprocal(out=Zs[:], in_=Zs[:]), reads=[BSM], writes=[BSM])
                    op("dve", lambda e: e.tensor_tensor(out=GATE[:].rearrange("p (h k) -> p h k", k=16), in0=EX[:], in1=Zs[:].unsqueeze(2).to_broadcast([128, 8, 16]), op=ALU.mult),
                       reads=[BSM], writes=[BGATE])
                    if dbg is not None and dbg[0] == "idx":
                        op("dve", lambda e: e.tensor_copy(out=yt[:, 0:128], in_=ID0[:]), reads=[BID], writes=[Byt])
                        op("dve", lambda e: e.tensor_copy(out=yt[:, 128:256], in_=GATE[:]), reads=[BGATE], writes=[Byt])
                        dma("sp", lambda e: e.dma_start(out=dbg_out[rows, :], in_=yt[:, 0:256]), reads=[Byt])
                        continue
                    sl_hist = {}
                    for kb4 in range(33):
                        if kb4 < 32:
                            sl4 = []
                            for k in range(kb4 * 4, kb4 * 4 + 4):
                                sgb = gbi % NGB; gbi += 1
                                sl4.append(sgb)
                                dma("pool", lambda e: e.indirect_dma_start(out=GB[sgb][:], out_offset=None, in_=uv_tab[:, :],
                                                                           in_offset=bass.IndirectOffsetOnAxis(ap=IDXu[:, k:k + 1], axis=0)),
                                    reads=[BIDX, B_uv], writes=[BGB[sgb]])
                                op("dve", lambda e: e.scalar_tensor_tensor(out=junkU[:], in0=GB[sgb][:, 0:D], scalar=1.0, in1=h2b[:], op0=ALU.mult, op1=ALU.mult, accum_out=ACTV[:, k:k + 1]),
                                   reads=[BGB[sgb], Bh2b], writes=[BjU, BAk[kb4]])
                            sl_hist[kb4] = sl4
                            k4 = slice(kb4 * 4, kb4 * 4 + 4)
                            op("act", lambda e: e.activation(out=G1[:, k4], in_=ACTV[:, k4], func=AF.Gelu), reads=[BAk[kb4]], writes=[BGk[kb4]])
                        if kb4 >= 1:
                            pb_ = kb4 - 1
                            k4p = slice(pb_ * 4, pb_ * 4 + 4)
                            op("dve", lambda e: e.tensor_tensor(out=WT[:, k4p], in0=G1[:, k4p], in1=GATE[:, k4p], op=ALU.mult), reads=[BGk[pb_], BGATE], writes=[BGk[pb_]])
                            for j4, k in enumerate(range(pb_ * 4, pb_ * 4 + 4)):
                                sgb = sl_hist[pb_][j4]
                                sd = dgi % 8; dgi += 1
                                op("dve", lambda e: e.tensor_scalar(out=DG[sd][:], in0=ident_b[:], scalar1=WT[:, k:k + 1], scalar2=None, op0=ALU.mult), reads=[BGk[pb_], B_const], writes=[BDG[sd]])
                                op("pe", lambda e: e.matmul(pF[0][:], lhsT=DG[sd][:], rhs=GB[sgb][:, D:D + 512], start=(k == 0), stop=(k == 127)), reads=[BDG[sd], BGB[sgb]], writes=[BpF[0]])
                                op("pe", lambda e: e.matmul(pF[1][:], lhsT=DG[sd][:], rhs=GB[sgb][:, D + 512:2 * D], start=(k == 0), stop=(k == 127)), reads=[BDG[sd], BGB[sgb]], writes=[BpF[1]])
                    op("dve", lambda e: e.tensor_tensor(out=x1t[:, 0:512], in0=x1t[:, 0:512], in1=pF[0][:], op=ALU.add), reads=[Bx1, BpF[0]], writes=[Bx1])
                    op("dve", lambda e: e.tensor_tensor(out=x1t[:, 512:D], in0=x1t[:, 512:D], in1=pF[1][:], op=ALU.add), reads=[Bx1, BpF[1]], writes=[Bx1])
                    rms_rstd("F", x1t[:], D, ss2[:], junkF[:], Bx1, Bss2, BjF)
                    op("dve", lambda e: e.scalar_tensor_tensor(out=yt[:], in0=x1t[:], scalar=ss2[:], in1=gf_bc[:], op0=ALU.mult, op1=ALU.mult),
                       reads=[Bx1, Bss2, B_const], writes=[Byt])
                    dma("sp", lambda e: e.dma_start(out=(dbg_out if dbg is not None else y_out)[rows, :], in_=yt[:]), reads=[Byt])
            kb.barrier()

        kb.finish()
    return nc, in_names


_CONSTS = None


def _consts():
    global _CONSTS
    if _CONSTS is None:
        j = np.arange(128)
        _CONSTS = {
            "c_ident": np.eye(128, dtype=np.float32),
            "c_ut": (j[:, None] <= j[None, :]).astype(np.float32),
            "c_lt": (j[:, None] >= j[None, :]).astype(np.float32),
            "c_iota": np.tile(np.concatenate([np.arange(16), np.arange(16) * 16, np.tile([1.0, 0.0], NT)]).astype(np.float32)[None, :], (128, 1)),
        }
    return _CONSTS


def make_in_maps(inputs):
    f = lambda a: np.ascontiguousarray(np.asarray(a, dtype=np.float32))
    shared = {
        "norm1_g": f(inputs["norm1_g"][0]), "w_in": f(inputs["w_in"][0]), "b_gates": f(inputs["b_gates"][0]),
        "mlstm_conv_w": f(inputs["mlstm_conv_w"][0]), "mlstm_conv_b": f(inputs["mlstm_conv_b"][0]),
        "mlstm_w_q": f(inputs["mlstm_w_q"][0]), "mlstm_w_k": f(inputs["mlstm_w_k"][0]), "mlstm_w_v": f(inputs["mlstm_w_v"][0]),
        "mlstm_norm_g": f(inputs["mlstm_norm_g"][0]), "lru_conv_w": f(inputs["lru_conv_w"][0]), "lru_conv_b": f(inputs["lru_conv_b"][0]),
        "lru_w_r": f(inputs["lru_w_r"][0]), "lru_b_r": f(inputs["lru_b_r"][0]), "lru_w_i": f(inputs["lru_w_i"][0]),
        "lru_b_i": f(inputs["lru_b_i"][0]), "lru_lambda": f(inputs["lru_lambda"][0]),
        "w_branch_mlstm": f(inputs["w_branch_mlstm"][0]), "w_branch_lru": f(inputs["w_branch_lru"][0]), "w_out": f(inputs["w_out"][0]),
        "norm2_g": f(inputs["norm2_g"][0]), "peer_w_q": f(inputs["peer_w_q"][0]),
        "peer_sub_keys": f(inputs["peer_sub_keys"][0]).reshape(16, 128, 128),
        "peer_u": f(inputs["peer_u"][0]), "peer_v": f(inputs["peer_v"][0]), "final_norm_g": f(inputs["final_norm_g"]),
    }
    shared.update(_consts())
    xs = f(inputs["x"])
    return [dict(shared, x=xs[b]) for b in range(8)]


def kernel(**inputs):
    nc, names = build_program()
    in_maps = [{k: m[k] for k in names} for m in make_in_maps(inputs)]
    res = run_bass_kernel_spmd(nc, in_maps, core_ids=list(range(8)))
    return np.stack([np.asarray(r["y"], dtype=np.float32) for r in res.results], axis=0)
```
